# Optimizing a Trainium2 kernel written in Bass

```python
import jax
import jax.numpy as jnp
from jax import lax
import numpy as np

D_MODEL = 2048
BATCH = 4
SEQ = 2048
DEPTH = 1

CHUNK = 64
NORM_EPS = 1e-6

RW_HEAD_DIM = 64
RW_WIDTH = D_MODEL // 2
RW_HEADS = RW_WIDTH // RW_HEAD_DIM
RW_DECAY_LORA = 96
RW_A_LORA = 96
RW_GATE_LORA = 256
RW_LN_EPS = 64e-5
RW_COLS = 3 * RW_WIDTH + RW_DECAY_LORA + RW_A_LORA + RW_GATE_LORA

RET_HEADS = 8
RET_QK_DIM = 128
RET_V_DIM = 256
RET_QK_WIDTH = RET_HEADS * RET_QK_DIM
RET_V_WIDTH = RET_HEADS * RET_V_DIM
RET_COLS = 2 * RET_QK_WIDTH + 2 * RET_V_WIDTH
ROPE_BASE = 10000.0

GATE_COLS = 2 * D_MODEL
IN_COLS = RW_COLS + RET_COLS + GATE_COLS

N_GROUPS = 4
EXPERTS_PER_GROUP = 8
N_EXPERTS = N_GROUPS * EXPERTS_PER_GROUP
TOP_K = 2
EXPERT_FF = 512
MOE_BLOCK = 128

kernel_name = 'hybrid_rwkv7_retention_hmoe_block'


def rms_norm(x, g):
    xf = x.astype(jnp.float32)
    y = xf * lax.rsqrt(jnp.mean(xf * xf, axis=-1, keepdims=True) + NORM_EPS)
    return (y * g.astype(jnp.float32)).astype(x.dtype)


def rotary(t, pos):
    half = t.shape[-1] // 2
    inv = ROPE_BASE ** (-jnp.arange(half, dtype=jnp.float32) / half)
    ang = pos[:, None] * inv[None, :]
    cos = jnp.cos(ang)[None, :, None, :]
    sin = jnp.sin(ang)[None, :, None, :]
    t1, t2 = t[..., :half], t[..., half:]
    return jnp.concatenate([t1 * cos - t2 * sin, t1 * sin + t2 * cos], axis=-1)


def rwkv7_recurrence(r, w, k, v, a, b):
    bsz, _, nh, n = r.shape

    def step(state, inp):
        r_t, w_t, k_t, v_t, a_t, b_t = inp
        sa = jnp.einsum('bhvk,bhk->bhv', state, a_t)
        state = (state * w_t[:, :, None, :] + sa[..., None] * b_t[:, :, None, :]
                 + v_t[..., None] * k_t[:, :, None, :])
        return state, jnp.einsum('bhvk,bhk->bhv', state, r_t)

    xs = tuple(jnp.swapaxes(t, 0, 1) for t in (r, w, k, v, a, b))
    state0 = jnp.zeros((bsz, nh, n, n), jnp.float32)
    _, y = lax.scan(step, state0, xs)
    return jnp.swapaxes(y, 0, 1)


def rwkv7_time_mix(p, mu, w0, w2, a0, a2, g2, k_k, k_a, r_k, lnx_g, lnx_b):
    bsz, seq, _ = p.shape
    f32 = jnp.float32
    hd = (RW_HEADS, RW_HEAD_DIM)
    shp = (bsz, seq) + hd
    p_prev = jnp.pad(p[:, :-1], ((0, 0), (1, 0), (0, 0)))
    p = p + (p_prev - p) * mu
    cuts = [RW_WIDTH, 2 * RW_WIDTH, 3 * RW_WIDTH, 3 * RW_WIDTH + RW_DECAY_LORA,
            3 * RW_WIDTH + RW_DECAY_LORA + RW_A_LORA]
    r, k, v, xw, xa, xg = jnp.split(p, cuts, axis=-1)
    w_log = -jax.nn.softplus(-(w0 + jnp.tanh(xw) @ w2).astype(f32)) - 0.5
    decay = jnp.exp(-jnp.exp(w_log)).reshape(shp)
    a = jax.nn.sigmoid((a0 + xa @ a2).astype(f32)).reshape(shp)
    g = (jax.nn.sigmoid(xg) @ g2).astype(f32)
    r_h = r.astype(f32).reshape(shp)
    k_h = k.astype(f32).reshape(shp)
    v_h = v.astype(f32).reshape(shp)
    kk = k_h * k_k.astype(f32).reshape(hd)
    kk = kk * lax.rsqrt(jnp.maximum(jnp.sum(kk * kk, axis=-1, keepdims=True), 1e-24))
    k_h = k_h * (1.0 + (a - 1.0) * k_a.astype(f32).reshape(hd))
    y = rwkv7_recurrence(r_h, decay, k_h, v_h, -kk, kk * a)
    mean = jnp.mean(y, axis=-1, keepdims=True)
    var = jnp.mean(jnp.square(y - mean), axis=-1, keepdims=True)
    y = (y - mean) * lax.rsqrt(var + RW_LN_EPS)
    y = y * lnx_g.astype(f32).reshape(hd) + lnx_b.astype(f32).reshape(hd)
    bonus = jnp.sum(r_h * k_h * r_k.astype(f32).reshape(hd), axis=-1, keepdims=True) * v_h
    y = (y + bonus).reshape(bsz, seq, RW_WIDTH)
    return (y * g).astype(p.dtype)


def retention_mix(p, norm_g):
    bsz, seq, _ = p.shape
    f32 = jnp.float32
    n_chunks = seq // CHUNK
    q, k, v, g = jnp.split(p, [RET_QK_WIDTH, 2 * RET_QK_WIDTH, 2 * RET_QK_WIDTH + RET_V_WIDTH], axis=-1)
    pos = jnp.arange(seq, dtype=f32)
    q = rotary(q.astype(f32).reshape(bsz, seq, RET_HEADS, RET_QK_DIM), pos)
    k = rotary(k.astype(f32).reshape(bsz, seq, RET_HEADS, RET_QK_DIM), pos) * (RET_QK_DIM ** -0.5)
    cs = (bsz, n_chunks, CHUNK, RET_HEADS)
    q = q.reshape(cs + (RET_QK_DIM,))
    k = k.reshape(cs + (RET_QK_DIM,))
    v = v.astype(f32).reshape(cs + (RET_V_DIM,))
    log_gamma = jnp.log1p(-jnp.exp2(-5.0 - jnp.arange(RET_HEADS, dtype=f32)))
    idx = jnp.arange(CHUNK, dtype=f32)
    dist = jnp.abs(idx[:, None] - idx[None, :])
    d_intra = jnp.exp(dist[None] * log_gamma[:, None, None])
    scores = jnp.einsum('bnihd,bnjhd->bnhij', q, k) * d_intra
    o_intra = jnp.einsum('bnhij,bnjhe->bnihe', scores, v)
    k_decay = jnp.exp((CHUNK - 1.0 - idx)[:, None] * log_gamma[None, :])
    kv = jnp.einsum('bnjhd,jh,bnjhe->nbhde', k, k_decay, v)
    chunk_decay = jnp.exp(CHUNK * log_gamma)[:, None, None]

    def step(state, kv_n):
        return state * chunk_decay + kv_n, state

    _, s_prev = lax.scan(step, jnp.zeros(kv.shape[1:], f32), kv)
    q_decay = jnp.exp((idx + 1.0)[:, None] * log_gamma[None, :])
    o_inter = jnp.einsum('bnihd,ih,nbhde->bnihe', q, q_decay, s_prev)
    o = (o_intra + o_inter).reshape(bsz, seq, RET_HEADS, RET_V_DIM)
    o = o * lax.rsqrt(jnp.mean(o * o, axis=-1, keepdims=True) + NORM_EPS)
    o = o.reshape(bsz, seq, RET_V_WIDTH) * norm_g.astype(f32)
    return (o * jax.nn.silu(g.astype(f32))).astype(p.dtype)


def hier_moe(h, w_group, b_group, w_expert, b_expert, e_gate, e_up, e_down):
    bsz, seq, d = h.shape
    f32 = jnp.float32
    n_tok = bsz * seq
    hf = h.reshape(n_tok, d)
    g_logits = (hf @ w_group).astype(f32) + b_group.astype(f32)
    g_prob = jax.nn.softmax(g_logits, axis=-1)
    g_sel = jnp.argmax(g_logits, axis=-1)
    g_w = jnp.take_along_axis(g_prob, g_sel[:, None], axis=-1)
    e_logits = ((hf @ w_expert).astype(f32) + b_expert.astype(f32)).reshape(n_tok, N_GROUPS, EXPERTS_PER_GROUP)
    e_in = jnp.take_along_axis(e_logits, g_sel[:, None, None], axis=1)[:, 0]
    top_l, top_i = lax.top_k(e_in, TOP_K)
    top_w = jax.nn.softmax(top_l, axis=-1) * g_w
    expert_id = (g_sel[:, None] * EXPERTS_PER_GROUP + top_i).reshape(-1).astype(jnp.int32)
    token_id = jnp.repeat(jnp.arange(n_tok, dtype=jnp.int32), TOP_K)
    weight = top_w.reshape(-1)
    n_assign = n_tok * TOP_K
    order = jnp.argsort(expert_id)
    e_sorted = expert_id[order]
    counts = jax.ops.segment_sum(jnp.ones((n_assign,), jnp.int32), expert_id, num_segments=N_EXPERTS)
    starts = jnp.cumsum(counts) - counts
    padded = (counts + MOE_BLOCK - 1) // MOE_BLOCK * MOE_BLOCK
    pad_end = jnp.cumsum(padded)
    pad_start = pad_end - padded
    dest = pad_start[e_sorted] + jnp.arange(n_assign, dtype=jnp.int32) - starts[e_sorted]
    n_blocks = -(-n_assign // MOE_BLOCK) + N_EXPERTS
    n_slots = n_blocks * MOE_BLOCK
    slot_tok = jnp.full((n_slots,), n_tok, jnp.int32).at[dest].set(token_id[order])
    slot_w = jnp.zeros((n_slots,), f32).at[dest].set(weight[order])
    block_e = jnp.minimum(
        jnp.searchsorted(pad_end, jnp.arange(n_blocks, dtype=jnp.int32) * MOE_BLOCK, side='right'),
        N_EXPERTS - 1)
    h_pad = jnp.concatenate([hf, jnp.zeros((1, d), hf.dtype)], axis=0)

    def expert_block(args):
        tok_b, e = args
        xb = h_pad[tok_b]
        return (jax.nn.silu(xb @ e_gate[e]) * (xb @ e_up[e])) @ e_down[e]

    out = lax.map(expert_block, (slot_tok.reshape(n_blocks, MOE_BLOCK), block_e))
    out = out.reshape(n_slots, d).astype(f32) * slot_w[:, None]
    y = jax.ops.segment_sum(out, slot_tok, num_segments=n_tok + 1)[:n_tok]
    return y.reshape(bsz, seq, d).astype(h.dtype)


def setup_inputs(seed: int = 0) -> dict:
    key = jax.random.key(seed)
    ks = jax.random.split(key, 32)
    f32 = jnp.float32
    L = DEPTH

    def nrm(k, shape, scale):
        return jax.random.normal(k, shape, f32) * scale

    def gain(k, shape):
        return 1.0 + 0.01 * jax.random.normal(k, shape, f32)

    return {
        'x': nrm(ks[0], (BATCH, SEQ, D_MODEL), 1.0),
        'norm1_g': gain(ks[1], (L, D_MODEL)),
        'w_in': nrm(ks[2], (L, D_MODEL, IN_COLS), D_MODEL ** -0.5),
        'mu_shift': jax.random.uniform(ks[3], (L, RW_COLS), f32),
        'rw_w0': jax.random.uniform(ks[4], (L, RW_WIDTH), f32, -6.0, 0.0),
        'rw_w2': nrm(ks[5], (L, RW_DECAY_LORA, RW_WIDTH), 0.5 * RW_DECAY_LORA ** -0.5),
        'rw_a0': nrm(ks[6], (L, RW_WIDTH), 0.1),
        'rw_a2': nrm(ks[7], (L, RW_A_LORA, RW_WIDTH), 0.5 * RW_A_LORA ** -0.5),
        'rw_g2': nrm(ks[8], (L, RW_GATE_LORA, RW_WIDTH), RW_GATE_LORA ** -0.5),
        'rw_k_k': 0.85 + nrm(ks[9], (L, RW_WIDTH), 0.05),
        'rw_k_a': 1.0 + nrm(ks[10], (L, RW_WIDTH), 0.05),
        'rw_r_k': nrm(ks[11], (L, RW_WIDTH), 0.1),
        'rw_lnx_g': gain(ks[12], (L, RW_WIDTH)),
        'rw_lnx_b': nrm(ks[13], (L, RW_WIDTH), 0.01),
        'ret_norm_g': gain(ks[14], (L, RET_V_WIDTH)),
        'w_up_a': nrm(ks[15], (L, RW_WIDTH, D_MODEL), RW_WIDTH ** -0.5),
        'w_up_b': nrm(ks[16], (L, RET_V_WIDTH, D_MODEL), RET_V_WIDTH ** -0.5),
        'w_out': nrm(ks[17], (L, D_MODEL, D_MODEL), D_MODEL ** -0.5),
        'norm2_g': gain(ks[18], (L, D_MODEL)),
        'w_group': nrm(ks[19], (L, D_MODEL, N_GROUPS), D_MODEL ** -0.5),
        'b_group': nrm(ks[20], (L, N_GROUPS), 0.01),
        'w_expert': nrm(ks[21], (L, D_MODEL, N_EXPERTS), D_MODEL ** -0.5),
        'b_expert': nrm(ks[22], (L, N_EXPERTS), 0.01),
        'e_gate': nrm(ks[23], (L, N_EXPERTS, D_MODEL, EXPERT_FF), D_MODEL ** -0.5),
        'e_up': nrm(ks[24], (L, N_EXPERTS, D_MODEL, EXPERT_FF), D_MODEL ** -0.5),
        'e_down': nrm(ks[25], (L, N_EXPERTS, EXPERT_FF, D_MODEL), EXPERT_FF ** -0.5),
        'final_norm_g': gain(ks[26], (D_MODEL,)),
    }


def reference(x, norm1_g, w_in, mu_shift, rw_w0, rw_w2, rw_a0, rw_a2, rw_g2, rw_k_k, rw_k_a,
              rw_r_k, rw_lnx_g, rw_lnx_b, ret_norm_g, w_up_a, w_up_b, w_out, norm2_g,
              w_group, b_group, w_expert, b_expert, e_gate, e_up, e_down, final_norm_g):
    for l in range(DEPTH):
        h = rms_norm(x, norm1_g[l])
        p = h @ w_in[l]
        p_rw, p_ret, p_gate = jnp.split(p, [RW_COLS, RW_COLS + RET_COLS], axis=-1)
        o_a = rwkv7_time_mix(p_rw, mu_shift[l], rw_w0[l], rw_w2[l], rw_a0[l], rw_a2[l], rw_g2[l],
                             rw_k_k[l], rw_k_a[l], rw_r_k[l], rw_lnx_g[l], rw_lnx_b[l])
        o_b = retention_mix(p_ret, ret_norm_g[l])
        gate_a, gate_b = jnp.split(jax.nn.sigmoid(p_gate), 2, axis=-1)
        merged = gate_a * (o_a @ w_up_a[l]) + gate_b * (o_b @ w_up_b[l])
        x = x + merged @ w_out[l]
        x = x + hier_moe(rms_norm(x, norm2_g[l]), w_group[l], b_group[l], w_expert[l], b_expert[l],
                         e_gate[l], e_up[l], e_down[l])
    return rms_norm(x, final_norm_g)
```

```python
from contextlib import ExitStack, contextmanager
import math
import numpy as np
import ml_dtypes
import concourse.bass as bass
import concourse.mybir as mybir
from concourse.bass_utils import run_bass_kernel_spmd

F32 = mybir.dt.float32
BF16 = mybir.dt.bfloat16
AF = mybir.ActivationFunctionType
ALU = mybir.AluOpType
AX = mybir.AxisListType

ENGS = ("tensor", "vector", "scalar", "gpsimd", "sync")
EPOCH = 30000
NDMA = 8

DM = 2048
T = 2048
TO = 1024
RW_COLS = 3520
RET_OFF = 3520
GATE_OFF = 3520 + 6144
IN_COLS = 13760
EPS = 1e-6
RW_LN_EPS = 64e-5
CDEC = -math.exp(-0.5)
NEXP = 32
EFF = 512


class Prog:
    def __init__(self, nc):
        self.nc = nc
        self.es = ExitStack()
        self.stack = [self.es]
        self.q = {e: [] for e in ENGS}
        self.cnt = {e: 0 for e in ENGS}
        self.esem = {}
        self.nsem = 0
        for e in ENGS:
            self.esem[e] = self._newsem(f"e_{e}")
        self.dsem = {e: [self._newsem(f"d_{e}{i}") for i in range(NDMA)] for e in ("sync", "scalar", "gpsimd")}
        self.dcount = {e: [0] * NDMA for e in self.dsem}
        self.dnext = {e: 0 for e in self.dsem}
        self.lastw = {}
        self.reads = {}
        self.known = {e: {} for e in ENGS}
        self.ntensors = 0
        self.rec = None

    def record(self, fn):
        assert self.rec is None
        self.rec = []
        try:
            fn()
        finally:
            lst, self.rec = self.rec, None
        return lst

    def replay(self, item):
        kind, eng, a, reads, writes, kw = item
        if kind == "op":
            self.op(eng, a, reads, writes)
        else:
            self.dma(eng, a[0], a[1], reads=reads, writes=writes, **kw)

    @staticmethod
    def _fsize(x):
        sh = getattr(x, "shape", None)
        if sh is None:
            return 64
        n = 1
        for d in sh[1:]:
            n *= d
        return n

    def _cost(self, eng, reads, writes):
        if eng == "tensor":
            n = self._fsize(reads[1]) if len(reads) > 1 else 128
            return 70.0 + n / 2.0
        n = max([self._fsize(w) for w in writes] + [1])
        if eng == "vector":
            return 120.0 + n * 0.9
        if eng == "scalar":
            return 220.0 + n * 0.8
        if eng == "gpsimd":
            return 200.0 + n * 1.5
        return 100.0

    def schedule(self, items, lat=20.0):
        n = len(items)
        if n == 0:
            return
        lastw, readers = {}, {}
        preds = [set() for _ in range(n)]
        succs = [[] for _ in range(n)]
        dur = [0.0] * n
        engs = [None] * n
        for i, it in enumerate(items):
            kind, eng, a, reads, writes, extra = it
            engs[i] = eng
            dur[i] = extra if (kind == "op" and extra is not None) else 100.0
            rk = [k for r in reads for k in self._keys(r)]
            wk = [k for w in writes for k in self._keys(w)]
            for k in rk:
                if k in lastw:
                    preds[i].add(lastw[k])
                if k.startswith("bank"):
                    for j in readers.get(k, ()):
                        if engs[j] != eng:
                            preds[i].add(j)
            for k in wk:
                if k in lastw:
                    preds[i].add(lastw[k])
                for j in readers.get(k, ()):
                    preds[i].add(j)
            for k in rk:
                readers.setdefault(k, []).append(i)
            for k in wk:
                lastw[k] = i
                readers[k] = []
            preds[i].discard(i)
            for j in preds[i]:
                succs[j].append(i)
        dlat = [2000.0 if items[i][0] == "dma" else dur[i] for i in range(n)]
        prio = [0.0] * n
        for i in range(n - 1, -1, -1):
            m = 0.0
            for j in succs[i]:
                if prio[j] > m:
                    m = prio[j]
            prio[i] = dlat[i] + lat + m
        npred = [len(p) for p in preds]
        ready = [i for i in range(n) if npred[i] == 0]
        fin = [0.0] * n
        efree = {}
        order = []
        rdy_t = [0.0] * n
        while ready:
            best = None
            bkey = None
            for i in ready:
                st = max(efree.get(engs[i], 0.0), rdy_t[i])
                key = (st, -prio[i], i)
                if bkey is None or key < bkey:
                    bkey, best = key, i
            ready.remove(best)
            st = bkey[0]
            issue = dur[best] if items[best][0] == "op" else 60.0
            efree[engs[best]] = st + issue
            fin[best] = st + dlat[best]
            order.append(best)
            for j in succs[best]:
                t = fin[best] + (lat if engs[j] != engs[best] or engs[j] != "tensor" else 0.0)
                if t > rdy_t[j]:
                    rdy_t[j] = t
                npred[j] -= 1
                if npred[j] == 0:
                    ready.append(j)
        assert len(order) == n
        for i in order:
            self.replay(items[i])

    def merge(self, A, B):
        a, b = len(A), len(B)
        if a == 0:
            for it in B:
                self.replay(it)
            return
        done = 0
        for i, it in enumerate(A):
            self.replay(it)
            upto = ((i + 1) * b) // a
            while done < upto:
                self.replay(B[done])
                done += 1
        while done < b:
            self.replay(B[done])
            done += 1

    def region(self, fn):
        self.schedule(self.record(fn))

    def _newsem(self, name):
        self.nsem += 1
        return self.es.enter_context(self.nc.semaphore(f"{name}_{self.nsem}"))

    def sb(self, shape, dt=F32, name=None):
        self.ntensors += 1
        return self.stack[-1].enter_context(self.nc.sbuf_tensor(f"{name or 'sb'}_{self.ntensors}", list(shape), dt))

    def ps(self, shape, dt=F32, name=None):
        self.ntensors += 1
        return self.stack[-1].enter_context(self.nc.psum_tensor(f"{name or 'ps'}_{self.ntensors}", list(shape), dt))

    @contextmanager
    def scope(self):
        st = ExitStack()
        self.stack.append(st)
        try:
            yield
        finally:
            self.barrier()
            self.stack.pop()
            st.close()

    @staticmethod
    def _keys(x):
        if isinstance(x, (str, tuple)):
            return [x]
        t = getattr(x, "tensor", None)
        if t is None:
            return [x.name]
        name = t.name
        if name.startswith("bankw"):
            apl = x.ap
            rs = apl[0][0]
            col0 = x.offset % rs
            ext = 1 + sum((n - 1) * st for st, n in apl[1:])
            half = rs // 2
            ks = []
            if col0 < half:
                ks.append(name + "_lo")
            if col0 + ext > half:
                ks.append(name + "_hi")
            return ks
        return [name]

    def _need(self, eng, ev, waits):
        if ev is None:
            return
        _, sem, val = ev
        k = self.known[eng]
        if k.get(sem, 0) >= val:
            return
        k[sem] = val
        waits[sem] = max(waits.get(sem, 0), val)

    def _deps(self, eng, reads, writes):
        waits = {}
        for r in reads:
            for k in self._keys(r):
                ev = self.lastw.get(k)
                if ev is not None and not (ev[0] == eng and eng == "tensor"):
                    self._need(eng, ev, waits)
                if k.startswith("bank"):
                    for rv in self.reads.get(k, ()):
                        if rv[0] != eng:
                            self._need(eng, rv, waits)
        for w in writes:
            for k in self._keys(w):
                ev = self.lastw.get(k)
                if ev is not None and ev[0] != eng:
                    self._need(eng, ev, waits)
                for rv in self.reads.get(k, ()):
                    if rv[0] != eng:
                        self._need(eng, rv, waits)
        return waits

    def _commit(self, ev, reads, writes):
        for r in reads:
            for k in self._keys(r):
                self.reads.setdefault(k, []).append(ev)
        for w in writes:
            for k in self._keys(w):
                self.lastw[k] = ev
                self.reads[k] = []

    def op(self, eng, fn, reads=(), writes=(), cost=None):
        if self.rec is not None:
            if cost is None:
                cost = self._cost(eng, reads, writes)
            self.rec.append(("op", eng, fn, list(reads), list(writes), cost))
            return None
        waits = self._deps(eng, reads, writes)
        if self.cnt[eng] >= EPOCH:
            self.esem[eng] = self._newsem(f"e_{eng}")
            self.cnt[eng] = 0
        self.cnt[eng] += 1
        sem = self.esem[eng]
        val = self.cnt[eng]
        wl = list(waits.items())

        def run(e, fn=fn, wl=wl, sem=sem):
            for s, v in wl:
                e.wait_ge(s, v)
            fn(e).then_inc(sem, 1)
        self.q[eng].append(run)
        ev = (eng, sem, val)
        self._commit(ev, reads, writes)
        return ev

    def dma(self, eng, out, in_, reads=None, writes=None, **kw):
        reads = [in_] if reads is None else reads
        writes = [out] if writes is None else writes
        if self.rec is not None:
            self.rec.append(("dma", eng, (out, in_), list(reads), list(writes), kw))
            return None
        waits = self._deps(eng, reads, writes)
        i = self.dnext[eng]
        self.dnext[eng] = (i + 1) % NDMA
        sem = self.dsem[eng][i]
        prev = self.dcount[eng][i]
        if prev > 0 and self.known[eng].get(sem, 0) < 16 * prev:
            waits[sem] = 16 * prev
            self.known[eng][sem] = 16 * prev
        self.dcount[eng][i] = prev + 1
        val = 16 * (prev + 1)
        wl = list(waits.items())

        def run(e, wl=wl, sem=sem, out=out, in_=in_, kw=kw):
            for s, v in wl:
                e.wait_ge(s, v)
            e.dma_start(out=out, in_=in_, **kw).then_inc(sem, 16)
        self.q[eng].append(run)
        ev = ("dma_" + eng, sem, val)
        self._commit(ev, reads, writes)
        return ev

    def barrier(self):
        evs = []
        for e in ENGS:
            if self.cnt[e] > 0:
                evs.append((e, self.esem[e], self.cnt[e]))
        for e in self.dsem:
            for i in range(NDMA):
                if self.dcount[e][i] > 0:
                    evs.append(("dma_" + e, self.dsem[e][i], 16 * self.dcount[e][i]))
        for eng in ENGS:
            waits = {}
            for ev in evs:
                if ev[0] != eng:
                    self._need(eng, ev, waits)
            wl = list(waits.items())
            if wl:
                def run(e, wl=wl):
                    for s, v in wl:
                        e.wait_ge(s, v)
                self.q[eng].append(run)

    def emit(self):
        nc = self.nc
        with nc.Block() as block:
            @block.tensor
            def _(e):
                for f in self.q["tensor"]:
                    f(e)

            @block.vector
            def _(e):
                for f in self.q["vector"]:
                    f(e)

            @block.scalar
            def _(e):
                for f in self.q["scalar"]:
                    f(e)

            @block.gpsimd
            def _(e):
                for f in self.q["gpsimd"]:
                    f(e)

            @block.sync
            def _(e):
                for f in self.q["sync"]:
                    f(e)
        self.es.close()

    def mm(self, out, lhsT, rhs, start=True, stop=True):
        return self.op("tensor", lambda e: e.matmul(out, lhsT, rhs, start=start, stop=stop),
                       reads=[lhsT, rhs], writes=[out])

    def tr(self, out, in_, ident):
        return self.op("tensor", lambda e: e.transpose(out, in_, ident), reads=[in_, ident], writes=[out])

    def act(self, out, in_, func, bias=None, scale=None, accum_out=None):
        kw = {}
        reads = [in_]
        writes = [out]
        if bias is not None:
            kw["bias"] = bias
            if not isinstance(bias, (int, float)):
                reads.append(bias)
        if scale is not None:
            kw["scale"] = scale
            if not isinstance(scale, (int, float)):
                reads.append(scale)
        if accum_out is not None:
            kw["accum_out"] = accum_out
            writes.append(accum_out)
        return self.op("scalar", lambda e: e.activation(out, in_, func, **kw), reads=reads, writes=writes)

    def tt(self, out, in0, in1, op, eng="vector"):
        return self.op(eng, lambda e: e.tensor_tensor(out, in0, in1, op), reads=[in0, in1], writes=[out])

    def ts(self, out, in0, s1, s2=None, op0=ALU.mult, op1=None, eng="vector"):
        reads = [in0] + [s for s in (s1, s2) if s is not None and not isinstance(s, (int, float))]
        kw = {}
        if op1 is not None:
            kw["op1"] = op1
        return self.op(eng, lambda e: e.tensor_scalar(out, in0, s1, s2, op0, **kw), reads=reads, writes=[out])

    def stt(self, out, in0, scalar, in1, op0, op1):
        reads = [in0, in1] + ([scalar] if not isinstance(scalar, (int, float)) else [])
        return self.op("vector", lambda e: e.scalar_tensor_tensor(out, in0, scalar, in1, op0, op1),
                       reads=reads, writes=[out])

    def copy(self, out, in_, eng="vector"):
        if eng == "scalar":
            return self.op(eng, lambda e: e.copy(out, in_), reads=[in_], writes=[out])
        return self.op(eng, lambda e: e.tensor_copy(out, in_), reads=[in_], writes=[out])

    def memset(self, ap, val, eng="vector"):
        return self.op(eng, lambda e: e.memset(ap, val), reads=[], writes=[ap])

    def recip(self, out, in_):
        return self.op("vector", lambda e: e.reciprocal(out, in_), reads=[in_], writes=[out])

    def reduce(self, out, in_, op=ALU.add, axis=AX.X):
        return self.op("vector", lambda e: e.tensor_reduce(out, in_, axis, op), reads=[in_], writes=[out])


def build_program(stop_after=99):
    nc = bass.Bass("TRN2", target_bir_lowering=False)

    declared = []

    def din(name, shape, dt=F32, need=0):
        if stop_after < need:
            return None
        declared.append(name)
        return nc.dram_tensor(name, list(shape), dt, kind="ExternalInput").ap()

    x = din("x", [T, DM])
    g1T_d = din("g1T", [128, 16])
    w_lora = din("w_lora", [DM, 448])
    w_rw = din("w_rw", [8, DM, 384])
    w_ret = din("w_ret", [8, DM, 768], need=2)
    w_in = din("w_in", [DM, IN_COLS], need=3)
    lprm_d = din("lprm", [128, 4])
    rwprm_d = din("rwprm", [128, 8, 8])
    rw_w2 = din("rw_w2", [96, 1024])
    rw_a2 = din("rw_a2", [96, 1024])
    rw_g2 = din("rw_g2", [256, 1024])
    lnx_g = din("lnx_g", [1, 1024])
    lnx_b = din("lnx_b", [1, 1024])
    ret_g = din("ret_g", [1, 2048], need=2)
    w_up_a = din("w_up_a", [1024, DM], need=3)
    w_up_b = din("w_up_b", [2048, DM], need=3)
    w_out = din("w_out", [DM, DM], need=4)
    g2_d = din("g2", [1, DM], need=4)
    gf_d = din("gf", [1, DM], need=4)
    w_rt = din("w_rt", [DM, 36], need=4)
    b_rt = din("b_rt", [1, 36], need=4)
    e_gate = din("e_gate", [NEXP, DM, EFF], need=4)
    e_up = din("e_up", [NEXP, DM, EFF], need=4)
    e_down = din("e_down", [NEXP, EFF, DM], need=4)
    c_identb = din("c_identb", [128, 128], BF16)
    c_identf = din("c_identf", [128, 128])
    c_maskS = din("c_maskS", [128, 384])
    c_maskL = din("c_maskL", [128, 128])
    c_bones = din("c_bones", [128, 128], BF16)
    c_hmask = din("c_hmask", [128, 2])
    c_reset = din("c_reset", [128, 1024], BF16)
    c_cs = din("c_cs", [128, 16, 4, 64], need=2)
    c_rmask = din("c_rmask", [128, 8, 128], need=2)
    c_rdec = din("c_rdec", [128, 16], need=2)
    c_tri = din("c_tri", [128, 128], BF16, need=4)
    c_iota = din("c_iota", [128, 128], need=4)
    out = nc.dram_tensor("out", [TO, DM], F32, kind="ExternalOutput").ap()
    oaT_d = nc.dram_tensor("oaT_scr", [8, 128, TO], BF16, kind="Internal").ap()
    obT_d = nc.dram_tensor("obT_scr", [16, 128, TO], BF16, kind="Internal").ap()
    mT_d = nc.dram_tensor("mT_scr", [16, 128, TO], BF16, kind="Internal").ap()
    dbg = None
    if stop_after < 99:
        dbg = nc.dram_tensor("dbg", [24, 128, TO], BF16, kind="ExternalOutput").ap()

    P = Prog(nc)
    P.es.enter_context(nc.allow_low_precision("bf16 PE transposes write bf16 PSUM"))
    pb = [P.ps([128, 512], F32, f"bank{i}") for i in range(6)]
    pbw = P.ps([128, 1024], F32, "bankw")
    pb.append(pbw[:, 0:512])
    pb.append(pbw[:, 512:1024])

    def pbb(i):
        return pb[i][:].bitcast(BF16)

    identb = P.sb([128, 128], BF16, "identb")
    identf = P.sb([128, 128], F32, "identf")
    P.dma("sync", identb[:], c_identb)
    P.dma("sync", identf[:], c_identf)

    with P.scope():
        hT_hi = P.sb([128, 16, 1024], BF16, "hT_hi")
        with P.scope():
            hT_lo = P.sb([128, 16, 1024], BF16, "hT_lo")

            def hTs(c, t0, n):
                if t0 < 1024:
                    return hT_lo[:, c, t0:t0 + n]
                return hT_hi[:, c, t0 - 1024:t0 - 1024 + n]

            g1T = P.sb([128, 16], F32, "g1T")
            P.dma("sync", g1T[:], g1T_d)
            with P.scope():
                xin = [P.sb([128, DM], F32, f"xin{i}") for i in range(4)]
                xnb = [P.sb([128, DM], BF16, f"xnb{i}") for i in range(4)]
                junk = P.sb([128, DM], BF16, "junk")
                st = [P.sb([128, 4], F32, f"st{i}") for i in range(4)]
                def _phase0():
                    for i in range(16):
                        xi = xin[i % 4]
                        xb = xnb[i % 4]
                        s = st[i % 4]
                        P.dma("sync", xi[:], x[i * 128:(i + 1) * 128, :])
                        P.act(junk[:], xi[:], AF.Square, accum_out=s[:, 0:1])
                        P.ts(s[:, 1:2], s[:, 0:1], 1.0 / DM, EPS, op0=ALU.mult, op1=ALU.add)
                        P.act(s[:, 2:3], s[:, 1:2], AF.Sqrt)
                        P.recip(s[:, 3:4], s[:, 2:3])
                        P.ts(xb[:], xi[:], s[:, 3:4], None, op0=ALU.mult)
                        for c4 in range(4):
                            bk = pbb(c4 % 2)
                            for k in range(4):
                                c = c4 * 4 + k
                                P.tr(bk[:, k * 128:(k + 1) * 128], xb[:, c * 128:(c + 1) * 128], identb[:])
                            for k in range(4):
                                c = c4 * 4 + k
                                dst = hTs(c, i * 128, 128)
                                if c4 % 2 == 0:
                                    P.ts(dst, bk[:, k * 128:(k + 1) * 128], g1T[:, c:c + 1], None, op0=ALU.mult)
                                else:
                                    P.act(dst, bk[:, k * 128:(k + 1) * 128], AF.Copy, scale=g1T[:, c:c + 1])

                P.region(_phase0)

            rw_phase(P, nc, locals())
            if stop_after >= 2:
                ret_phase(P, nc, locals())
        if stop_after < 3:
            with P.scope():
                dt_ = P.sb([128, 24, TO], BF16, "dbgt")
                P.dma("sync", dt_[:, 0:8, :], oaT_d.rearrange("c p t -> p c t"))
                if stop_after >= 2:
                    P.dma("sync", dt_[:, 8:24, :], obT_d.rearrange("c p t -> p c t"))
                else:
                    P.memset(dt_[:, 8:24, :], 0.0)
                P.dma("sync", dbg.rearrange("c p t -> p c t"), dt_[:])
        else:
            gate_phase(P, nc, locals())
    if stop_after >= 4:
        tail_phase(P, nc, locals())
    else:
        with P.scope():
            zt = P.sb([128, DM], F32, "zt")
            P.memset(zt[:], 0.0)
            for i in range(8):
                P.dma("sync", out[i * 128:(i + 1) * 128, :], zt[:])
    P.barrier()
    P.emit()
    return nc, declared


def rw_phase(P, nc, L):
    pb = L["pb"]; pbb = L["pbb"]; hTs = L["hTs"]; identb = L["identb"]; pbw = L["pbw"]
    with P.scope():
        lprm = P.sb([128, 8], F32, "lprm")
        P.dma("sync", lprm[:, 0:4], L["lprm_d"])
        P.ts(lprm[:, 4:8], lprm[:, 0:4], -1.0, 1.0, op0=ALU.mult, op1=ALU.add)
        rwprm = P.sb([128, 8, 14], F32, "rwprm")
        onec = P.sb([128, 2], F32, "onec")
        P.memset(onec[:], 1.0)
        P.dma("sync", rwprm[:, :, 0:8], L["rwprm_d"])
        P.ts(rwprm[:, :, 8:11], rwprm[:, :, 0:3], -1.0, 1.0, op0=ALU.mult, op1=ALU.add)
        P.ts(rwprm[:, :, 11:12], rwprm[:, :, 6:7], -1.0, 1.0, op0=ALU.mult, op1=ALU.add)
        P.ts(rwprm[:, :, 12:14], rwprm[:, :, 3:5], -1.0, None, op0=ALU.mult)
        maskL = P.sb([128, 128], F32, "maskL")
        bones = P.sb([128, 128], BF16, "bones")
        hmask = P.sb([128, 2], F32, "hmask")
        reset = P.sb([128, 1024], BF16, "reset")
        P.dma("sync", maskL[:], L["c_maskL"])
        P.dma("sync", bones[:], L["c_bones"])
        P.dma("sync", hmask[:], L["c_hmask"])
        P.dma("sync", reset[:], L["c_reset"])
        w2 = P.sb([96, 1024], BF16, "w2")
        a2 = P.sb([96, 1024], BF16, "a2")
        g2 = P.sb([128, 2, 1024], BF16, "g2")
        P.dma("gpsimd", w2[:], L["rw_w2"])
        P.dma("gpsimd", a2[:], L["rw_a2"])
        P.dma("gpsimd", g2[:], L["rw_g2"].rearrange("(c p) n -> p c n", p=128))
        lgb = P.sb([64, 256], F32, "lgb")

        txw = P.sb([96, T], BF16, "txw")
        xab = P.sb([96, T], BF16, "xab")
        sgx = P.sb([128, 2, TO], BF16, "sgx")
        saved = P.sb([128, 4], F32, "saved")

        with P.scope():
            wl = P.sb([128, 16, 448], BF16, "wl")
            raw = P.sb([128, 1025], F32, "raw")
            tmpF = P.sb([128, 1024], F32, "tmpF")

            def project(wt, c0, M, t0):
                for blk in range(2):
                    ps = pb[blk % 2]
                    for c in range(16):
                        P.mm(ps[0:M, :], wt[:, c, c0:c0 + M], hTs(c, t0 + blk * 512, 512),
                             start=(c == 0), stop=(c == 15))
                    P.act(raw[0:M, 1 + blk * 512:1 + (blk + 1) * 512], ps[0:M, :], AF.Copy)
            P.dma("gpsimd", wl[:], L["w_lora"].rearrange("(c p) n -> p c n", p=128))
            svl = P.sb([128, 4], F32, "svl")
            def _lora():
                for half in range(2):
                    t0 = half * 1024
                    for gi, (c0, M) in enumerate([(0, 96), (96, 96), (192, 128), (320, 128)]):
                        if half == 0:
                            P.memset(raw[0:M, 0:1], 0.0)
                        else:
                            P.copy(raw[0:M, 0:1], svl[0:M, gi:gi + 1])
                        project(wl, c0, M, t0)
                        tmp = tmpF[0:M, :]
                        P.ts(tmp, raw[0:M, 1:1025], lprm[0:M, 4 + gi:5 + gi], None, op0=ALU.mult)
                        P.stt(tmp, raw[0:M, 0:1024], lprm[0:M, gi:gi + 1], tmp, ALU.mult, ALU.add)
                        P.copy(svl[0:M, gi:gi + 1], raw[0:M, 1024:1025])
                        if gi == 0:
                            P.act(txw[:, t0:t0 + 1024], tmp, AF.Tanh)
                        elif gi == 1:
                            P.act(xab[:, t0:t0 + 1024], tmp, AF.Copy)
                        elif half == 1:
                            P.act(sgx[:, gi - 2, :], tmp, AF.Sigmoid)

            P.region(_lora)

        UL = 512
        NCU = 8
        ARx = [P.sb([128, NCU, 192], BF16, f"ARx{i}") for i in range(2)]
        BKx = [P.sb([128, NCU, 2, 2, 64], BF16, f"BKx{i}") for i in range(2)]
        BHx = [P.sb([128, NCU, 2, 64], BF16, f"BHx{i}") for i in range(2)]
        KHx = [P.sb([128, NCU, 2, 64], BF16, f"KHx{i}") for i in range(2)]
        Vx = [P.sb([128, NCU, 2, 64], BF16, f"Vx{i}") for i in range(2)]
        Vc = [P.sb([128, UL], BF16, f"Vc{i}") for i in range(2)]
        RKb = [P.sb([128, UL], BF16, f"RKb{i}") for i in range(2)]
        Pall = [P.sb([128, NCU, 128], BF16, f"Pall{i}") for i in range(2)]
        WC = [P.sb([128, NCU], F32, f"WC{i}") for i in range(2)]
        for lst in (ARx, BKx, BHx, KHx, Vx):
            for tl in lst:
                P.memset(tl[:], 0.0, eng="gpsimd")
        Hst = P.sb([128, 128], F32, "Hst")
        Hbf = P.sb([128, 128], BF16, "Hbf")
        wts = [P.sb([128, 16, 128], BF16, f"wts{i}") for i in range(3)]
        rksel = P.sb([128, 2], BF16, "rksel")
        maskS = P.sb([128, 384], F32, "maskS")
        P.dma("sync", maskS[:], L["c_maskS"])
        SCm = [P.sb([128, 384], BF16, f"SCm{i}") for i in range(2)]
        TM = [P.sb([128, 384], BF16, f"TM{i}") for i in range(2)]
        TMc = [P.sb([64, 2, 128], BF16, f"TMc{i}") for i in range(2)]
        Zs = [P.sb([128, 128], BF16, f"Zs{i}") for i in range(2)]
        Us = [P.sb([128, 128], BF16, f"Us{i}") for i in range(2)]
        XXg = [[P.sb([128, 1024], BF16, f"XX{g_}_{i}") for i in range(2)] for g_ in range(2)]
        Pmg = [P.sb([128, 512], BF16, f"Pm{g_}") for g_ in range(2)]
        ep = [P.sb([64, 32], F32, f"ep{i}") for i in range(2)]
        ey = [P.sb([64, 256], F32, f"ey{i}") for i in range(2)]
        ey2 = [P.sb([64, 256], F32, f"eyb{i}") for i in range(2)]
        eg = [P.sb([64, 256], F32, f"eg{i}") for i in range(2)]
        eo = [P.sb([64, 256], BF16, f"eo{i}") for i in range(2)]
        oaT_st = P.sb([128, 1024], BF16, "oaT_st")
        mS = maskS[:, 0:128]
        Fq = [P.sb([128, UL], F32, f"Fq{i}") for i in range(6)]
        Bq = [P.sb([128, UL], BF16, f"Bq{i}") for i in range(4)]
        rawq = P.sb([128, UL + 1], F32, "rawq")

        def v3(t_, hs):
            return t_[hs, :].rearrange("p (c t) -> p c t", t=64)

        def wload(p, xi):
            P.dma("gpsimd", wts[xi][:], L["w_rw"][p].rearrange("(c p) n -> p c n", p=128)[:, :, xi * 128:(xi + 1) * 128])

        def prep(p, q, ub):
            t0 = q * UL
            need_y = q >= 2
            prm = rwprm[:, p, :]
            cs = slice(p * 128, (p + 1) * 128)
            Rf, Kf, T1, SGb, Epos, CUMb = Fq
            ALb, KKb, RNb, KK2b = Bq
            arx, bkx, bhx, khx, vx, vc, rkb, pall, wc = ARx[ub], BKx[ub], BHx[ub], KHx[ub], Vx[ub], Vc[ub], RKb[ub], Pall[ub], WC[ub]
            for xi, dst in enumerate((Rf, Kf, T1)):
                if xi == 0 and not need_y:
                    if q == 1:
                        for c in range(16):
                            P.mm(pb[0][:, 0:2], wts[0][:, c, :], hTs(c, t0 + UL - 2, 2), start=(c == 0), stop=(c == 15))
                        P.act(saved[:, 0:1], pb[0][:, 1:2], AF.Copy)
                    continue
                if q == 0:
                    P.memset(rawq[:, 0:1], 0.0, eng="gpsimd")
                else:
                    P.copy(rawq[:, 0:1], saved[:, xi:xi + 1])
                ps = pb[xi % 2]
                for c in range(16):
                    P.mm(ps[:], wts[xi][:, c, :], hTs(c, t0, UL), start=(c == 0), stop=(c == 15))
                if q == 3 and p + 1 < 8:
                    wload(p + 1, xi)
                P.act(rawq[:, 1:UL + 1], ps[:], AF.Copy)
                P.act(dst[:], ps[:], AF.Copy, scale=prm[:, 8 + xi:9 + xi])
                P.stt(dst[:], rawq[:, 0:UL], prm[:, xi:xi + 1], dst[:], ALU.mult, ALU.add)
                P.copy(saved[:, xi:xi + 1], rawq[:, UL:UL + 1])
            for h in range(2):
                hs = slice(h * 64, (h + 1) * 64)
                P.act(vx[hs, :, h, :], v3(T1, hs), AF.Copy)
            if need_y:
                P.act(vc[:], T1[:], AF.Copy)
            P.mm(pb[0][:], w2[:, cs], txw[:, t0:t0 + UL])
            P.act(SGb[:], pb[0][:], AF.Exp, scale=-1.0, bias=prm[:, 12:13])
            P.act(SGb[:], SGb[:], AF.Ln, bias=onec[:, 0:1])
            P.act(SGb[:], SGb[:], AF.Exp, scale=-1.0)
            P.mm(pb[1][:], a2[:, cs], xab[:, t0:t0 + UL])
            P.act(ALb[:], pb[1][:], AF.Exp, scale=-1.0, bias=prm[:, 13:14])
            P.act(ALb[:], ALb[:], AF.Ln, bias=onec[:, 0:1])
            P.act(ALb[:], ALb[:], AF.Exp, scale=-1.0)
            P.op("vector", lambda e, o=CUMb, r_=reset, s_=SGb: e.tensor_tensor_scan(o[:], r_[:, 0:UL], s_[:], 0.0, ALU.mult, ALU.add),
                 reads=[reset, SGb], writes=[CUMb])
            P.tt(T1[:], CUMb[:], SGb[:], ALU.subtract)
            P.act(T1[:], T1[:], AF.Exp, scale=CDEC)
            cv = CUMb[:].rearrange("p (c t) -> p c t", t=64)
            P.tt(SGb[:].rearrange("p (c t) -> p c t", t=64), cv[:, :, 63:64].to_broadcast([128, NCU, 64]), cv, ALU.subtract)
            P.act(SGb[:], SGb[:], AF.Exp, scale=CDEC)
            P.act(Epos[:], CUMb[:], AF.Exp, scale=CDEC)
            P.act(CUMb[:], CUMb[:], AF.Exp, scale=-CDEC)
            Eprev, Eend, Eneg = T1, SGb, CUMb
            P.copy(wc[:], Epos[:].rearrange("p (c t) -> p c t", t=64)[:, :, 63])
            P.act(KKb[:], Kf[:], AF.Copy, scale=prm[:, 5:6])
            P.tt(KK2b[:], KKb[:], KKb[:], ALU.mult)
            P.mm(pb[0][:], bones[:], KK2b[:])
            P.ts(RNb[:], pb[0][:], 1e-18, None, op0=ALU.max)
            P.act(RNb[:], RNb[:], AF.Ln)
            P.act(RNb[:], RNb[:], AF.Exp, scale=-0.5)
            P.tt(KKb[:], KKb[:], RNb[:], ALU.mult)
            for h in range(2):
                hs = slice(h * 64, (h + 1) * 64)
                P.tt(arx[hs, :, h * 64:(h + 1) * 64], v3(KKb, hs), v3(Eprev, hs), ALU.mult)
            P.tt(KKb[:], KKb[:], ALb[:], ALU.mult)
            P.ts(RNb[:], ALb[:], prm[:, 6:7], prm[:, 11:12], op0=ALU.mult, op1=ALU.add)
            P.tt(Kf[:], Kf[:], RNb[:], ALU.mult)
            for h in range(2):
                hs = slice(h * 64, (h + 1) * 64)
                e1 = "vector"
                e2 = "vector"
                P.tt(bkx[hs, :, 0, h, :], v3(KKb, hs), v3(Eneg, hs), ALU.mult, eng=e1)
                P.tt(bkx[hs, :, 1, h, :], v3(Kf, hs), v3(Eneg, hs), ALU.mult, eng=e2)
                P.tt(bhx[hs, :, h, :], v3(KKb, hs), v3(Eend, hs), ALU.mult, eng=e1)
                P.tt(khx[hs, :, h, :], v3(Kf, hs), v3(Eend, hs), ALU.mult, eng=e2)
            if need_y:
                P.tt(arx[:, :, 128:192], Rf[:].rearrange("p (c t) -> p c t", t=64),
                     Epos[:].rearrange("p (c t) -> p c t", t=64), ALU.mult)
                P.tt(rkb[:], Rf[:], Kf[:], ALU.mult)
            r4 = lambda t_: t_.rearrange("p (k n) -> p k n", n=128)
            for g in range(NCU // 4):
                XX = XXg[g % 2]
                Pm = Pmg[g % 2]
                xx = XX[0]
                for k in range(4):
                    c = g * 4 + k
                    bcols = bkx[:, c, 0, :, :].rearrange("p a b -> p (a b)")
                    acols = arx[:, c, 0:128]
                    P.mm(pbw[:, k * 128:(k + 1) * 128], bcols, acols)
                    P.mm(pbw[:, 512 + k * 128:512 + (k + 1) * 128], acols, bcols)
                P.tt(r4(xx[:, 0:512]), r4(pbw[:, 0:512]), mS.unsqueeze(1).to_broadcast([128, 4, 128]), ALU.mult)
                P.tt(r4(xx[:, 512:1024]), r4(pbw[:, 512:1024]), maskL[:].unsqueeze(1).to_broadcast([128, 4, 128]), ALU.mult)
                P.tt(r4(Pm[:]), L["identf"][:].unsqueeze(1).to_broadcast([128, 4, 128]), r4(xx[:, 0:512]), ALU.subtract)
                for lvl in range(6):
                    if lvl < 5:
                        for k in range(4):
                            ks = slice(k * 128, (k + 1) * 128)
                            kt = slice(512 + k * 128, 512 + (k + 1) * 128)
                            P.mm(pbw[:, ks], xx[:, kt], xx[:, ks])
                            P.mm(pbw[:, kt], xx[:, ks], xx[:, kt])
                    if lvl >= 1:
                        for k in range(4):
                            ks = slice(k * 128, (k + 1) * 128)
                            kt = slice(512 + k * 128, 512 + (k + 1) * 128)
                            P.mm(pb[1][:, ks], xx[:, kt], Pm[:, ks])
                    if lvl < 5:
                        xn = XX[(lvl + 1) % 2]
                        P.act(xn[:], pbw[:], AF.Copy)
                    if lvl >= 1:
                        if lvl == 5:
                            P.tt(pall[:, g * 4:(g + 1) * 4, :], r4(Pm[:]), r4(pb[1][:]), ALU.add)
                        else:
                            P.tt(Pm[:], Pm[:], pb[1][:], ALU.add)
                    if lvl < 5:
                        xx = xn

        def rec(p, q, ub):
            t0 = q * UL
            need_y = q >= 2
            prm = rwprm[:, p, :]
            cs = slice(p * 128, (p + 1) * 128)
            arx, bkx, bhx, khx, vx, vc, rkb, pall, wc = ARx[ub], BKx[ub], BHx[ub], KHx[ub], Vx[ub], Vc[ub], RKb[ub], Pall[ub], WC[ub]
            if q == 0:
                P.memset(Hst[:], 0.0)
                P.memset(Hbf[:], 0.0)
            if q == 2:
                P.ts(rksel[:], hmask[:], prm[:, 7:8], None, op0=ALU.mult)
                P.dma("sync", lgb[:, 0:128], L["lnx_g"][:, cs].partition_broadcast(64))
                P.dma("sync", lgb[:, 128:256], L["lnx_b"][:, cs].partition_broadcast(64))
            for c in range(NCU):
                par = c % 2
                sc = SCm[par]; tm = TM[par]; zs = Zs[par]; us = Us[par]
                ppar = (c // 2) % 2
                tmc = TMc[ppar]
                bcols = bkx[:, c, 0, :, :].rearrange("p a b -> p (a b)")
                kcols = bkx[:, c, 1, :, :].rearrange("p a b -> p (a b)")
                acols = arx[:, c, 0:128]
                rcols = arx[:, c, 128:192]
                nar = 192 if need_y else 128
                P.mm(pb[3][:, 0:nar], bcols, arx[:, c, 0:nar])
                P.mm(pb[3][:, 192:192 + nar], kcols, arx[:, c, 0:nar])
                s3 = lambda t_: t_[:, 0:384].rearrange("p (a n) -> p a n", n=192)[:, :, 0:nar]
                P.tt(s3(sc), s3(pb[3]), s3(maskS), ALU.mult)
                nabT, nrbT, nakT, nrkT = sc[:, 0:128], sc[:, 128:192], sc[:, 192:320], sc[:, 320:384]
                bT = pbb(2)
                P.tr(bT[:, 0:128], bhx[:, c, :, :].rearrange("p a b -> p (a b)"), identb[:])
                P.tr(bT[:, 128:256], khx[:, c, :, :].rearrange("p a b -> p (a b)"), identb[:])
                P.tr(bT[:, 256:384], vx[:, c, :, :].rearrange("p a b -> p (a b)"), identb[:])
                P.act(tm[:, 0:384], bT[:, 0:384], AF.Copy)
                if need_y:
                    P.tr(bT[0:64, 384:512], vc[:, c * 64:(c + 1) * 64], identb[:])
                    P.act(tmc[:, c % 2, :], bT[0:64, 384:512], AF.Copy)
                bht, kht, vt = tm[:, 0:128], tm[:, 128:256], tm[:, 256:384]
                P.mm(pb[4][:, 0:128], acols, Hbf[:], start=True, stop=False)
                P.mm(pb[4][:, 0:128], nakT, vt, start=False, stop=True)
                P.copy(zs[:], pb[4][:, 0:128])
                P.mm(pb[4][:, 128:256], pall[:, c, :], zs[:])
                P.act(us[:], pb[4][:, 128:256], AF.Copy, scale=-1.0)
                if need_y:
                    yc = slice((c % 2) * 128, (c % 2 + 1) * 128)
                    P.mm(pb[5][0:64, yc], rcols, Hbf[:], start=True, stop=False)
                    P.mm(pb[5][0:64, yc], nrbT, us[:], start=False, stop=False)
                    P.mm(pb[5][0:64, yc], nrkT, vt, start=False, stop=True)
                P.mm(pb[4][:, 256:384], bht, us[:], start=True, stop=False)
                P.mm(pb[4][:, 256:384], kht, vt, start=False, stop=True)
                P.stt(Hbf[:], Hst[:], wc[:, c:c + 1], pb[4][:, 256:384], ALU.mult, ALU.add)
                P.stt(Hst[:], Hst[:], wc[:, c:c + 1], pb[4][:, 256:384], ALU.mult, ALU.add)
                if need_y and c % 2 == 1:
                    e = ep[ppar]; y = ey[ppar]; y2 = ey2[ppar]; o = eo[ppar]; gg = eg[ppar]
                    to0 = (q - 2) * UL + (c - 1) * 64
                    tu0 = (c - 1) * 64
                    P.copy(y[:], pb[5][0:64, 0:256])
                    for j in range(2):
                        for kc in range(2):
                            P.mm(pb[5][0:64, 256 + j * 128:256 + (j + 1) * 128], sgx[:, kc, to0 + j * 64:to0 + (j + 1) * 64],
                                 g2[:, kc, cs], start=(kc == 0), stop=(kc == 1))
                    P.act(gg[:], pb[5][0:64, 256:512], AF.Copy)
                    for j in range(2):
                        P.mm(pb[3][0:64, 384 + 2 * j:386 + 2 * j], rkb[:, tu0 + j * 64:tu0 + (j + 1) * 64], rksel[:])
                    P.act(e[:, 24:28], pb[3][0:64, 384:388], AF.Copy)
                    y4 = y[:].rearrange("p (a v) -> p a v", v=64)
                    P.reduce(e[:, 0:4], y4)
                    P.tt(y2[:], y[:], y[:], ALU.mult)
                    P.reduce(e[:, 4:8], y2[:].rearrange("p (a v) -> p a v", v=64))
                    P.ts(e[:, 8:12], e[:, 0:4], 1.0 / 64, None, op0=ALU.mult)
                    P.tt(e[:, 12:16], e[:, 8:12], e[:, 8:12], ALU.mult)
                    P.ts(e[:, 16:20], e[:, 4:8], 1.0 / 64, RW_LN_EPS, op0=ALU.mult, op1=ALU.add)
                    P.tt(e[:, 16:20], e[:, 16:20], e[:, 12:16], ALU.subtract)
                    P.act(e[:, 16:20], e[:, 16:20], AF.Ln)
                    P.act(e[:, 20:24], e[:, 16:20], AF.Exp, scale=-0.5)
                    P.tt(y4, y4, e[:, 8:12].unsqueeze(2).to_broadcast([64, 4, 64]), ALU.subtract)
                    P.tt(y4, y4, e[:, 20:24].unsqueeze(2).to_broadcast([64, 4, 64]), ALU.mult)
                    y3 = y[:].rearrange("p (j n) -> p j n", n=128)
                    P.tt(y3, y3, lgb[:, 0:128].unsqueeze(1).to_broadcast([64, 2, 128]), ALU.mult)
                    P.tt(y3, y3, lgb[:, 128:256].unsqueeze(1).to_broadcast([64, 2, 128]), ALU.add)
                    P.tt(y2[:].rearrange("p (a v) -> p a v", v=64), tmc[:].rearrange("p j (h v) -> p (j h) v", v=64),
                         e[:, 24:28].unsqueeze(2).to_broadcast([64, 4, 64]), ALU.mult)
                    P.tt(y[:], y[:], y2[:], ALU.add)
                    P.tt(o[:], y[:], gg[:], ALU.mult)
                    for j in range(2):
                        P.tr(bT[:, 512 + j * 64:512 + (j + 1) * 64], o[:, j * 128:(j + 1) * 128], identb[0:64, 0:64])
                    P.act(oaT_st[:, to0:to0 + 128], bT[:, 512:640], AF.Copy)
            if q == 3:
                P.dma("sync", L["oaT_d"][p], oaT_st[:])

        units = [(p, q) for p in range(8) for q in range(4)]
        for xi in range(3):
            wload(0, xi)
        prep(0, 0, 0)
        WIN = 8
        for u0 in range(0, len(units), WIN):
            items = []
            for u in range(u0, min(u0 + WIN, len(units))):
                p, q = units[u]
                items += P.record(lambda p=p, q=q, u=u: rec(p, q, u % 2))
                if u + 1 < len(units):
                    pn, qn = units[u + 1]
                    items += P.record(lambda pn=pn, qn=qn, u=u: prep(pn, qn, (u + 1) % 2))
            P.schedule(items)


def ret_phase(P, nc, L):
    pb = L["pb"]; pbb = L["pbb"]; hTs = L["hTs"]; identb = L["identb"]
    with P.scope():
        cst = P.sb([128, 16, 4, 64], F32, "cst")
        rmask = P.sb([128, 8, 128], F32, "rmask")
        rdec = P.sb([128, 16], F32, "rdec")
        P.dma("sync", cst[:], L["c_cs"])
        P.dma("sync", rmask[:], L["c_rmask"])
        P.dma("sync", rdec[:], L["c_rdec"])
        retg = P.sb([128, 256], F32, "retg")
        wr = [P.sb([128, 16, 768], BF16, f"wr{i}") for i in range(2)]
        S = P.sb([128, 256], F32, "S")
        Sbf = P.sb([128, 256], BF16, "Sbf")
        obT_st = P.sb([128, 2, 1024], BF16, "obT_st")
        A_ = [P.sb([128, 2, 128], F32, f"rA{i}") for i in range(2)]
        B_ = [P.sb([128, 2, 128], F32, f"rB{i}") for i in range(2)]
        rot = [P.sb([128, 2, 128], F32, f"rot{i}") for i in range(2)]
        qkb = [P.sb([128, 3, 128], BF16, f"qkb{i}") for i in range(2)]
        qkT = [P.sb([128, 2, 128], BF16, f"qkT{i}") for i in range(2)]
        vb = [P.sb([128, 256], BF16, f"vb{i}") for i in range(2)]
        scm = [P.sb([128, 128], BF16, f"scm{i}") for i in range(2)]
        sg = [P.sb([128, 256], F32, f"sg{i}") for i in range(2)]
        ob = [P.sb([128, 256], BF16, f"ob{i}") for i in range(2)]
        rs = [P.sb([128, 4], F32, f"rs{i}") for i in range(2)]
        junk = P.sb([128, 256], BF16, "rjunk")
        onec2 = P.sb([128, 2], F32, "onec2")
        P.memset(onec2[:], 1.0)
        lgam = [math.log1p(-2.0 ** (-5.0 - h)) for h in range(8)]
        P.dma("gpsimd", wr[0][:], L["w_ret"][0].rearrange("(c p) n -> p c n", p=128))
        def _head(h):
            wt = wr[h % 2]
            if h + 1 < 8:
                P.dma("gpsimd", wr[(h + 1) % 2][:], L["w_ret"][h + 1].rearrange("(c p) n -> p c n", p=128))
            P.dma("sync", retg[:], L["ret_g"][:, h * 256:(h + 1) * 256].partition_broadcast(128))
            P.memset(S[:], 0.0)
            P.memset(Sbf[:], 0.0)
            g128 = math.exp(128.0 * lgam[h])
            def proj(i):
                par = i % 2
                need_o = i >= 8
                t0 = i * 128
                pa = pb[par]
                lo = 0 if need_o else 128
                for c in range(16):
                    P.mm(pa[:, lo:512], hTs(c, t0, 128), wt[:, c, lo:512], start=(c == 0), stop=(c == 15))
                if need_o:
                    pg = pb[6] if par == 0 else pb[7]
                    for c in range(16):
                        P.mm(pg[:, 0:256], hTs(c, t0, 128), wt[:, c, 512:768], start=(c == 0), stop=(c == 15))

            proj(0)
            for i in range(16):
                par = i % 2
                need_o = i >= 8
                t0 = i * 128
                pa = pb[par]
                pg = pb[6] if par == 0 else pb[7]
                A = A_[par]; Bm = B_[par]; ro = rot[par]; qk = qkb[par]; qT = qkT[par]
                for xi in ([0, 1] if need_o else [1]):
                    xs = pa[:, xi * 128:(xi + 1) * 128].rearrange("p (a d) -> p a d", d=64)
                    ci = 0 if xi == 0 else 2
                    cosb = cst[:, i, ci, :].unsqueeze(1).to_broadcast([128, 2, 64])
                    sinb = cst[:, i, ci + 1, :].unsqueeze(1).to_broadcast([128, 2, 64])
                    Av = A[:, xi, :].rearrange("p (a d) -> p a d", d=64)
                    Bv = Bm[:, xi, :].rearrange("p (a d) -> p a d", d=64)
                    P.tt(Av, xs, cosb, ALU.mult)
                    P.tt(Bv, xs, sinb, ALU.mult)
                    P.tt(ro[:, xi, 0:64], A[:, xi, 0:64], Bm[:, xi, 64:128], ALU.subtract)
                    P.tt(ro[:, xi, 64:128], Bm[:, xi, 0:64], A[:, xi, 64:128], ALU.add)
                if need_o:
                    P.act(qk[:, 0, :], ro[:, 0, :], AF.Copy, scale=rdec[:, h:h + 1])
                    P.act(qk[:, 1, :], ro[:, 1, :], AF.Copy)
                P.act(qk[:, 2, :], ro[:, 1, :], AF.Copy, scale=rdec[:, 8 + h:9 + h])
                P.act(vb[par][:], pa[:, 256:512], AF.Copy)
                if i + 1 < 16:
                    proj(i + 1)
                if need_o:
                    bT = pbb(5)
                    P.tr(bT[:, 0:128], qk[:, 0, :], identb[:])
                    P.tr(bT[:, 128:256], qk[:, 1, :], identb[:])
                    P.copy(qT[:].rearrange("p a d -> p (a d)"), bT[:, 0:256])
                    P.mm(pb[2][:, 0:128], qT[:, 1, :], qT[:, 0, :])
                    P.tt(scm[par][:], pb[2][:, 0:128], rmask[:, h, :], ALU.mult)
                    P.mm(pb[3][:, 0:256], scm[par][:], vb[par][:], start=True, stop=False)
                    P.mm(pb[3][:, 0:256], qT[:, 0, :], Sbf[:], start=False, stop=True)
                P.mm(pb[4][:, 0:256], qk[:, 2, :], vb[par][:])
                P.stt(S[:], S[:], g128, pb[4][:, 0:256], ALU.mult, ALU.add)
                P.act(Sbf[:], S[:], AF.Copy)
                if need_o:
                    r = rs[par]
                    P.act(junk[:], pb[3][:, 0:256], AF.Square, accum_out=r[:, 0:1])
                    P.ts(r[:, 1:2], r[:, 0:1], 1.0 / 256, EPS, op0=ALU.mult, op1=ALU.add)
                    P.act(r[:, 2:3], r[:, 1:2], AF.Ln)
                    P.act(r[:, 3:4], r[:, 2:3], AF.Exp, scale=-0.5)
                    P.act(sg[par][:], pg[:, 0:256], AF.Exp, scale=-1.0)
                    P.act(sg[par][:], sg[par][:], AF.Ln, bias=onec2[:, 0:1])
                    P.act(sg[par][:], sg[par][:], AF.Exp, scale=-1.0)
                    P.tt(sg[par][:], sg[par][:], pg[:, 0:256], ALU.mult)
                    P.tt(sg[par][:], sg[par][:], retg[:], ALU.mult)
                    P.stt(ob[par][:], pb[3][:, 0:256], r[:, 3:4], sg[par][:], ALU.mult, ALU.mult)
                    bT2 = pbb(5)
                    for ec in range(2):
                        P.tr(bT2[:, 256 + ec * 128:256 + (ec + 1) * 128], ob[par][:, ec * 128:(ec + 1) * 128], identb[:])
                    P.act(obT_st[:, :, (i - 8) * 128:(i - 7) * 128], bT2[:, 256:512].rearrange("p (a d) -> p a d", d=128), AF.Copy)
            for ec in range(2):
                P.dma("sync", L["obT_d"][h * 2 + ec], obT_st[:, ec, :])


        def _allheads():
            for h in range(8):
                _head(h)
        P.region(_allheads)


def gate_phase(P, nc, L):
    pb = L["pb"]; hT_hi = L["hT_hi"]; w_in = L["w_in"]
    with P.scope():
        oaT = P.sb([128, 8, TO], BF16, "oaT")
        obT = P.sb([128, 16, TO], BF16, "obT")
        P.dma("sync", oaT[:], L["oaT_d"].rearrange("c p t -> p c t"))
        P.dma("sync", obT[:], L["obT_d"].rearrange("c p t -> p c t"))
        GA_ = [P.sb([128, 16, 512], BF16, f"GA{i}") for i in range(2)]
        GB_ = [P.sb([128, 16, 512], BF16, f"GB{i}") for i in range(2)]
        UA_ = [P.sb([128, 8, 512], BF16, f"UA{i}") for i in range(2)]
        UB_ = [P.sb([128, 16, 512], BF16, f"UB{i}") for i in range(2)]
        sA = [P.sb([128, 512], F32, f"sA{i}") for i in range(2)]
        sB = [P.sb([128, 512], F32, f"sB{i}") for i in range(2)]
        mT = [P.sb([128, TO], BF16, f"mT{i}") for i in range(2)]
        wv = w_in.rearrange("(c p) n -> p c n", p=128)
        def _quad(q):
            GA, GB, UA, UB = GA_[q % 2], GB_[q % 2], UA_[q % 2], UB_[q % 2]
            P.dma("gpsimd", GA[:], wv[:, :, GATE_OFF + q * 512:GATE_OFF + (q + 1) * 512])
            P.dma("gpsimd", GB[:], wv[:, :, GATE_OFF + 2048 + q * 512:GATE_OFF + 2048 + (q + 1) * 512])
            P.dma("gpsimd", UA[:], L["w_up_a"].rearrange("(c p) n -> p c n", p=128)[:, :, q * 512:(q + 1) * 512])
            P.dma("gpsimd", UB[:], L["w_up_b"].rearrange("(c p) n -> p c n", p=128)[:, :, q * 512:(q + 1) * 512])
            for jj in range(4):
                j = q * 4 + jj
                cols = slice(jj * 128, (jj + 1) * 128)
                mt = mT[j % 2]
                for blk in range(2):
                    par = blk
                    bs = slice(blk * 512, (blk + 1) * 512)
                    b0, b1, b2, b3 = pb[par * 4], pb[par * 4 + 1], pb[par * 4 + 2], pb[par * 4 + 3]
                    for c in range(16):
                        P.mm(b0[:], GA[:, c, cols], hT_hi[:, c, bs], start=(c == 0), stop=(c == 15))
                    for c in range(16):
                        P.mm(b1[:], GB[:, c, cols], hT_hi[:, c, bs], start=(c == 0), stop=(c == 15))
                    for c in range(8):
                        P.mm(b2[:], UA[:, c, cols], oaT[:, c, bs], start=(c == 0), stop=(c == 7))
                    for c in range(16):
                        P.mm(b3[:], UB[:, c, cols], obT[:, c, bs], start=(c == 0), stop=(c == 15))
                    P.act(sA[par][:], b0[:], AF.Sigmoid)
                    P.act(sB[par][:], b1[:], AF.Sigmoid)
                    P.tt(sA[par][:], sA[par][:], b2[:], ALU.mult)
                    P.tt(sB[par][:], sB[par][:], b3[:], ALU.mult)
                    P.tt(mt[:, bs], sA[par][:], sB[par][:], ALU.add)
                P.dma("sync", L["mT_d"][j], mt[:])


        def _allq():
            for q in range(4):
                _quad(q)
        P.region(_allq)


def tail_phase(P, nc, L):
    pb = L["pb"]; pbb = L["pbb"]; identb = L["identb"]; identf = L["identf"]
    x = L["x"]; out = L["out"]
    with P.scope():
        x2 = P.sb([128, 8, DM], F32, "x2")
        with P.scope():
            mg = P.sb([128, 16, TO], BF16, "mg")
            P.dma("sync", mg[:], L["mT_d"].rearrange("c p t -> p c t"))
            Wo = [P.sb([128, 16, 512], BF16, f"Wo{i}") for i in range(2)]
            xin = [P.sb([128, 512], F32, f"xin3_{i}") for i in range(2)]
            wov = L["w_out"].rearrange("(c p) n -> p c n", p=128)
            P.dma("gpsimd", Wo[0][:], wov[:, :, 0:512])
            def _wout():
                k = 0
                for nb in range(4):
                    if nb + 1 < 4:
                        P.dma("gpsimd", Wo[(nb + 1) % 2][:], wov[:, :, (nb + 1) * 512:(nb + 2) * 512])
                    for i in range(8):
                        bk = pb[k % 4]
                        xi = xin[k % 2]
                        k += 1
                        P.dma("sync", xi[:], x[1024 + i * 128:1024 + (i + 1) * 128, nb * 512:(nb + 1) * 512])
                        for c in range(16):
                            P.mm(bk[:], mg[:, c, i * 128:(i + 1) * 128], Wo[nb % 2][:, c, :], start=(c == 0), stop=(c == 15))
                        P.tt(x2[:, i, nb * 512:(nb + 1) * 512], bk[:], xi[:], ALU.add)

            P.region(_wout)

        with P.scope():
            h2 = P.sb([128, 8, DM], BF16, "h2")
            asg = P.sb([128, 8, 32], F32, "asg")
            asgb = P.sb([128, 8, 32], BF16, "asgb")
            wmat = P.sb([128, 8, 32], F32, "wmat")
            pos = P.sb([128, 8, 32], F32, "pos")
            iota = P.sb([128, 128], F32, "iota")
            tri = P.sb([128, 128], BF16, "tri")
            onesb = P.sb([128, 128], BF16, "onesb")
            P.dma("sync", iota[:], L["c_iota"])
            P.dma("sync", tri[:], L["c_tri"])
            P.memset(onesb[:], 1.0)
            Wg = [P.sb([128, 16, EFF], BF16, "Wg0")]
            Wu = [P.sb([128, 16, EFF], BF16, "Wu0")]
            P.dma("gpsimd", Wg[0][:], L["e_gate"][0].rearrange("(c p) f -> p c f", p=128))
            P.dma("gpsimd", Wu[0][:], L["e_up"][0].rearrange("(c p) f -> p c f", p=128))
            with P.scope():
                g2b = P.sb([128, DM], F32, "g2b")
                P.dma("sync", g2b[:], L["g2_d"].partition_broadcast(128))
                wrt = P.sb([128, 16, 36], F32, "wrt")
                P.dma("sync", wrt[:], L["w_rt"].rearrange("(c p) n -> p c n", p=128))
                brt = P.sb([128, 36], F32, "brt")
                P.dma("sync", brt[:], L["b_rt"].partition_broadcast(128))
                h2f = [P.sb([128, DM], F32, f"h2f{i}") for i in range(2)]
                h2T = [P.sb([128, 16, 128], F32, f"h2T{i}") for i in range(2)]
                junk = P.sb([128, DM], BF16, "junk4")
                rr = [P.sb([128, 24], F32, f"rr{i}") for i in range(2)]
                lgt = [P.sb([128, 36], F32, f"lgt{i}") for i in range(2)]
                em = [P.sb([128, 3, 32], F32, f"em{i}") for i in range(2)]
                def _router():
                    for i in range(8):
                        par = i % 2
                        r = rr[par]; hf = h2f[par]; hT_ = h2T[par]; lg = lgt[par]; e_ = em[par]
                        P.act(junk[:], x2[:, i, :], AF.Square, accum_out=r[:, 0:1])
                        P.ts(r[:, 1:2], r[:, 0:1], 1.0 / DM, EPS, op0=ALU.mult, op1=ALU.add)
                        P.act(r[:, 2:3], r[:, 1:2], AF.Ln)
                        P.act(r[:, 3:4], r[:, 2:3], AF.Exp, scale=-0.5)
                        P.stt(hf[:], x2[:, i, :], r[:, 3:4], g2b[:], ALU.mult, ALU.mult)
                        P.act(h2[:, i, :], hf[:], AF.Copy)
                        for c4 in range(4):
                            bk = pb[c4 % 2]
                            for kk in range(4):
                                c = c4 * 4 + kk
                                P.tr(bk[:, kk * 128:(kk + 1) * 128], hf[:, c * 128:(c + 1) * 128], identf[:])
                            P.act(hT_[:, c4 * 4:(c4 + 1) * 4, :], bk[:].rearrange("p (a d) -> p a d", d=128), AF.Copy)
                        for c in range(16):
                            P.mm(pb[2][:, 0:36], hT_[:, c, :], wrt[:, c, :], start=(c == 0), stop=(c == 15))
                        P.tt(lg[:], pb[2][:, 0:36], brt[:], ALU.add)
                        P.reduce(r[:, 4:5], lg[:, 0:4], op=ALU.max)
                        P.ts(r[:, 8:12], lg[:, 0:4], r[:, 4:5], None, op0=ALU.is_equal)
                        P.ts(r[:, 5:6], r[:, 4:5], -1.0, None, op0=ALU.mult)
                        P.act(r[:, 12:16], lg[:, 0:4], AF.Exp, bias=r[:, 5:6], accum_out=r[:, 6:7])
                        P.recip(r[:, 7:8], r[:, 6:7])
                        P.ts(r[:, 16:20], r[:, 8:12], 1e30, -1e30, op0=ALU.mult, op1=ALU.add)
                        ev = e_[:, 0, :].rearrange("p (g k) -> p g k", k=8)
                        P.tt(ev, lg[:, 4:36].rearrange("p (g k) -> p g k", k=8),
                             r[:, 16:20].unsqueeze(2).to_broadcast([128, 4, 8]), ALU.add)
                        P.reduce(r[:, 20:21], e_[:, 0, :], op=ALU.max)
                        P.ts(e_[:, 1, :], e_[:, 0, :], r[:, 20:21], None, op0=ALU.is_equal)
                        P.stt(e_[:, 0, :], e_[:, 1, :], -1e30, e_[:, 0, :], ALU.mult, ALU.add)
                        P.reduce(r[:, 21:22], e_[:, 0, :], op=ALU.max)
                        P.ts(e_[:, 2, :], e_[:, 0, :], r[:, 21:22], None, op0=ALU.is_equal)
                        P.tt(r[:, 22:23], r[:, 20:21], r[:, 21:22], ALU.subtract)
                        P.act(r[:, 22:23], r[:, 22:23], AF.Exp, scale=-1.0)
                        P.ts(r[:, 22:23], r[:, 22:23], 1.0, None, op0=ALU.add)
                        P.recip(r[:, 22:23], r[:, 22:23])
                        P.tt(r[:, 22:23], r[:, 22:23], r[:, 7:8], ALU.mult)
                        P.tt(r[:, 23:24], r[:, 7:8], r[:, 22:23], ALU.subtract)
                        P.tt(asg[:, i, :], e_[:, 1, :], e_[:, 2, :], ALU.add)
                        P.copy(asgb[:, i, :], asg[:, i, :])
                        P.ts(wmat[:, i, :], e_[:, 1, :], r[:, 22:23], None, op0=ALU.mult)
                        P.stt(wmat[:, i, :], e_[:, 2, :], r[:, 23:24], wmat[:, i, :], ALU.mult, ALU.add)
                    for i in range(8):
                        for j in range(i):
                            P.mm(pb[3][:, 0:32], onesb[:], asgb[:, j, :], start=(j == 0), stop=False)
                        P.mm(pb[3][:, 0:32], tri[:], asgb[:, i, :], start=(i == 0), stop=True)
                        P.copy(pos[:, i, :], pb[3][:, 0:32])

                P.region(_router)

            Wg.append(P.sb([128, 16, EFF], BF16, "Wg1"))
            Wu.append(P.sb([128, 16, EFF], BF16, "Wu1"))
            Wd = [P.sb([128, 4, DM], BF16, f"Wd{i}") for i in range(1)]
            sel = [P.sb([128, 8, 128], BF16, f"sel{i}") for i in range(2)]
            selw = [P.sb([128, 8, 128], BF16, f"selw{i}") for i in range(2)]
            selwT = P.sb([128, 8, 128], BF16, "selwT")
            XeT = P.sb([128, 16, 128], BF16, "XeT")
            sgt = P.sb([128, 512], F32, "sgt")
            hidT = [P.sb([128, 4, 128], BF16, f"hidT{i}") for i in range(2)]
            ye = P.sb([128, DM], BF16, "ye")

            def load_e(e):
                P.dma("gpsimd", Wg[e % 2][:], L["e_gate"][e].rearrange("(c p) f -> p c f", p=128))
                P.dma("gpsimd", Wu[e % 2][:], L["e_up"][e].rearrange("(c p) f -> p c f", p=128))

            def stageA(e):
                par = e % 2
                if e + 1 < NEXP:
                    load_e(e + 1)
                sl = sel[par]; sw = selw[par]
                for i in range(8):
                    P.ts(sl[:, i, :], iota[:], pos[:, i, e:e + 1], asg[:, i, e:e + 1], op0=ALU.is_equal, op1=ALU.mult)
                    P.ts(sw[:, i, :], iota[:], pos[:, i, e:e + 1], wmat[:, i, e:e + 1], op0=ALU.is_equal, op1=ALU.mult)
                for c4 in range(4):
                    bk = pb[c4 % 2]
                    for kk in range(4):
                        c = c4 * 4 + kk
                        for i in range(8):
                            P.mm(bk[:, kk * 128:(kk + 1) * 128], h2[:, i, c * 128:(c + 1) * 128], sl[:, i, :],
                                 start=(i == 0), stop=(i == 7))
                    P.act(XeT[:, c4 * 4:(c4 + 1) * 4, :], bk[:].rearrange("p (a d) -> p a d", d=128), AF.Copy)
                for fc in range(4):
                    for c in range(16):
                        P.mm(pb[6][:, fc * 128:(fc + 1) * 128], Wg[par][:, c, fc * 128:(fc + 1) * 128], XeT[:, c, :],
                             start=(c == 0), stop=(c == 15))
                for fc in range(4):
                    for c in range(16):
                        P.mm(pb[7][:, fc * 128:(fc + 1) * 128], Wu[par][:, c, fc * 128:(fc + 1) * 128], XeT[:, c, :],
                             start=(c == 0), stop=(c == 15))
                P.act(sgt[:], pb[6][:], AF.Silu)
                P.tt(hidT[par][:].rearrange("p a d -> p (a d)"), sgt[:], pb[7][:], ALU.mult)

            def stageB(e):
                par = e % 2
                sw = selw[par]
                P.dma("gpsimd", Wd[0][:], L["e_down"][e].rearrange("(c p) n -> p c n", p=128))
                bT = pbb(5)
                for i in range(8):
                    P.tr(bT[:, i * 128:(i + 1) * 128], sw[:, i, :], identb[:])
                P.act(selwT[:].rearrange("p a d -> p (a d)"), bT[:, 0:1024], AF.Copy)
                for nb in range(4):
                    bk = pb[2 + nb % 2]
                    for fc in range(4):
                        P.mm(bk[:], hidT[par][:, fc, :], Wd[0][:, fc, nb * 512:(nb + 1) * 512], start=(fc == 0), stop=(fc == 3))
                    P.act(ye[:, nb * 512:(nb + 1) * 512], bk[:], AF.Copy)
                k = 0
                for i in range(8):
                    for nb in range(4):
                        bk = pb[2 + (k % 3)]
                        k += 1
                        P.mm(bk[:], selwT[:, i, :], ye[:, nb * 512:(nb + 1) * 512])
                        P.tt(x2[:, i, nb * 512:(nb + 1) * 512], x2[:, i, nb * 512:(nb + 1) * 512], bk[:], ALU.add)

            P.region(lambda: stageA(0))
            EW = 8
            for e0 in range(0, NEXP, EW):
                items = []
                for e in range(e0, min(e0 + EW, NEXP)):
                    items += P.record(lambda e=e: stageB(e))
                    if e + 1 < NEXP:
                        items += P.record(lambda e=e: stageA(e + 1))
                P.schedule(items)

        with P.scope():
            gfb = P.sb([128, DM], F32, "gfb")
            P.dma("sync", gfb[:], L["gf_d"].partition_broadcast(128))
            ot = [P.sb([128, DM], F32, f"ot{i}") for i in range(2)]
            junk = P.sb([128, DM], BF16, "junk5")
            rr = [P.sb([128, 4], F32, f"rf{i}") for i in range(2)]
            def _final():
                for i in range(8):
                    r = rr[i % 2]
                    P.act(junk[:], x2[:, i, :], AF.Square, accum_out=r[:, 0:1])
                    P.ts(r[:, 1:2], r[:, 0:1], 1.0 / DM, EPS, op0=ALU.mult, op1=ALU.add)
                    P.act(r[:, 2:3], r[:, 1:2], AF.Sqrt)
                    P.recip(r[:, 3:4], r[:, 2:3])
                    P.stt(ot[i % 2][:], x2[:, i, :], r[:, 3:4], gfb[:], ALU.mult, ALU.mult)
                    P.dma("sync", out[i * 128:(i + 1) * 128, :], ot[i % 2][:])

            P.region(_final)


def _constants(core_half):
    bf = ml_dtypes.bfloat16
    c = {}
    c["c_identb"] = np.eye(128, dtype=np.float32).astype(bf)
    c["c_identf"] = np.eye(128, dtype=np.float32)
    hp = np.arange(128) // 64
    sp = np.arange(128) % 64
    same = (hp[:, None] == hp[None, :])
    strict_bd = (same & (sp[:, None] < sp[None, :])).astype(np.float32)
    incl_c = (sp[:, None] <= np.arange(64)[None, :]).astype(np.float32)
    c["c_maskS"] = np.concatenate([strict_bd, incl_c, strict_bd, incl_c], axis=1)
    c["c_maskL"] = (same & (sp[None, :] < sp[:, None])).astype(np.float32)
    c["c_bones"] = same.astype(np.float32).astype(bf)
    c["c_hmask"] = np.stack([(hp == 0), (hp == 1)], axis=1).astype(np.float32)
    rs = np.ones((128, 1024), np.float32)
    rs[:, ::64] = 0.0
    c["c_reset"] = rs.astype(bf)
    pos = np.arange(T, dtype=np.float32) - (1024.0 if core_half == 0 else 0.0)
    inv = (10000.0 ** (-np.arange(64, dtype=np.float32) / 64)).astype(np.float32)
    ang = pos[:, None] * inv[None, :]
    cos, sin = np.cos(ang).astype(np.float32), np.sin(ang).astype(np.float32)
    sc = np.float32(128 ** -0.5)
    cs = np.stack([cos, sin, cos * sc, sin * sc], axis=1)
    c["c_cs"] = np.ascontiguousarray(cs.reshape(16, 128, 4, 64).transpose(1, 0, 2, 3))
    lg = np.log1p(-np.exp2(-5.0 - np.arange(8, dtype=np.float64)))
    idx = np.arange(128, dtype=np.float64)
    j = idx[:, None]
    i = idx[None, :]
    same_c = (j // 64 == i // 64)
    rm = np.zeros((128, 8, 128), np.float64)
    for h in range(8):
        m = np.where(same_c, np.exp(np.abs(i - j) * lg[h]), np.where(j < i, np.exp((i - j) * lg[h]), 0.0))
        rm[:, h, :] = m * np.exp(-(i + 1) * lg[h])
    c["c_rmask"] = rm.astype(np.float32)
    rd = np.zeros((128, 16), np.float64)
    for h in range(8):
        rd[:, h] = np.exp((idx + 1) * lg[h])
        rd[:, 8 + h] = np.exp((127 - idx) * lg[h])
    c["c_rdec"] = rd.astype(np.float32)
    c["c_tri"] = (idx[:, None] < idx[None, :]).astype(np.float32).astype(bf)
    c["c_iota"] = np.tile(np.arange(128, dtype=np.float32)[None, :], (128, 1))
    return c


def _prep_shared(inp):
    f = np.float32
    w_in = np.asarray(inp["w_in"][0], f)
    d = {}
    d["g1T"] = np.ascontiguousarray(np.asarray(inp["norm1_g"][0], f).reshape(16, 128).T)
    d["w_lora"] = np.ascontiguousarray(w_in[:, 3072:3520])
    d["w_rw"] = np.ascontiguousarray(np.stack(
        [np.concatenate([w_in[:, k * 1024 + p * 128:k * 1024 + (p + 1) * 128] for k in range(3)], axis=1) for p in range(8)]))
    ro = RET_OFF
    d["w_ret"] = np.ascontiguousarray(np.stack(
        [np.concatenate([w_in[:, ro + h * 128:ro + (h + 1) * 128],
                         w_in[:, ro + 1024 + h * 128:ro + 1024 + (h + 1) * 128],
                         w_in[:, ro + 2048 + h * 256:ro + 2048 + (h + 1) * 256],
                         w_in[:, ro + 4096 + h * 256:ro + 4096 + (h + 1) * 256]], axis=1) for h in range(8)]))
    d["w_in"] = w_in
    mu = np.asarray(inp["mu_shift"][0], f)
    lp = np.zeros((128, 4), f)
    lp[:96, 0] = mu[3072:3168]
    lp[:96, 1] = mu[3168:3264]
    lp[:, 2] = mu[3264:3392]
    lp[:, 3] = mu[3392:3520]
    d["lprm"] = lp
    vecs = [mu[0:1024], mu[1024:2048], mu[2048:3072], np.asarray(inp["rw_w0"][0], f), np.asarray(inp["rw_a0"][0], f),
            np.asarray(inp["rw_k_k"][0], f), np.asarray(inp["rw_k_a"][0], f), np.asarray(inp["rw_r_k"][0], f)]
    d["rwprm"] = np.ascontiguousarray(np.stack([v.reshape(8, 128).T for v in vecs], axis=2))
    d["rw_w2"] = np.asarray(inp["rw_w2"][0], f)
    d["rw_a2"] = np.asarray(inp["rw_a2"][0], f)
    d["rw_g2"] = np.asarray(inp["rw_g2"][0], f)
    d["lnx_g"] = np.asarray(inp["rw_lnx_g"][0], f).reshape(1, 1024)
    d["lnx_b"] = np.asarray(inp["rw_lnx_b"][0], f).reshape(1, 1024)
    d["ret_g"] = np.asarray(inp["ret_norm_g"][0], f).reshape(1, 2048)
    d["w_up_a"] = np.asarray(inp["w_up_a"][0], f)
    d["w_up_b"] = np.asarray(inp["w_up_b"][0], f)
    d["w_out"] = np.asarray(inp["w_out"][0], f)
    d["g2"] = np.asarray(inp["norm2_g"][0], f).reshape(1, DM)
    d["gf"] = np.asarray(inp["final_norm_g"], f).reshape(1, DM)
    d["w_rt"] = np.ascontiguousarray(np.concatenate([np.asarray(inp["w_group"][0], f), np.asarray(inp["w_expert"][0], f)], axis=1))
    d["b_rt"] = np.concatenate([np.asarray(inp["b_group"][0], f), np.asarray(inp["b_expert"][0], f)]).reshape(1, 36)
    d["e_gate"] = np.asarray(inp["e_gate"][0], f)
    d["e_up"] = np.asarray(inp["e_up"][0], f)
    d["e_down"] = np.asarray(inp["e_down"][0], f)
    return d


_CACHE = {}


def run(inp, stop_after=99, cores=None, trace=False):
    key = stop_after
    if key not in _CACHE:
        _CACHE[key] = build_program(stop_after)
    nc, declared = _CACHE[key]
    shared = _prep_shared(inp)
    consts = [_constants(0), _constants(1)]
    xfull = np.asarray(inp["x"], np.float32)
    cores = list(range(8)) if cores is None else cores
    in_maps = []
    for c in cores:
        b, half = c // 2, c % 2
        if half == 0:
            xs = np.concatenate([np.zeros((1024, DM), np.float32), xfull[b, :1024]], axis=0)
        else:
            xs = xfull[b]
        m = {"x": np.ascontiguousarray(xs)}
        m.update(shared)
        m.update(consts[half])
        in_maps.append({k: m[k] for k in declared})
    res = run_bass_kernel_spmd(nc, in_maps, core_ids=list(range(len(cores))), trace=trace)
    return res


def kernel(**inputs):
    res = run(inputs)
    outp = np.zeros((4, 2048, DM), np.float32)
    for c in range(8):
        b, half = c // 2, c % 2
        outp[b, half * 1024:(half + 1) * 1024] = res.results[c]["out"]
    return outp
```

```python
from contextlib import ExitStack, contextmanager
import math
import numpy as np
import ml_dtypes
import concourse.bass as bass
import concourse.mybir as mybir
from concourse.bass_utils import run_bass_kernel_spmd

F32 = mybir.dt.float32
BF16 = mybir.dt.bfloat16
AF = mybir.ActivationFunctionType
ALU = mybir.AluOpType
AX = mybir.AxisListType

ENGS = ("tensor", "vector", "scalar", "gpsimd", "sync")
EPOCH = 30000
NDMA = 8

DM = 2048
T = 2048
TO = 1024
RW_COLS = 3520
RET_OFF = 3520
GATE_OFF = 3520 + 6144
IN_COLS = 13760
EPS = 1e-6
RW_LN_EPS = 64e-5
CDEC = -math.exp(-0.5)
NEXP = 32
EFF = 512


class Prog:
    def __init__(self, nc):
        self.nc = nc
        self.es = ExitStack()
        self.stack = [self.es]
        self.q = {e: [] for e in ENGS}
        self.cnt = {e: 0 for e in ENGS}
        self.esem = {}
        self.nsem = 0
        for e in ENGS:
            self.esem[e] = self._newsem(f"e_{e}")
        self.dsem = {e: [self._newsem(f"d_{e}{i}") for i in range(NDMA)] for e in ("sync", "scalar", "gpsimd")}
        self.dcount = {e: [0] * NDMA for e in self.dsem}
        self.dnext = {e: 0 for e in self.dsem}
        self.lastw = {}
        self.reads = {}
        self.known = {e: {} for e in ENGS}
        self.ntensors = 0
        self.rec = None

    def record(self, fn):
        assert self.rec is None
        self.rec = []
        try:
            fn()
        finally:
            lst, self.rec = self.rec, None
        return lst

    def replay(self, item):
        kind, eng, a, reads, writes, kw = item
        if kind == "op":
            self.op(eng, a, reads, writes)
        else:
            self.dma(eng, a[0], a[1], reads=reads, writes=writes, **kw)

    @staticmethod
    def _fsize(x):
        sh = getattr(x, "shape", None)
        if sh is None:
            return 64
        n = 1
        for d in sh[1:]:
            n *= d
        return n

    def _cost(self, eng, reads, writes):
        if eng == "tensor":
            n = self._fsize(reads[1]) if len(reads) > 1 else 128
            return 70.0 + n / 2.0
        n = max([self._fsize(w) for w in writes] + [1])
        if eng == "vector":
            return 120.0 + n * 0.9
        if eng == "scalar":
            return 220.0 + n * 0.8
        if eng == "gpsimd":
            return 200.0 + n * 1.5
        return 100.0

    def schedule(self, items, lat=20.0):
        n = len(items)
        if n == 0:
            return
        lastw, readers = {}, {}
        preds = [set() for _ in range(n)]
        succs = [[] for _ in range(n)]
        dur = [0.0] * n
        engs = [None] * n
        for i, it in enumerate(items):
            kind, eng, a, reads, writes, extra = it
            engs[i] = eng
            dur[i] = extra if (kind == "op" and extra is not None) else 100.0
            rk = [k for r in reads for k in self._keys(r)]
            wk = [k for w in writes for k in self._keys(w)]
            for k in rk:
                if k in lastw:
                    preds[i].add(lastw[k])
                if k.startswith("bank"):
                    for j in readers.get(k, ()):
                        if engs[j] != eng:
                            preds[i].add(j)
            for k in wk:
                if k in lastw:
                    preds[i].add(lastw[k])
                for j in readers.get(k, ()):
                    preds[i].add(j)
            for k in rk:
                readers.setdefault(k, []).append(i)
            for k in wk:
                lastw[k] = i
                readers[k] = []
            preds[i].discard(i)
            for j in preds[i]:
                succs[j].append(i)
        dlat = [2000.0 if items[i][0] == "dma" else dur[i] for i in range(n)]
        prio = [0.0] * n
        for i in range(n - 1, -1, -1):
            m = 0.0
            for j in succs[i]:
                if prio[j] > m:
                    m = prio[j]
            prio[i] = dlat[i] + lat + m
        npred = [len(p) for p in preds]
        ready = [i for i in range(n) if npred[i] == 0]
        fin = [0.0] * n
        efree = {}
        order = []
        rdy_t = [0.0] * n
        while ready:
            best = None
            bkey = None
            for i in ready:
                st = max(efree.get(engs[i], 0.0), rdy_t[i])
                key = (st, -prio[i], i)
                if bkey is None or key < bkey:
                    bkey, best = key, i
            ready.remove(best)
            st = bkey[0]
            issue = dur[best] if items[best][0] == "op" else 60.0
            efree[engs[best]] = st + issue
            fin[best] = st + dlat[best]
            order.append(best)
            for j in succs[best]:
                t = fin[best] + (lat if engs[j] != engs[best] or engs[j] != "tensor" else 0.0)
                if t > rdy_t[j]:
                    rdy_t[j] = t
                npred[j] -= 1
                if npred[j] == 0:
                    ready.append(j)
        assert len(order) == n
        for i in order:
            self.replay(items[i])

    def merge(self, A, B):
        a, b = len(A), len(B)
        if a == 0:
            for it in B:
                self.replay(it)
            return
        done = 0
        for i, it in enumerate(A):
            self.replay(it)
            upto = ((i + 1) * b) // a
            while done < upto:
                self.replay(B[done])
                done += 1
        while done < b:
            self.replay(B[done])
            done += 1

    def region(self, fn):
        self.schedule(self.record(fn))

    def _newsem(self, name):
        self.nsem += 1
        return self.es.enter_context(self.nc.semaphore(f"{name}_{self.nsem}"))

    def sb(self, shape, dt=F32, name=None):
        self.ntensors += 1
        return self.stack[-1].enter_context(self.nc.sbuf_tensor(f"{name or 'sb'}_{self.ntensors}", list(shape), dt))

    def ps(self, shape, dt=F32, name=None):
        self.ntensors += 1
        return self.stack[-1].enter_context(self.nc.psum_tensor(f"{name or 'ps'}_{self.ntensors}", list(shape), dt))

    @contextmanager
    def scope(self):
        st = ExitStack()
        self.stack.append(st)
        try:
            yield
        finally:
            self.barrier()
            self.stack.pop()
            st.close()

    @staticmethod
    def _keys(x):
        if isinstance(x, (str, tuple)):
            return [x]
        t = getattr(x, "tensor", None)
        if t is None:
            return [x.name]
        name = t.name
        if name.startswith("bankw"):
            apl = x.ap
            rs = apl[0][0]
            col0 = x.offset % rs
            ext = 1 + sum((n - 1) * st for st, n in apl[1:])
            half = rs // 2
            ks = []
            if col0 < half:
                ks.append(name + "_lo")
            if col0 + ext > half:
                ks.append(name + "_hi")
            return ks
        return [name]

    def _need(self, eng, ev, waits):
        if ev is None:
            return
        _, sem, val = ev
        k = self.known[eng]
        if k.get(sem, 0) >= val:
            return
        k[sem] = val
        waits[sem] = max(waits.get(sem, 0), val)

    def _deps(self, eng, reads, writes):
        waits = {}
        for r in reads:
            for k in self._keys(r):
                ev = self.lastw.get(k)
                if ev is not None and not (ev[0] == eng and eng == "tensor"):
                    self._need(eng, ev, waits)
                if k.startswith("bank"):
                    for rv in self.reads.get(k, ()):
                        if rv[0] != eng:
                            self._need(eng, rv, waits)
        for w in writes:
            for k in self._keys(w):
                ev = self.lastw.get(k)
                if ev is not None and ev[0] != eng:
                    self._need(eng, ev, waits)
                for rv in self.reads.get(k, ()):
                    if rv[0] != eng:
                        self._need(eng, rv, waits)
        return waits

    def _commit(self, ev, reads, writes):
        for r in reads:
            for k in self._keys(r):
                self.reads.setdefault(k, []).append(ev)
        for w in writes:
            for k in self._keys(w):
                self.lastw[k] = ev
                self.reads[k] = []

    def op(self, eng, fn, reads=(), writes=(), cost=None):
        if self.rec is not None:
            if cost is None:
                cost = self._cost(eng, reads, writes)
            self.rec.append(("op", eng, fn, list(reads), list(writes), cost))
            return None
        waits = self._deps(eng, reads, writes)
        if self.cnt[eng] >= EPOCH:
            self.esem[eng] = self._newsem(f"e_{eng}")
            self.cnt[eng] = 0
        self.cnt[eng] += 1
        sem = self.esem[eng]
        val = self.cnt[eng]
        wl = list(waits.items())

        def run(e, fn=fn, wl=wl, sem=sem):
            for s, v in wl:
                e.wait_ge(s, v)
            fn(e).then_inc(sem, 1)
        self.q[eng].append(run)
        ev = (eng, sem, val)
        self._commit(ev, reads, writes)
        return ev

    def dma(self, eng, out, in_, reads=None, writes=None, **kw):
        reads = [in_] if reads is None else reads
        writes = [out] if writes is None else writes
        if self.rec is not None:
            self.rec.append(("dma", eng, (out, in_), list(reads), list(writes), kw))
            return None
        waits = self._deps(eng, reads, writes)
        i = self.dnext[eng]
        self.dnext[eng] = (i + 1) % NDMA
        sem = self.dsem[eng][i]
        prev = self.dcount[eng][i]
        if prev > 0 and self.known[eng].get(sem, 0) < 16 * prev:
            waits[sem] = 16 * prev
            self.known[eng][sem] = 16 * prev
        self.dcount[eng][i] = prev + 1
        val = 16 * (prev + 1)
        wl = list(waits.items())

        def run(e, wl=wl, sem=sem, out=out, in_=in_, kw=kw):
            for s, v in wl:
                e.wait_ge(s, v)
            e.dma_start(out=out, in_=in_, **kw).then_inc(sem, 16)
        self.q[eng].append(run)
        ev = ("dma_" + eng, sem, val)
        self._commit(ev, reads, writes)
        return ev

    def barrier(self):
        evs = []
        for e in ENGS:
            if self.cnt[e] > 0:
                evs.append((e, self.esem[e], self.cnt[e]))
        for e in self.dsem:
            for i in range(NDMA):
                if self.dcount[e][i] > 0:
                    evs.append(("dma_" + e, self.dsem[e][i], 16 * self.dcount[e][i]))
        for eng in ENGS:
            waits = {}
            for ev in evs:
                if ev[0] != eng:
                    self._need(eng, ev, waits)
            wl = list(waits.items())
            if wl:
                def run(e, wl=wl):
                    for s, v in wl:
                        e.wait_ge(s, v)
                self.q[eng].append(run)

    def emit(self):
        nc = self.nc
        with nc.Block() as block:
            @block.tensor
            def _(e):
                for f in self.q["tensor"]:
                    f(e)

            @block.vector
            def _(e):
                for f in self.q["vector"]:
                    f(e)

            @block.scalar
            def _(e):
                for f in self.q["scalar"]:
                    f(e)

            @block.gpsimd
            def _(e):
                for f in self.q["gpsimd"]:
                    f(e)

            @block.sync
            def _(e):
                for f in self.q["sync"]:
                    f(e)
        self.es.close()

    def mm(self, out, lhsT, rhs, start=True, stop=True):
        return self.op("tensor", lambda e: e.matmul(out, lhsT, rhs, start=start, stop=stop),
                       reads=[lhsT, rhs], writes=[out])

    def tr(self, out, in_, ident):
        return self.op("tensor", lambda e: e.transpose(out, in_, ident), reads=[in_, ident], writes=[out])

    def act(self, out, in_, func, bias=None, scale=None, accum_out=None):
        kw = {}
        reads = [in_]
        writes = [out]
        if bias is not None:
            kw["bias"] = bias
            if not isinstance(bias, (int, float)):
                reads.append(bias)
        if scale is not None:
            kw["scale"] = scale
            if not isinstance(scale, (int, float)):
                reads.append(scale)
        if accum_out is not None:
            kw["accum_out"] = accum_out
            writes.append(accum_out)
        return self.op("scalar", lambda e: e.activation(out, in_, func, **kw), reads=reads, writes=writes)

    def tt(self, out, in0, in1, op, eng="vector"):
        return self.op(eng, lambda e: e.tensor_tensor(out, in0, in1, op), reads=[in0, in1], writes=[out])

    def ts(self, out, in0, s1, s2=None, op0=ALU.mult, op1=None, eng="vector"):
        reads = [in0] + [s for s in (s1, s2) if s is not None and not isinstance(s, (int, float))]
        kw = {}
        if op1 is not None:
            kw["op1"] = op1
        return self.op(eng, lambda e: e.tensor_scalar(out, in0, s1, s2, op0, **kw), reads=reads, writes=[out])

    def stt(self, out, in0, scalar, in1, op0, op1):
        reads = [in0, in1] + ([scalar] if not isinstance(scalar, (int, float)) else [])
        return self.op("vector", lambda e: e.scalar_tensor_tensor(out, in0, scalar, in1, op0, op1),
                       reads=reads, writes=[out])

    def copy(self, out, in_, eng="vector"):
        if eng == "scalar":
            return self.op(eng, lambda e: e.copy(out, in_), reads=[in_], writes=[out])
        return self.op(eng, lambda e: e.tensor_copy(out, in_), reads=[in_], writes=[out])

    def memset(self, ap, val, eng="vector"):
        return self.op(eng, lambda e: e.memset(ap, val), reads=[], writes=[ap])

    def recip(self, out, in_):
        return self.op("vector", lambda e: e.reciprocal(out, in_), reads=[in_], writes=[out])

    def reduce(self, out, in_, op=ALU.add, axis=AX.X):
        return self.op("vector", lambda e: e.tensor_reduce(out, in_, axis, op), reads=[in_], writes=[out])


def build_program(stop_after=99):
    nc = bass.Bass("TRN2", target_bir_lowering=False)

    declared = []

    def din(name, shape, dt=F32, need=0):
        if stop_after < need:
            return None
        declared.append(name)
        return nc.dram_tensor(name, list(shape), dt, kind="ExternalInput").ap()

    x = din("x", [T, DM])
    g1T_d = din("g1T", [128, 16])
    w_lora = din("w_lora", [DM, 448])
    w_rw = din("w_rw", [8, DM, 384])
    w_ret = din("w_ret", [8, DM, 768], need=2)
    w_in = din("w_in", [DM, IN_COLS], need=3)
    lprm_d = din("lprm", [128, 4])
    rwprm_d = din("rwprm", [128, 8, 8])
    rw_w2 = din("rw_w2", [96, 1024])
    rw_a2 = din("rw_a2", [96, 1024])
    rw_g2 = din("rw_g2", [256, 1024])
    lnx_g = din("lnx_g", [1, 1024])
    lnx_b = din("lnx_b", [1, 1024])
    ret_g = din("ret_g", [1, 2048], need=2)
    w_up_a = din("w_up_a", [1024, DM], need=3)
    w_up_b = din("w_up_b", [2048, DM], need=3)
    w_out = din("w_out", [DM, DM], need=4)
    g2_d = din("g2", [1, DM], need=4)
    gf_d = din("gf", [1, DM], need=4)
    w_rt = din("w_rt", [DM, 36], need=4)
    b_rt = din("b_rt", [1, 36], need=4)
    e_gate = din("e_gate", [NEXP, DM, EFF], need=4)
    e_up = din("e_up", [NEXP, DM, EFF], need=4)
    e_down = din("e_down", [NEXP, EFF, DM], need=4)
    c_identb = din("c_identb", [128, 128], BF16)
    c_identf = din("c_identf", [128, 128])
    c_maskS = din("c_maskS", [128, 384])
    c_maskL = din("c_maskL", [128, 128])
    c_bones = din("c_bones", [128, 128], BF16)
    c_hmask = din("c_hmask", [128, 2])
    c_reset = din("c_reset", [128, 1024], BF16)
    c_cs = din("c_cs", [128, 16, 4, 64], need=2)
    c_rmask = din("c_rmask", [128, 8, 128], need=2)
    c_rdec = din("c_rdec", [128, 16], need=2)
    c_tri = din("c_tri", [128, 128], BF16, need=4)
    c_iota = din("c_iota", [128, 128], need=4)
    out = nc.dram_tensor("out", [TO, DM], F32, kind="ExternalOutput").ap()
    oaT_d = nc.dram_tensor("oaT_scr", [8, 128, TO], BF16, kind="Internal").ap()
    obT_d = nc.dram_tensor("obT_scr", [16, 128, TO], BF16, kind="Internal").ap()
    mT_d = nc.dram_tensor("mT_scr", [16, 128, TO], BF16, kind="Internal").ap()
    dbg = None
    if stop_after < 99:
        dbg = nc.dram_tensor("dbg", [24, 128, TO], BF16, kind="ExternalOutput").ap()

    P = Prog(nc)
    P.es.enter_context(nc.allow_low_precision("bf16 PE transposes write bf16 PSUM"))
    pb = [P.ps([128, 512], F32, f"bank{i}") for i in range(6)]
    pbw = P.ps([128, 1024], F32, "bankw")
    pb.append(pbw[:, 0:512])
    pb.append(pbw[:, 512:1024])

    def pbb(i):
        return pb[i][:].bitcast(BF16)

    identb = P.sb([128, 128], BF16, "identb")
    identf = P.sb([128, 128], F32, "identf")
    P.dma("sync", identb[:], c_identb)
    P.dma("sync", identf[:], c_identf)

    with P.scope():
        hT_hi = P.sb([128, 16, 1024], BF16, "hT_hi")
        with P.scope():
            hT_lo = P.sb([128, 16, 1024], BF16, "hT_lo")

            def hTs(c, t0, n):
                if t0 < 1024:
                    return hT_lo[:, c, t0:t0 + n]
                return hT_hi[:, c, t0 - 1024:t0 - 1024 + n]

            g1T = P.sb([128, 16], F32, "g1T")
            P.dma("sync", g1T[:], g1T_d)
            with P.scope():
                xin = [P.sb([128, DM], F32, f"xin{i}") for i in range(4)]
                xnb = [P.sb([128, DM], BF16, f"xnb{i}") for i in range(4)]
                junk = P.sb([128, DM], BF16, "junk")
                st = [P.sb([128, 4], F32, f"st{i}") for i in range(4)]
                def _phase0():
                    for i in range(16):
                        xi = xin[i % 4]
                        xb = xnb[i % 4]
                        s = st[i % 4]
                        P.dma("sync", xi[:], x[i * 128:(i + 1) * 128, :])
                        P.act(junk[:], xi[:], AF.Square, accum_out=s[:, 0:1])
                        P.ts(s[:, 1:2], s[:, 0:1], 1.0 / DM, EPS, op0=ALU.mult, op1=ALU.add)
                        P.act(s[:, 2:3], s[:, 1:2], AF.Sqrt)
                        P.recip(s[:, 3:4], s[:, 2:3])
                        P.ts(xb[:], xi[:], s[:, 3:4], None, op0=ALU.mult)
                        for c4 in range(4):
                            bk = pbb(c4 % 2)
                            for k in range(4):
                                c = c4 * 4 + k
                                P.tr(bk[:, k * 128:(k + 1) * 128], xb[:, c * 128:(c + 1) * 128], identb[:])
                            for k in range(4):
                                c = c4 * 4 + k
                                dst = hTs(c, i * 128, 128)
                                if c4 % 2 == 0:
                                    P.ts(dst, bk[:, k * 128:(k + 1) * 128], g1T[:, c:c + 1], None, op0=ALU.mult)
                                else:
                                    P.act(dst, bk[:, k * 128:(k + 1) * 128], AF.Copy, scale=g1T[:, c:c + 1])

                P.region(_phase0)

            rw_phase(P, nc, locals())
            if stop_after >= 2:
                ret_phase(P, nc, locals())
        if stop_after < 3:
            with P.scope():
                dt_ = P.sb([128, 24, TO], BF16, "dbgt")
                P.dma("sync", dt_[:, 0:8, :], oaT_d.rearrange("c p t -> p c t"))
                if stop_after >= 2:
                    P.dma("sync", dt_[:, 8:24, :], obT_d.rearrange("c p t -> p c t"))
                else:
                    P.memset(dt_[:, 8:24, :], 0.0)
                P.dma("sync", dbg.rearrange("c p t -> p c t"), dt_[:])
        else:
            gate_phase(P, nc, locals())
    if stop_after >= 4:
        tail_phase(P, nc, locals())
    else:
        with P.scope():
            zt = P.sb([128, DM], F32, "zt")
            P.memset(zt[:], 0.0)
            for i in range(8):
                P.dma("sync", out[i * 128:(i + 1) * 128, :], zt[:])
    P.barrier()
    P.emit()
    return nc, declared


def rw_phase(P, nc, L):
    pb = L["pb"]; pbb = L["pbb"]; hTs = L["hTs"]; identb = L["identb"]; pbw = L["pbw"]
    with P.scope():
        lprm = P.sb([128, 8], F32, "lprm")
        P.dma("sync", lprm[:, 0:4], L["lprm_d"])
        P.ts(lprm[:, 4:8], lprm[:, 0:4], -1.0, 1.0, op0=ALU.mult, op1=ALU.add)
        rwprm = P.sb([128, 8, 14], F32, "rwprm")
        onec = P.sb([128, 2], F32, "onec")
        P.memset(onec[:], 1.0)
        P.dma("sync", rwprm[:, :, 0:8], L["rwprm_d"])
        P.ts(rwprm[:, :, 8:11], rwprm[:, :, 0:3], -1.0, 1.0, op0=ALU.mult, op1=ALU.add)
        P.ts(rwprm[:, :, 11:12], rwprm[:, :, 6:7], -1.0, 1.0, op0=ALU.mult, op1=ALU.add)
        P.ts(rwprm[:, :, 12:14], rwprm[:, :, 3:5], -1.0, None, op0=ALU.mult)
        maskL = P.sb([128, 128], F32, "maskL")
        bones = P.sb([128, 128], BF16, "bones")
        hmask = P.sb([128, 2], F32, "hmask")
        reset = P.sb([128, 1024], BF16, "reset")
        P.dma("sync", maskL[:], L["c_maskL"])
        P.dma("sync", bones[:], L["c_bones"])
        P.dma("sync", hmask[:], L["c_hmask"])
        P.dma("sync", reset[:], L["c_reset"])
        w2 = P.sb([96, 1024], BF16, "w2")
        a2 = P.sb([96, 1024], BF16, "a2")
        g2 = P.sb([128, 2, 1024], BF16, "g2")
        P.dma("gpsimd", w2[:], L["rw_w2"])
        P.dma("gpsimd", a2[:], L["rw_a2"])
        P.dma("gpsimd", g2[:], L["rw_g2"].rearrange("(c p) n -> p c n", p=128))
        lgb = P.sb([64, 256], F32, "lgb")

        txw = P.sb([96, T], BF16, "txw")
        xab = P.sb([96, T], BF16, "xab")
        sgx = P.sb([128, 2, TO], BF16, "sgx")
        saved = P.sb([128, 4], F32, "saved")

        with P.scope():
            wl = P.sb([128, 16, 448], BF16, "wl")
            raw = P.sb([128, 1025], F32, "raw")
            tmpF = P.sb([128, 1024], F32, "tmpF")

            def project(wt, c0, M, t0):
                for blk in range(2):
                    ps = pb[blk % 2]
                    for c in range(16):
                        P.mm(ps[0:M, :], wt[:, c, c0:c0 + M], hTs(c, t0 + blk * 512, 512),
                             start=(c == 0), stop=(c == 15))
                    P.act(raw[0:M, 1 + blk * 512:1 + (blk + 1) * 512], ps[0:M, :], AF.Copy)
            P.dma("gpsimd", wl[:], L["w_lora"].rearrange("(c p) n -> p c n", p=128))
            svl = P.sb([128, 4], F32, "svl")
            def _lora():
                for half in range(2):
                    t0 = half * 1024
                    for gi, (c0, M) in enumerate([(0, 96), (96, 96), (192, 128), (320, 128)]):
                        if half == 0:
                            P.memset(raw[0:M, 0:1], 0.0)
                        else:
                            P.copy(raw[0:M, 0:1], svl[0:M, gi:gi + 1])
                        project(wl, c0, M, t0)
                        tmp = tmpF[0:M, :]
                        P.ts(tmp, raw[0:M, 1:1025], lprm[0:M, 4 + gi:5 + gi], None, op0=ALU.mult)
                        P.stt(tmp, raw[0:M, 0:1024], lprm[0:M, gi:gi + 1], tmp, ALU.mult, ALU.add)
                        P.copy(svl[0:M, gi:gi + 1], raw[0:M, 1024:1025])
                        if gi == 0:
                            P.act(txw[:, t0:t0 + 1024], tmp, AF.Tanh)
                        elif gi == 1:
                            P.act(xab[:, t0:t0 + 1024], tmp, AF.Copy)
                        elif half == 1:
                            P.act(sgx[:, gi - 2, :], tmp, AF.Sigmoid)

            P.region(_lora)

        UL = 512
        NCU = 8
        ARx = [P.sb([128, NCU, 192], BF16, f"ARx{i}") for i in range(2)]
        BKx = [P.sb([128, NCU, 2, 2, 64], BF16, f"BKx{i}") for i in range(2)]
        BHx = [P.sb([128, NCU, 2, 64], BF16, f"BHx{i}") for i in range(2)]
        KHx = [P.sb([128, NCU, 2, 64], BF16, f"KHx{i}") for i in range(2)]
        Vx = [P.sb([128, NCU, 2, 64], BF16, f"Vx{i}") for i in range(2)]
        Vc = [P.sb([128, UL], BF16, f"Vc{i}") for i in range(2)]
        RKb = [P.sb([128, UL], BF16, f"RKb{i}") for i in range(2)]
        Pall = [P.sb([128, NCU, 128], BF16, f"Pall{i}") for i in range(2)]
        WC = [P.sb([128, NCU], F32, f"WC{i}") for i in range(2)]
        for lst in (ARx, BKx, BHx, KHx, Vx):
            for tl in lst:
                P.memset(tl[:], 0.0, eng="gpsimd")
        Hst = P.sb([128, 128], F32, "Hst")
        Hbf = P.sb([128, 128], BF16, "Hbf")
        wts = [P.sb([128, 16, 128], BF16, f"wts{i}") for i in range(3)]
        rksel = P.sb([128, 2], BF16, "rksel")
        maskS = P.sb([128, 384], F32, "maskS")
        P.dma("sync", maskS[:], L["c_maskS"])
        SCm = [P.sb([128, 384], BF16, f"SCm{i}") for i in range(2)]
        TM = [P.sb([128, 384], BF16, f"TM{i}") for i in range(2)]
        TMc = [P.sb([64, 2, 128], BF16, f"TMc{i}") for i in range(2)]
        Zs = [P.sb([128, 128], BF16, f"Zs{i}") for i in range(2)]
        Us = [P.sb([128, 128], BF16, f"Us{i}") for i in range(2)]
        XXg = [[P.sb([128, 1024], BF16, f"XX{g_}_{i}") for i in range(2)] for g_ in range(2)]
        Pmg = [P.sb([128, 512], BF16, f"Pm{g_}") for g_ in range(2)]
        ep = [P.sb([64, 32], F32, f"ep{i}") for i in range(2)]
        ey = [P.sb([64, 256], F32, f"ey{i}") for i in range(2)]
        ey2 = [P.sb([64, 256], F32, f"eyb{i}") for i in range(2)]
        eg = [P.sb([64, 256], F32, f"eg{i}") for i in range(2)]
        eo = [P.sb([64, 256], BF16, f"eo{i}") for i in range(2)]
        oaT_st = P.sb([128, 1024], BF16, "oaT_st")
        mS = maskS[:, 0:128]
        Fq = [P.sb([128, UL], F32, f"Fq{i}") for i in range(6)]
        Bq = [P.sb([128, UL], BF16, f"Bq{i}") for i in range(4)]
        rawq = P.sb([128, UL + 1], F32, "rawq")

        def v3(t_, hs):
            return t_[hs, :].rearrange("p (c t) -> p c t", t=64)

        def wload(p, xi):
            P.dma("gpsimd", wts[xi][:], L["w_rw"][p].rearrange("(c p) n -> p c n", p=128)[:, :, xi * 128:(xi + 1) * 128])

        def prep(p, q, ub):
            t0 = q * UL
            need_y = q >= 2
            prm = rwprm[:, p, :]
            cs = slice(p * 128, (p + 1) * 128)
            Rf, Kf, T1, SGb, Epos, CUMb = Fq
            ALb, KKb, RNb, KK2b = Bq
            arx, bkx, bhx, khx, vx, vc, rkb, pall, wc = ARx[ub], BKx[ub], BHx[ub], KHx[ub], Vx[ub], Vc[ub], RKb[ub], Pall[ub], WC[ub]
            for xi, dst in enumerate((Rf, Kf, T1)):
                if xi == 0 and not need_y:
                    if q == 1:
                        for c in range(16):
                            P.mm(pb[0][:, 0:2], wts[0][:, c, :], hTs(c, t0 + UL - 2, 2), start=(c == 0), stop=(c == 15))
                        P.act(saved[:, 0:1], pb[0][:, 1:2], AF.Copy)
                    continue
                if q == 0:
                    P.memset(rawq[:, 0:1], 0.0, eng="gpsimd")
                else:
                    P.copy(rawq[:, 0:1], saved[:, xi:xi + 1])
                ps = pb[xi % 2]
                for c in range(16):
                    P.mm(ps[:], wts[xi][:, c, :], hTs(c, t0, UL), start=(c == 0), stop=(c == 15))
                if q == 3 and p + 1 < 8:
                    wload(p + 1, xi)
                P.act(rawq[:, 1:UL + 1], ps[:], AF.Copy)
                P.act(dst[:], ps[:], AF.Copy, scale=prm[:, 8 + xi:9 + xi])
                P.stt(dst[:], rawq[:, 0:UL], prm[:, xi:xi + 1], dst[:], ALU.mult, ALU.add)
                P.copy(saved[:, xi:xi + 1], rawq[:, UL:UL + 1])
            for h in range(2):
                hs = slice(h * 64, (h + 1) * 64)
                P.act(vx[hs, :, h, :], v3(T1, hs), AF.Copy)
            if need_y:
                P.act(vc[:], T1[:], AF.Copy)
            P.mm(pb[0][:], w2[:, cs], txw[:, t0:t0 + UL])
            P.act(SGb[:], pb[0][:], AF.Exp, scale=-1.0, bias=prm[:, 12:13])
            P.act(SGb[:], SGb[:], AF.Ln, bias=onec[:, 0:1])
            P.act(SGb[:], SGb[:], AF.Exp, scale=-1.0)
            P.mm(pb[1][:], a2[:, cs], xab[:, t0:t0 + UL])
            P.act(ALb[:], pb[1][:], AF.Exp, scale=-1.0, bias=prm[:, 13:14])
            P.act(ALb[:], ALb[:], AF.Ln, bias=onec[:, 0:1])
            P.act(ALb[:], ALb[:], AF.Exp, scale=-1.0)
            P.op("vector", lambda e, o=CUMb, r_=reset, s_=SGb: e.tensor_tensor_scan(o[:], r_[:, 0:UL], s_[:], 0.0, ALU.mult, ALU.add),
                 reads=[reset, SGb], writes=[CUMb])
            P.tt(T1[:], CUMb[:], SGb[:], ALU.subtract)
            P.act(T1[:], T1[:], AF.Exp, scale=CDEC)
            cv = CUMb[:].rearrange("p (c t) -> p c t", t=64)
            P.act(Epos[:], CUMb[:], AF.Exp, scale=CDEC)
            P.act(CUMb[:], CUMb[:], AF.Exp, scale=-CDEC)
            P.tt(SGb[:].rearrange("p (c t) -> p c t", t=64), cv,
                 Epos[:].rearrange("p (c t) -> p c t", t=64)[:, :, 63:64].to_broadcast([128, NCU, 64]), ALU.mult)
            Eprev, Eend, Eneg = T1, SGb, CUMb
            P.copy(wc[:], Epos[:].rearrange("p (c t) -> p c t", t=64)[:, :, 63])
            P.act(KKb[:], Kf[:], AF.Copy, scale=prm[:, 5:6])
            P.act(KK2b[:], Kf[:], AF.Square, scale=prm[:, 5:6])
            P.mm(pb[0][:], bones[:], KK2b[:])
            P.ts(RNb[:], pb[0][:], 1e-18, None, op0=ALU.max)
            P.act(RNb[:], RNb[:], AF.Ln)
            P.act(RNb[:], RNb[:], AF.Exp, scale=-0.5)
            P.tt(KKb[:], KKb[:], RNb[:], ALU.mult)
            for h in range(2):
                hs = slice(h * 64, (h + 1) * 64)
                P.tt(arx[hs, :, h * 64:(h + 1) * 64], v3(KKb, hs), v3(Eprev, hs), ALU.mult)
            P.tt(KKb[:], KKb[:], ALb[:], ALU.mult)
            P.ts(RNb[:], ALb[:], prm[:, 6:7], prm[:, 11:12], op0=ALU.mult, op1=ALU.add)
            P.tt(Kf[:], Kf[:], RNb[:], ALU.mult)
            for h in range(2):
                hs = slice(h * 64, (h + 1) * 64)
                e1 = "vector"
                e2 = "vector"
                P.tt(bkx[hs, :, 0, h, :], v3(KKb, hs), v3(Eneg, hs), ALU.mult, eng=e1)
                P.tt(bkx[hs, :, 1, h, :], v3(Kf, hs), v3(Eneg, hs), ALU.mult, eng=e2)
                P.tt(bhx[hs, :, h, :], v3(KKb, hs), v3(Eend, hs), ALU.mult, eng=e1)
                P.tt(khx[hs, :, h, :], v3(Kf, hs), v3(Eend, hs), ALU.mult, eng=e2)
            if need_y:
                P.tt(arx[:, :, 128:192], Rf[:].rearrange("p (c t) -> p c t", t=64),
                     Epos[:].rearrange("p (c t) -> p c t", t=64), ALU.mult)
                P.tt(rkb[:], Rf[:], Kf[:], ALU.mult)
            r4 = lambda t_: t_.rearrange("p (k n) -> p k n", n=128)
            for g in range(NCU // 4):
                XX = XXg[g % 2]
                Pm = Pmg[g % 2]
                xx = XX[0]
                for k in range(4):
                    c = g * 4 + k
                    bcols = bkx[:, c, 0, :, :].rearrange("p a b -> p (a b)")
                    acols = arx[:, c, 0:128]
                    P.mm(pbw[:, k * 128:(k + 1) * 128], bcols, acols)
                    P.mm(pbw[:, 512 + k * 128:512 + (k + 1) * 128], acols, bcols)
                P.tt(r4(xx[:, 0:512]), r4(pbw[:, 0:512]), mS.unsqueeze(1).to_broadcast([128, 4, 128]), ALU.mult)
                P.tt(r4(xx[:, 512:1024]), r4(pbw[:, 512:1024]), maskL[:].unsqueeze(1).to_broadcast([128, 4, 128]), ALU.mult)
                P.tt(r4(Pm[:]), L["identf"][:].unsqueeze(1).to_broadcast([128, 4, 128]), r4(xx[:, 0:512]), ALU.subtract)
                for lvl in range(6):
                    if lvl < 5:
                        for k in range(4):
                            ks = slice(k * 128, (k + 1) * 128)
                            kt = slice(512 + k * 128, 512 + (k + 1) * 128)
                            P.mm(pbw[:, ks], xx[:, kt], xx[:, ks])
                            P.mm(pbw[:, kt], xx[:, ks], xx[:, kt])
                    if lvl >= 1:
                        for k in range(4):
                            ks = slice(k * 128, (k + 1) * 128)
                            kt = slice(512 + k * 128, 512 + (k + 1) * 128)
                            P.mm(pb[1][:, ks], xx[:, kt], Pm[:, ks])
                    if lvl < 5:
                        xn = XX[(lvl + 1) % 2]
                        P.act(xn[:], pbw[:], AF.Copy)
                    if lvl >= 1:
                        if lvl == 5:
                            P.tt(pall[:, g * 4:(g + 1) * 4, :], r4(Pm[:]), r4(pb[1][:]), ALU.add)
                        else:
                            P.tt(Pm[:], Pm[:], pb[1][:], ALU.add)
                    if lvl < 5:
                        xx = xn

        def rec(p, q, ub):
            t0 = q * UL
            need_y = q >= 2
            prm = rwprm[:, p, :]
            cs = slice(p * 128, (p + 1) * 128)
            arx, bkx, bhx, khx, vx, vc, rkb, pall, wc = ARx[ub], BKx[ub], BHx[ub], KHx[ub], Vx[ub], Vc[ub], RKb[ub], Pall[ub], WC[ub]
            if q == 0:
                P.memset(Hst[:], 0.0)
                P.memset(Hbf[:], 0.0)
            if q == 2:
                P.ts(rksel[:], hmask[:], prm[:, 7:8], None, op0=ALU.mult)
                P.dma("sync", lgb[:, 0:128], L["lnx_g"][:, cs].partition_broadcast(64))
                P.dma("sync", lgb[:, 128:256], L["lnx_b"][:, cs].partition_broadcast(64))
            for c in range(NCU):
                par = c % 2
                sc = SCm[par]; tm = TM[par]; zs = Zs[par]; us = Us[par]
                ppar = (c // 2) % 2
                tmc = TMc[ppar]
                bcols = bkx[:, c, 0, :, :].rearrange("p a b -> p (a b)")
                kcols = bkx[:, c, 1, :, :].rearrange("p a b -> p (a b)")
                acols = arx[:, c, 0:128]
                rcols = arx[:, c, 128:192]
                nar = 192 if need_y else 128
                P.mm(pb[3][:, 0:nar], bcols, arx[:, c, 0:nar])
                P.mm(pb[3][:, 192:192 + nar], kcols, arx[:, c, 0:nar])
                s3 = lambda t_: t_[:, 0:384].rearrange("p (a n) -> p a n", n=192)[:, :, 0:nar]
                P.tt(s3(sc), s3(pb[3]), s3(maskS), ALU.mult)
                nabT, nrbT, nakT, nrkT = sc[:, 0:128], sc[:, 128:192], sc[:, 192:320], sc[:, 320:384]
                bT = pbb(2)
                P.tr(bT[:, 0:128], bhx[:, c, :, :].rearrange("p a b -> p (a b)"), identb[:])
                P.tr(bT[:, 128:256], khx[:, c, :, :].rearrange("p a b -> p (a b)"), identb[:])
                P.tr(bT[:, 256:384], vx[:, c, :, :].rearrange("p a b -> p (a b)"), identb[:])
                P.act(tm[:, 0:384], bT[:, 0:384], AF.Copy)
                if need_y:
                    P.tr(bT[0:64, 384:512], vc[:, c * 64:(c + 1) * 64], identb[:])
                    P.act(tmc[:, c % 2, :], bT[0:64, 384:512], AF.Copy)
                bht, kht, vt = tm[:, 0:128], tm[:, 128:256], tm[:, 256:384]
                P.mm(pb[4][:, 0:128], acols, Hbf[:], start=True, stop=False)
                P.mm(pb[4][:, 0:128], nakT, vt, start=False, stop=True)
                P.copy(zs[:], pb[4][:, 0:128])
                P.mm(pb[4][:, 128:256], pall[:, c, :], zs[:])
                P.act(us[:], pb[4][:, 128:256], AF.Copy, scale=-1.0)
                if need_y:
                    yc = slice((c % 2) * 128, (c % 2 + 1) * 128)
                    P.mm(pb[5][0:64, yc], rcols, Hbf[:], start=True, stop=False)
                    P.mm(pb[5][0:64, yc], nrbT, us[:], start=False, stop=False)
                    P.mm(pb[5][0:64, yc], nrkT, vt, start=False, stop=True)
                P.mm(pb[4][:, 256:384], bht, us[:], start=True, stop=False)
                P.mm(pb[4][:, 256:384], kht, vt, start=False, stop=True)
                P.stt(Hbf[:], Hst[:], wc[:, c:c + 1], pb[4][:, 256:384], ALU.mult, ALU.add)
                P.stt(Hst[:], Hst[:], wc[:, c:c + 1], pb[4][:, 256:384], ALU.mult, ALU.add)
                if need_y and c % 2 == 1:
                    e = ep[ppar]; y = ey[ppar]; y2 = ey2[ppar]; o = eo[ppar]; gg = eg[ppar]
                    to0 = (q - 2) * UL + (c - 1) * 64
                    tu0 = (c - 1) * 64
                    P.copy(y[:], pb[5][0:64, 0:256])
                    for j in range(2):
                        for kc in range(2):
                            P.mm(pb[5][0:64, 256 + j * 128:256 + (j + 1) * 128], sgx[:, kc, to0 + j * 64:to0 + (j + 1) * 64],
                                 g2[:, kc, cs], start=(kc == 0), stop=(kc == 1))
                    P.act(gg[:], pb[5][0:64, 256:512], AF.Copy)
                    for j in range(2):
                        P.mm(pb[3][0:64, 384 + 2 * j:386 + 2 * j], rkb[:, tu0 + j * 64:tu0 + (j + 1) * 64], rksel[:])
                    P.act(e[:, 24:28], pb[3][0:64, 384:388], AF.Copy)
                    y4 = y[:].rearrange("p (a v) -> p a v", v=64)
                    P.reduce(e[:, 0:4], y4)
                    P.tt(y2[:], y[:], y[:], ALU.mult)
                    P.reduce(e[:, 4:8], y2[:].rearrange("p (a v) -> p a v", v=64))
                    P.ts(e[:, 8:12], e[:, 0:4], 1.0 / 64, None, op0=ALU.mult)
                    P.tt(e[:, 12:16], e[:, 8:12], e[:, 8:12], ALU.mult)
                    P.ts(e[:, 16:20], e[:, 4:8], 1.0 / 64, RW_LN_EPS, op0=ALU.mult, op1=ALU.add)
                    P.tt(e[:, 16:20], e[:, 16:20], e[:, 12:16], ALU.subtract)
                    P.act(e[:, 16:20], e[:, 16:20], AF.Ln)
                    P.act(e[:, 20:24], e[:, 16:20], AF.Exp, scale=-0.5)
                    P.tt(y4, y4, e[:, 8:12].unsqueeze(2).to_broadcast([64, 4, 64]), ALU.subtract)
                    P.tt(y4, y4, e[:, 20:24].unsqueeze(2).to_broadcast([64, 4, 64]), ALU.mult)
                    y3 = y[:].rearrange("p (j n) -> p j n", n=128)
                    P.tt(y3, y3, lgb[:, 0:128].unsqueeze(1).to_broadcast([64, 2, 128]), ALU.mult)
                    P.tt(y3, y3, lgb[:, 128:256].unsqueeze(1).to_broadcast([64, 2, 128]), ALU.add)
                    P.tt(y2[:].rearrange("p (a v) -> p a v", v=64), tmc[:].rearrange("p j (h v) -> p (j h) v", v=64),
                         e[:, 24:28].unsqueeze(2).to_broadcast([64, 4, 64]), ALU.mult)
                    P.tt(y[:], y[:], y2[:], ALU.add)
                    P.tt(o[:], y[:], gg[:], ALU.mult)
                    for j in range(2):
                        P.tr(bT[:, 512 + j * 64:512 + (j + 1) * 64], o[:, j * 128:(j + 1) * 128], identb[0:64, 0:64])
                    P.act(oaT_st[:, to0:to0 + 128], bT[:, 512:640], AF.Copy)
            if q == 3:
                P.dma("sync", L["oaT_d"][p], oaT_st[:])

        units = [(p, q) for p in range(8) for q in range(4)]
        for xi in range(3):
            wload(0, xi)
        prep(0, 0, 0)
        WIN = 8
        for u0 in range(0, len(units), WIN):
            items = []
            for u in range(u0, min(u0 + WIN, len(units))):
                p, q = units[u]
                items += P.record(lambda p=p, q=q, u=u: rec(p, q, u % 2))
                if u + 1 < len(units):
                    pn, qn = units[u + 1]
                    items += P.record(lambda pn=pn, qn=qn, u=u: prep(pn, qn, (u + 1) % 2))
            P.schedule(items)


def ret_phase(P, nc, L):
    pb = L["pb"]; pbb = L["pbb"]; hTs = L["hTs"]; identb = L["identb"]
    with P.scope():
        cst = P.sb([128, 16, 4, 64], F32, "cst")
        rmask = P.sb([128, 8, 128], F32, "rmask")
        rdec = P.sb([128, 16], F32, "rdec")
        P.dma("sync", cst[:], L["c_cs"])
        P.dma("sync", rmask[:], L["c_rmask"])
        P.dma("sync", rdec[:], L["c_rdec"])
        retg = P.sb([128, 256], F32, "retg")
        wr = [P.sb([128, 16, 768], BF16, f"wr{i}") for i in range(2)]
        S = P.sb([128, 256], F32, "S")
        Sbf = P.sb([128, 256], BF16, "Sbf")
        obT_st = P.sb([128, 2, 1024], BF16, "obT_st")
        A_ = [P.sb([128, 2, 128], F32, f"rA{i}") for i in range(2)]
        B_ = [P.sb([128, 2, 128], F32, f"rB{i}") for i in range(2)]
        rot = [P.sb([128, 2, 128], F32, f"rot{i}") for i in range(2)]
        qkb = [P.sb([128, 3, 128], BF16, f"qkb{i}") for i in range(2)]
        qkT = [P.sb([128, 2, 128], BF16, f"qkT{i}") for i in range(2)]
        vb = [P.sb([128, 256], BF16, f"vb{i}") for i in range(2)]
        scm = [P.sb([128, 128], BF16, f"scm{i}") for i in range(2)]
        sg = [P.sb([128, 256], F32, f"sg{i}") for i in range(2)]
        ob = [P.sb([128, 256], BF16, f"ob{i}") for i in range(2)]
        rs = [P.sb([128, 4], F32, f"rs{i}") for i in range(2)]
        junk = P.sb([128, 256], BF16, "rjunk")
        onec2 = P.sb([128, 2], F32, "onec2")
        P.memset(onec2[:], 1.0)
        lgam = [math.log1p(-2.0 ** (-5.0 - h)) for h in range(8)]
        P.dma("gpsimd", wr[0][:], L["w_ret"][0].rearrange("(c p) n -> p c n", p=128))
        def _head(h):
            wt = wr[h % 2]
            if h + 1 < 8:
                P.dma("gpsimd", wr[(h + 1) % 2][:], L["w_ret"][h + 1].rearrange("(c p) n -> p c n", p=128))
            P.dma("sync", retg[:], L["ret_g"][:, h * 256:(h + 1) * 256].partition_broadcast(128))
            P.memset(S[:], 0.0)
            P.memset(Sbf[:], 0.0)
            g128 = math.exp(128.0 * lgam[h])
            def proj(i):
                par = i % 2
                need_o = i >= 8
                t0 = i * 128
                pa = pb[par]
                lo = 0 if need_o else 128
                for c in range(16):
                    P.mm(pa[:, lo:512], hTs(c, t0, 128), wt[:, c, lo:512], start=(c == 0), stop=(c == 15))
                if need_o:
                    pg = pb[6] if par == 0 else pb[7]
                    for c in range(16):
                        P.mm(pg[:, 0:256], hTs(c, t0, 128), wt[:, c, 512:768], start=(c == 0), stop=(c == 15))

            proj(0)
            for i in range(16):
                par = i % 2
                need_o = i >= 8
                t0 = i * 128
                pa = pb[par]
                pg = pb[6] if par == 0 else pb[7]
                A = A_[par]; Bm = B_[par]; ro = rot[par]; qk = qkb[par]; qT = qkT[par]
                for xi in ([0, 1] if need_o else [1]):
                    xs = pa[:, xi * 128:(xi + 1) * 128].rearrange("p (a d) -> p a d", d=64)
                    ci = 0 if xi == 0 else 2
                    cosb = cst[:, i, ci, :].unsqueeze(1).to_broadcast([128, 2, 64])
                    sinb = cst[:, i, ci + 1, :].unsqueeze(1).to_broadcast([128, 2, 64])
                    Av = A[:, xi, :].rearrange("p (a d) -> p a d", d=64)
                    Bv = Bm[:, xi, :].rearrange("p (a d) -> p a d", d=64)
                    P.tt(Av, xs, cosb, ALU.mult)
                    P.tt(Bv, xs, sinb, ALU.mult)
                    P.tt(ro[:, xi, 0:64], A[:, xi, 0:64], Bm[:, xi, 64:128], ALU.subtract)
                    P.tt(ro[:, xi, 64:128], Bm[:, xi, 0:64], A[:, xi, 64:128], ALU.add)
                if need_o:
                    P.act(qk[:, 0, :], ro[:, 0, :], AF.Copy, scale=rdec[:, h:h + 1])
                    P.act(qk[:, 1, :], ro[:, 1, :], AF.Copy)
                P.act(qk[:, 2, :], ro[:, 1, :], AF.Copy, scale=rdec[:, 8 + h:9 + h])
                P.act(vb[par][:], pa[:, 256:512], AF.Copy)
                if i + 1 < 16:
                    proj(i + 1)
                if need_o:
                    bT = pbb(5)
                    P.tr(bT[:, 0:128], qk[:, 0, :], identb[:])
                    P.tr(bT[:, 128:256], qk[:, 1, :], identb[:])
                    P.copy(qT[:].rearrange("p a d -> p (a d)"), bT[:, 0:256])
                    P.mm(pb[2][:, 0:128], qT[:, 1, :], qT[:, 0, :])
                    P.tt(scm[par][:], pb[2][:, 0:128], rmask[:, h, :], ALU.mult)
                    P.mm(pb[3][:, 0:256], scm[par][:], vb[par][:], start=True, stop=False)
                    P.mm(pb[3][:, 0:256], qT[:, 0, :], Sbf[:], start=False, stop=True)
                P.mm(pb[4][:, 0:256], qk[:, 2, :], vb[par][:])
                P.stt(S[:], S[:], g128, pb[4][:, 0:256], ALU.mult, ALU.add)
                P.act(Sbf[:], S[:], AF.Copy)
                if need_o:
                    r = rs[par]
                    P.act(junk[:], pb[3][:, 0:256], AF.Square, accum_out=r[:, 0:1])
                    P.ts(r[:, 1:2], r[:, 0:1], 1.0 / 256, EPS, op0=ALU.mult, op1=ALU.add)
                    P.act(r[:, 2:3], r[:, 1:2], AF.Ln)
                    P.act(r[:, 3:4], r[:, 2:3], AF.Exp, scale=-0.5)
                    P.act(sg[par][:], pg[:, 0:256], AF.Exp, scale=-1.0)
                    P.act(sg[par][:], sg[par][:], AF.Ln, bias=onec2[:, 0:1])
                    P.act(sg[par][:], sg[par][:], AF.Exp, scale=-1.0)
                    P.tt(sg[par][:], sg[par][:], pg[:, 0:256], ALU.mult)
                    P.tt(sg[par][:], sg[par][:], retg[:], ALU.mult)
                    P.stt(ob[par][:], pb[3][:, 0:256], r[:, 3:4], sg[par][:], ALU.mult, ALU.mult)
                    bT2 = pbb(5)
                    for ec in range(2):
                        P.tr(bT2[:, 256 + ec * 128:256 + (ec + 1) * 128], ob[par][:, ec * 128:(ec + 1) * 128], identb[:])
                    P.act(obT_st[:, :, (i - 8) * 128:(i - 7) * 128], bT2[:, 256:512].rearrange("p (a d) -> p a d", d=128), AF.Copy)
            for ec in range(2):
                P.dma("sync", L["obT_d"][h * 2 + ec], obT_st[:, ec, :])


        def _allheads():
            for h in range(8):
                _head(h)
        P.region(_allheads)


def gate_phase(P, nc, L):
    pb = L["pb"]; hT_hi = L["hT_hi"]; w_in = L["w_in"]
    with P.scope():
        oaT = P.sb([128, 8, TO], BF16, "oaT")
        obT = P.sb([128, 16, TO], BF16, "obT")
        P.dma("sync", oaT[:], L["oaT_d"].rearrange("c p t -> p c t"))
        P.dma("sync", obT[:], L["obT_d"].rearrange("c p t -> p c t"))
        GA_ = [P.sb([128, 16, 512], BF16, f"GA{i}") for i in range(2)]
        GB_ = [P.sb([128, 16, 512], BF16, f"GB{i}") for i in range(2)]
        UA_ = [P.sb([128, 8, 512], BF16, f"UA{i}") for i in range(2)]
        UB_ = [P.sb([128, 16, 512], BF16, f"UB{i}") for i in range(2)]
        sA = [P.sb([128, 512], F32, f"sA{i}") for i in range(2)]
        sB = [P.sb([128, 512], F32, f"sB{i}") for i in range(2)]
        mT = [P.sb([128, TO], BF16, f"mT{i}") for i in range(2)]
        wv = w_in.rearrange("(c p) n -> p c n", p=128)
        def _quad(q):
            GA, GB, UA, UB = GA_[q % 2], GB_[q % 2], UA_[q % 2], UB_[q % 2]
            P.dma("gpsimd", GA[:], wv[:, :, GATE_OFF + q * 512:GATE_OFF + (q + 1) * 512])
            P.dma("gpsimd", GB[:], wv[:, :, GATE_OFF + 2048 + q * 512:GATE_OFF + 2048 + (q + 1) * 512])
            P.dma("gpsimd", UA[:], L["w_up_a"].rearrange("(c p) n -> p c n", p=128)[:, :, q * 512:(q + 1) * 512])
            P.dma("gpsimd", UB[:], L["w_up_b"].rearrange("(c p) n -> p c n", p=128)[:, :, q * 512:(q + 1) * 512])
            for jj in range(4):
                j = q * 4 + jj
                cols = slice(jj * 128, (jj + 1) * 128)
                mt = mT[j % 2]
                for blk in range(2):
                    par = blk
                    bs = slice(blk * 512, (blk + 1) * 512)
                    b0, b1, b2, b3 = pb[par * 4], pb[par * 4 + 1], pb[par * 4 + 2], pb[par * 4 + 3]
                    for c in range(16):
                        P.mm(b0[:], GA[:, c, cols], hT_hi[:, c, bs], start=(c == 0), stop=(c == 15))
                    for c in range(16):
                        P.mm(b1[:], GB[:, c, cols], hT_hi[:, c, bs], start=(c == 0), stop=(c == 15))
                    for c in range(8):
                        P.mm(b2[:], UA[:, c, cols], oaT[:, c, bs], start=(c == 0), stop=(c == 7))
                    for c in range(16):
                        P.mm(b3[:], UB[:, c, cols], obT[:, c, bs], start=(c == 0), stop=(c == 15))
                    P.act(sA[par][:], b0[:], AF.Sigmoid)
                    P.act(sB[par][:], b1[:], AF.Sigmoid)
                    P.tt(sA[par][:], sA[par][:], b2[:], ALU.mult)
                    P.tt(sB[par][:], sB[par][:], b3[:], ALU.mult)
                    P.tt(mt[:, bs], sA[par][:], sB[par][:], ALU.add)
                P.dma("sync", L["mT_d"][j], mt[:])


        def _allq():
            for q in range(4):
                _quad(q)
        P.region(_allq)


def tail_phase(P, nc, L):
    pb = L["pb"]; pbb = L["pbb"]; identb = L["identb"]; identf = L["identf"]
    x = L["x"]; out = L["out"]
    with P.scope():
        x2 = P.sb([128, 8, DM], F32, "x2")
        with P.scope():
            mg = P.sb([128, 16, TO], BF16, "mg")
            P.dma("sync", mg[:], L["mT_d"].rearrange("c p t -> p c t"))
            Wo = [P.sb([128, 16, 512], BF16, f"Wo{i}") for i in range(2)]
            xin = [P.sb([128, 512], F32, f"xin3_{i}") for i in range(2)]
            wov = L["w_out"].rearrange("(c p) n -> p c n", p=128)
            P.dma("gpsimd", Wo[0][:], wov[:, :, 0:512])
            def _wout():
                k = 0
                for nb in range(4):
                    if nb + 1 < 4:
                        P.dma("gpsimd", Wo[(nb + 1) % 2][:], wov[:, :, (nb + 1) * 512:(nb + 2) * 512])
                    for i in range(8):
                        bk = pb[k % 4]
                        xi = xin[k % 2]
                        k += 1
                        P.dma("sync", xi[:], x[1024 + i * 128:1024 + (i + 1) * 128, nb * 512:(nb + 1) * 512])
                        for c in range(16):
                            P.mm(bk[:], mg[:, c, i * 128:(i + 1) * 128], Wo[nb % 2][:, c, :], start=(c == 0), stop=(c == 15))
                        P.tt(x2[:, i, nb * 512:(nb + 1) * 512], bk[:], xi[:], ALU.add)

            P.region(_wout)

        with P.scope():
            h2 = P.sb([128, 8, DM], BF16, "h2")
            asg = P.sb([128, 8, 32], F32, "asg")
            asgb = P.sb([128, 8, 32], BF16, "asgb")
            wmat = P.sb([128, 8, 32], F32, "wmat")
            pos = P.sb([128, 8, 32], F32, "pos")
            iota = P.sb([128, 128], F32, "iota")
            tri = P.sb([128, 128], BF16, "tri")
            onesb = P.sb([128, 128], BF16, "onesb")
            P.dma("sync", iota[:], L["c_iota"])
            P.dma("sync", tri[:], L["c_tri"])
            P.memset(onesb[:], 1.0)
            Wg = [P.sb([128, 16, EFF], BF16, "Wg0")]
            Wu = [P.sb([128, 16, EFF], BF16, "Wu0")]
            P.dma("gpsimd", Wg[0][:], L["e_gate"][0].rearrange("(c p) f -> p c f", p=128))
            P.dma("gpsimd", Wu[0][:], L["e_up"][0].rearrange("(c p) f -> p c f", p=128))
            with P.scope():
                g2b = P.sb([128, DM], F32, "g2b")
                P.dma("sync", g2b[:], L["g2_d"].partition_broadcast(128))
                wrt = P.sb([128, 16, 36], F32, "wrt")
                P.dma("sync", wrt[:], L["w_rt"].rearrange("(c p) n -> p c n", p=128))
                brt = P.sb([128, 36], F32, "brt")
                P.dma("sync", brt[:], L["b_rt"].partition_broadcast(128))
                h2f = [P.sb([128, DM], F32, f"h2f{i}") for i in range(2)]
                h2T = [P.sb([128, 16, 128], F32, f"h2T{i}") for i in range(2)]
                junk = P.sb([128, DM], BF16, "junk4")
                rr = [P.sb([128, 24], F32, f"rr{i}") for i in range(2)]
                lgt = [P.sb([128, 36], F32, f"lgt{i}") for i in range(2)]
                em = [P.sb([128, 3, 32], F32, f"em{i}") for i in range(2)]
                def _router():
                    for i in range(8):
                        par = i % 2
                        r = rr[par]; hf = h2f[par]; hT_ = h2T[par]; lg = lgt[par]; e_ = em[par]
                        P.act(junk[:], x2[:, i, :], AF.Square, accum_out=r[:, 0:1])
                        P.ts(r[:, 1:2], r[:, 0:1], 1.0 / DM, EPS, op0=ALU.mult, op1=ALU.add)
                        P.act(r[:, 2:3], r[:, 1:2], AF.Ln)
                        P.act(r[:, 3:4], r[:, 2:3], AF.Exp, scale=-0.5)
                        P.stt(hf[:], x2[:, i, :], r[:, 3:4], g2b[:], ALU.mult, ALU.mult)
                        P.act(h2[:, i, :], hf[:], AF.Copy)
                        for c4 in range(4):
                            bk = pb[c4 % 2]
                            for kk in range(4):
                                c = c4 * 4 + kk
                                P.tr(bk[:, kk * 128:(kk + 1) * 128], hf[:, c * 128:(c + 1) * 128], identf[:])
                            P.act(hT_[:, c4 * 4:(c4 + 1) * 4, :], bk[:].rearrange("p (a d) -> p a d", d=128), AF.Copy)
                        for c in range(16):
                            P.mm(pb[2][:, 0:36], hT_[:, c, :], wrt[:, c, :], start=(c == 0), stop=(c == 15))
                        P.tt(lg[:], pb[2][:, 0:36], brt[:], ALU.add)
                        P.reduce(r[:, 4:5], lg[:, 0:4], op=ALU.max)
                        P.ts(r[:, 8:12], lg[:, 0:4], r[:, 4:5], None, op0=ALU.is_equal)
                        P.ts(r[:, 5:6], r[:, 4:5], -1.0, None, op0=ALU.mult)
                        P.act(r[:, 12:16], lg[:, 0:4], AF.Exp, bias=r[:, 5:6], accum_out=r[:, 6:7])
                        P.recip(r[:, 7:8], r[:, 6:7])
                        P.ts(r[:, 16:20], r[:, 8:12], 1e30, -1e30, op0=ALU.mult, op1=ALU.add)
                        ev = e_[:, 0, :].rearrange("p (g k) -> p g k", k=8)
                        P.tt(ev, lg[:, 4:36].rearrange("p (g k) -> p g k", k=8),
                             r[:, 16:20].unsqueeze(2).to_broadcast([128, 4, 8]), ALU.add)
                        P.reduce(r[:, 20:21], e_[:, 0, :], op=ALU.max)
                        P.ts(e_[:, 1, :], e_[:, 0, :], r[:, 20:21], None, op0=ALU.is_equal)
                        P.stt(e_[:, 0, :], e_[:, 1, :], -1e30, e_[:, 0, :], ALU.mult, ALU.add)
                        P.reduce(r[:, 21:22], e_[:, 0, :], op=ALU.max)
                        P.ts(e_[:, 2, :], e_[:, 0, :], r[:, 21:22], None, op0=ALU.is_equal)
                        P.tt(r[:, 22:23], r[:, 20:21], r[:, 21:22], ALU.subtract)
                        P.act(r[:, 22:23], r[:, 22:23], AF.Exp, scale=-1.0)
                        P.ts(r[:, 22:23], r[:, 22:23], 1.0, None, op0=ALU.add)
                        P.recip(r[:, 22:23], r[:, 22:23])
                        P.tt(r[:, 22:23], r[:, 22:23], r[:, 7:8], ALU.mult)
                        P.tt(r[:, 23:24], r[:, 7:8], r[:, 22:23], ALU.subtract)
                        P.tt(asg[:, i, :], e_[:, 1, :], e_[:, 2, :], ALU.add)
                        P.copy(asgb[:, i, :], asg[:, i, :])
                        P.ts(wmat[:, i, :], e_[:, 1, :], r[:, 22:23], None, op0=ALU.mult)
                        P.stt(wmat[:, i, :], e_[:, 2, :], r[:, 23:24], wmat[:, i, :], ALU.mult, ALU.add)
                    for i in range(8):
                        for j in range(i):
                            P.mm(pb[3][:, 0:32], onesb[:], asgb[:, j, :], start=(j == 0), stop=False)
                        P.mm(pb[3][:, 0:32], tri[:], asgb[:, i, :], start=(i == 0), stop=True)
                        P.copy(pos[:, i, :], pb[3][:, 0:32])

                P.region(_router)

            Wg.append(P.sb([128, 16, EFF], BF16, "Wg1"))
            Wu.append(P.sb([128, 16, EFF], BF16, "Wu1"))
            Wd = [P.sb([128, 4, DM], BF16, f"Wd{i}") for i in range(1)]
            sel = [P.sb([128, 8, 128], BF16, f"sel{i}") for i in range(2)]
            selw = [P.sb([128, 8, 128], BF16, f"selw{i}") for i in range(2)]
            selwT = P.sb([128, 8, 128], BF16, "selwT")
            XeT = P.sb([128, 16, 128], BF16, "XeT")
            sgt = P.sb([128, 512], F32, "sgt")
            hidT = [P.sb([128, 4, 128], BF16, f"hidT{i}") for i in range(2)]
            ye = P.sb([128, DM], BF16, "ye")

            def load_e(e):
                P.dma("gpsimd", Wg[e % 2][:], L["e_gate"][e].rearrange("(c p) f -> p c f", p=128))
                P.dma("gpsimd", Wu[e % 2][:], L["e_up"][e].rearrange("(c p) f -> p c f", p=128))

            def stageA(e):
                par = e % 2
                if e + 1 < NEXP:
                    load_e(e + 1)
                sl = sel[par]; sw = selw[par]
                for i in range(8):
                    P.ts(sl[:, i, :], iota[:], pos[:, i, e:e + 1], asg[:, i, e:e + 1], op0=ALU.is_equal, op1=ALU.mult)
                    P.ts(sw[:, i, :], iota[:], pos[:, i, e:e + 1], wmat[:, i, e:e + 1], op0=ALU.is_equal, op1=ALU.mult)
                for c4 in range(4):
                    bk = pb[c4 % 2]
                    for kk in range(4):
                        c = c4 * 4 + kk
                        for i in range(8):
                            P.mm(bk[:, kk * 128:(kk + 1) * 128], h2[:, i, c * 128:(c + 1) * 128], sl[:, i, :],
                                 start=(i == 0), stop=(i == 7))
                    P.act(XeT[:, c4 * 4:(c4 + 1) * 4, :], bk[:].rearrange("p (a d) -> p a d", d=128), AF.Copy)
                for fc in range(4):
                    for c in range(16):
                        P.mm(pb[6][:, fc * 128:(fc + 1) * 128], Wg[par][:, c, fc * 128:(fc + 1) * 128], XeT[:, c, :],
                             start=(c == 0), stop=(c == 15))
                for fc in range(4):
                    for c in range(16):
                        P.mm(pb[7][:, fc * 128:(fc + 1) * 128], Wu[par][:, c, fc * 128:(fc + 1) * 128], XeT[:, c, :],
                             start=(c == 0), stop=(c == 15))
                P.act(sgt[:], pb[6][:], AF.Silu)
                P.tt(hidT[par][:].rearrange("p a d -> p (a d)"), sgt[:], pb[7][:], ALU.mult)

            def stageB(e):
                par = e % 2
                sw = selw[par]
                P.dma("gpsimd", Wd[0][:], L["e_down"][e].rearrange("(c p) n -> p c n", p=128))
                bT = pbb(5)
                for i in range(8):
                    P.tr(bT[:, i * 128:(i + 1) * 128], sw[:, i, :], identb[:])
                P.act(selwT[:].rearrange("p a d -> p (a d)"), bT[:, 0:1024], AF.Copy)
                for nb in range(4):
                    bk = pb[2 + nb % 2]
                    for fc in range(4):
                        P.mm(bk[:], hidT[par][:, fc, :], Wd[0][:, fc, nb * 512:(nb + 1) * 512], start=(fc == 0), stop=(fc == 3))
                    P.act(ye[:, nb * 512:(nb + 1) * 512], bk[:], AF.Copy)
                k = 0
                for i in range(8):
                    for nb in range(4):
                        bk = pb[2 + (k % 3)]
                        k += 1
                        P.mm(bk[:], selwT[:, i, :], ye[:, nb * 512:(nb + 1) * 512])
                        P.tt(x2[:, i, nb * 512:(nb + 1) * 512], x2[:, i, nb * 512:(nb + 1) * 512], bk[:], ALU.add)

            P.region(lambda: stageA(0))
            EW = 8
            for e0 in range(0, NEXP, EW):
                items = []
                for e in range(e0, min(e0 + EW, NEXP)):
                    items += P.record(lambda e=e: stageB(e))
                    if e + 1 < NEXP:
                        items += P.record(lambda e=e: stageA(e + 1))
                P.schedule(items)

        with P.scope():
            gfb = P.sb([128, DM], F32, "gfb")
            P.dma("sync", gfb[:], L["gf_d"].partition_broadcast(128))
            ot = [P.sb([128, DM], F32, f"ot{i}") for i in range(2)]
            junk = P.sb([128, DM], BF16, "junk5")
            rr = [P.sb([128, 4], F32, f"rf{i}") for i in range(2)]
            def _final():
                for i in range(8):
                    r = rr[i % 2]
                    P.act(junk[:], x2[:, i, :], AF.Square, accum_out=r[:, 0:1])
                    P.ts(r[:, 1:2], r[:, 0:1], 1.0 / DM, EPS, op0=ALU.mult, op1=ALU.add)
                    P.act(r[:, 2:3], r[:, 1:2], AF.Sqrt)
                    P.recip(r[:, 3:4], r[:, 2:3])
                    P.stt(ot[i % 2][:], x2[:, i, :], r[:, 3:4], gfb[:], ALU.mult, ALU.mult)
                    P.dma("sync", out[i * 128:(i + 1) * 128, :], ot[i % 2][:])

            P.region(_final)


def _constants(core_half):
    bf = ml_dtypes.bfloat16
    c = {}
    c["c_identb"] = np.eye(128, dtype=np.float32).astype(bf)
    c["c_identf"] = np.eye(128, dtype=np.float32)
    hp = np.arange(128) // 64
    sp = np.arange(128) % 64
    same = (hp[:, None] == hp[None, :])
    strict_bd = (same & (sp[:, None] < sp[None, :])).astype(np.float32)
    incl_c = (sp[:, None] <= np.arange(64)[None, :]).astype(np.float32)
    c["c_maskS"] = np.concatenate([strict_bd, incl_c, strict_bd, incl_c], axis=1)
    c["c_maskL"] = (same & (sp[None, :] < sp[:, None])).astype(np.float32)
    c["c_bones"] = same.astype(np.float32).astype(bf)
    c["c_hmask"] = np.stack([(hp == 0), (hp == 1)], axis=1).astype(np.float32)
    rs = np.ones((128, 1024), np.float32)
    rs[:, ::64] = 0.0
    c["c_reset"] = rs.astype(bf)
    pos = np.arange(T, dtype=np.float32) - (1024.0 if core_half == 0 else 0.0)
    inv = (10000.0 ** (-np.arange(64, dtype=np.float32) / 64)).astype(np.float32)
    ang = pos[:, None] * inv[None, :]
    cos, sin = np.cos(ang).astype(np.float32), np.sin(ang).astype(np.float32)
    sc = np.float32(128 ** -0.5)
    cs = np.stack([cos, sin, cos * sc, sin * sc], axis=1)
    c["c_cs"] = np.ascontiguousarray(cs.reshape(16, 128, 4, 64).transpose(1, 0, 2, 3))
    lg = np.log1p(-np.exp2(-5.0 - np.arange(8, dtype=np.float64)))
    idx = np.arange(128, dtype=np.float64)
    j = idx[:, None]
    i = idx[None, :]
    same_c = (j // 64 == i // 64)
    rm = np.zeros((128, 8, 128), np.float64)
    for h in range(8):
        m = np.where(same_c, np.exp(np.abs(i - j) * lg[h]), np.where(j < i, np.exp((i - j) * lg[h]), 0.0))
        rm[:, h, :] = m * np.exp(-(i + 1) * lg[h])
    c["c_rmask"] = rm.astype(np.float32)
    rd = np.zeros((128, 16), np.float64)
    for h in range(8):
        rd[:, h] = np.exp((idx + 1) * lg[h])
        rd[:, 8 + h] = np.exp((127 - idx) * lg[h])
    c["c_rdec"] = rd.astype(np.float32)
    c["c_tri"] = (idx[:, None] < idx[None, :]).astype(np.float32).astype(bf)
    c["c_iota"] = np.tile(np.arange(128, dtype=np.float32)[None, :], (128, 1))
    return c


def _prep_shared(inp):
    f = np.float32
    w_in = np.asarray(inp["w_in"][0], f)
    d = {}
    d["g1T"] = np.ascontiguousarray(np.asarray(inp["norm1_g"][0], f).reshape(16, 128).T)
    d["w_lora"] = np.ascontiguousarray(w_in[:, 3072:3520])
    d["w_rw"] = np.ascontiguousarray(np.stack(
        [np.concatenate([w_in[:, k * 1024 + p * 128:k * 1024 + (p + 1) * 128] for k in range(3)], axis=1) for p in range(8)]))
    ro = RET_OFF
    d["w_ret"] = np.ascontiguousarray(np.stack(
        [np.concatenate([w_in[:, ro + h * 128:ro + (h + 1) * 128],
                         w_in[:, ro + 1024 + h * 128:ro + 1024 + (h + 1) * 128],
                         w_in[:, ro + 2048 + h * 256:ro + 2048 + (h + 1) * 256],
                         w_in[:, ro + 4096 + h * 256:ro + 4096 + (h + 1) * 256]], axis=1) for h in range(8)]))
    d["w_in"] = w_in
    mu = np.asarray(inp["mu_shift"][0], f)
    lp = np.zeros((128, 4), f)
    lp[:96, 0] = mu[3072:3168]
    lp[:96, 1] = mu[3168:3264]
    lp[:, 2] = mu[3264:3392]
    lp[:, 3] = mu[3392:3520]
    d["lprm"] = lp
    vecs = [mu[0:1024], mu[1024:2048], mu[2048:3072], np.asarray(inp["rw_w0"][0], f), np.asarray(inp["rw_a0"][0], f),
            np.asarray(inp["rw_k_k"][0], f), np.asarray(inp["rw_k_a"][0], f), np.asarray(inp["rw_r_k"][0], f)]
    d["rwprm"] = np.ascontiguousarray(np.stack([v.reshape(8, 128).T for v in vecs], axis=2))
    d["rw_w2"] = np.asarray(inp["rw_w2"][0], f)
    d["rw_a2"] = np.asarray(inp["rw_a2"][0], f)
    d["rw_g2"] = np.asarray(inp["rw_g2"][0], f)
    d["lnx_g"] = np.asarray(inp["rw_lnx_g"][0], f).reshape(1, 1024)
    d["lnx_b"] = np.asarray(inp["rw_lnx_b"][0], f).reshape(1, 1024)
    d["ret_g"] = np.asarray(inp["ret_norm_g"][0], f).reshape(1, 2048)
    d["w_up_a"] = np.asarray(inp["w_up_a"][0], f)
    d["w_up_b"] = np.asarray(inp["w_up_b"][0], f)
    d["w_out"] = np.asarray(inp["w_out"][0], f)
    d["g2"] = np.asarray(inp["norm2_g"][0], f).reshape(1, DM)
    d["gf"] = np.asarray(inp["final_norm_g"], f).reshape(1, DM)
    d["w_rt"] = np.ascontiguousarray(np.concatenate([np.asarray(inp["w_group"][0], f), np.asarray(inp["w_expert"][0], f)], axis=1))
    d["b_rt"] = np.concatenate([np.asarray(inp["b_group"][0], f), np.asarray(inp["b_expert"][0], f)]).reshape(1, 36)
    d["e_gate"] = np.asarray(inp["e_gate"][0], f)
    d["e_up"] = np.asarray(inp["e_up"][0], f)
    d["e_down"] = np.asarray(inp["e_down"][0], f)
    return d


_CACHE = {}


def run(inp, stop_after=99, cores=None, trace=False):
    key = stop_after
    if key not in _CACHE:
        _CACHE[key] = build_program(stop_after)
    nc, declared = _CACHE[key]
    shared = _prep_shared(inp)
    consts = [_constants(0), _constants(1)]
    xfull = np.asarray(inp["x"], np.float32)
    cores = list(range(8)) if cores is None else cores
    in_maps = []
    for c in cores:
        b, half = c // 2, c % 2
        if half == 0:
            xs = np.concatenate([np.zeros((1024, DM), np.float32), xfull[b, :1024]], axis=0)
        else:
            xs = xfull[b]
        m = {"x": np.ascontiguousarray(xs)}
        m.update(shared)
        m.update(consts[half])
        in_maps.append({k: m[k] for k in declared})
    res = run_bass_kernel_spmd(nc, in_maps, core_ids=list(range(len(cores))), trace=trace)
    return res


def kernel(**inputs):
    res = run(inputs)
    outp = np.zeros((4, 2048, DM), np.float32)
    for c in range(8):
        b, half = c // 2, c % 2
        outp[b, half * 1024:(half + 1) * 1024] = res.results[c]["out"]
    return outp
```

```python
from contextlib import ExitStack, contextmanager
import math
import numpy as np
import ml_dtypes
import concourse.bass as bass
import concourse.mybir as mybir
from concourse.bass_utils import run_bass_kernel_spmd

F32 = mybir.dt.float32
BF16 = mybir.dt.bfloat16
AF = mybir.ActivationFunctionType
ALU = mybir.AluOpType
AX = mybir.AxisListType

ENGS = ("tensor", "vector", "scalar", "gpsimd", "sync")
EPOCH = 30000
NDMA = 8

DM = 2048
T = 2048
TO = 1024
RW_COLS = 3520
RET_OFF = 3520
GATE_OFF = 3520 + 6144
IN_COLS = 13760
EPS = 1e-6
RW_LN_EPS = 64e-5
CDEC = -math.exp(-0.5)
NEXP = 32
EFF = 512


class Prog:
    def __init__(self, nc):
        self.nc = nc
        self.es = ExitStack()
        self.stack = [self.es]
        self.q = {e: [] for e in ENGS}
        self.cnt = {e: 0 for e in ENGS}
        self.esem = {}
        self.nsem = 0
        for e in ENGS:
            self.esem[e] = self._newsem(f"e_{e}")
        self.dsem = {e: [self._newsem(f"d_{e}{i}") for i in range(NDMA)] for e in ("sync", "scalar", "gpsimd")}
        self.dcount = {e: [0] * NDMA for e in self.dsem}
        self.dnext = {e: 0 for e in self.dsem}
        self.lastw = {}
        self.reads = {}
        self.known = {e: {} for e in ENGS}
        self.ntensors = 0
        self.rec = None

    def record(self, fn):
        assert self.rec is None
        self.rec = []
        try:
            fn()
        finally:
            lst, self.rec = self.rec, None
        return lst

    def replay(self, item):
        kind, eng, a, reads, writes, kw = item
        if kind == "op":
            self.op(eng, a, reads, writes)
        else:
            self.dma(eng, a[0], a[1], reads=reads, writes=writes, **kw)

    @staticmethod
    def _fsize(x):
        sh = getattr(x, "shape", None)
        if sh is None:
            return 64
        n = 1
        for d in sh[1:]:
            n *= d
        return n

    def _cost(self, eng, reads, writes):
        if eng == "tensor":
            n = self._fsize(reads[1]) if len(reads) > 1 else 128
            return 70.0 + n / 2.0
        n = max([self._fsize(w) for w in writes] + [1])
        if eng == "vector":
            return 120.0 + n * 0.9
        if eng == "scalar":
            return 220.0 + n * 0.8
        if eng == "gpsimd":
            return 200.0 + n * 1.5
        return 100.0

    def schedule(self, items, lat=20.0):
        n = len(items)
        if n == 0:
            return
        lastw, readers = {}, {}
        preds = [set() for _ in range(n)]
        succs = [[] for _ in range(n)]
        dur = [0.0] * n
        engs = [None] * n
        for i, it in enumerate(items):
            kind, eng, a, reads, writes, extra = it
            engs[i] = eng
            dur[i] = extra if (kind == "op" and extra is not None) else 100.0
            rk = [k for r in reads for k in self._keys(r)]
            wk = [k for w in writes for k in self._keys(w)]
            for k in rk:
                if k in lastw:
                    preds[i].add(lastw[k])
                if k.startswith("bank"):
                    for j in readers.get(k, ()):
                        if engs[j] != eng:
                            preds[i].add(j)
            for k in wk:
                if k in lastw:
                    preds[i].add(lastw[k])
                for j in readers.get(k, ()):
                    preds[i].add(j)
            for k in rk:
                readers.setdefault(k, []).append(i)
            for k in wk:
                lastw[k] = i
                readers[k] = []
            preds[i].discard(i)
            for j in preds[i]:
                succs[j].append(i)
        dlat = [2000.0 if items[i][0] == "dma" else dur[i] for i in range(n)]
        prio = [0.0] * n
        for i in range(n - 1, -1, -1):
            m = 0.0
            for j in succs[i]:
                if prio[j] > m:
                    m = prio[j]
            prio[i] = dlat[i] + lat + m
        npred = [len(p) for p in preds]
        ready = [i for i in range(n) if npred[i] == 0]
        fin = [0.0] * n
        efree = {}
        order = []
        rdy_t = [0.0] * n
        while ready:
            best = None
            bkey = None
            for i in ready:
                st = max(efree.get(engs[i], 0.0), rdy_t[i])
                key = (st, -prio[i], i)
                if bkey is None or key < bkey:
                    bkey, best = key, i
            ready.remove(best)
            st = bkey[0]
            issue = dur[best] if items[best][0] == "op" else 60.0
            efree[engs[best]] = st + issue
            fin[best] = st + dlat[best]
            order.append(best)
            for j in succs[best]:
                t = fin[best] + (lat if engs[j] != engs[best] or engs[j] != "tensor" else 0.0)
                if t > rdy_t[j]:
                    rdy_t[j] = t
                npred[j] -= 1
                if npred[j] == 0:
                    ready.append(j)
        assert len(order) == n
        for i in order:
            self.replay(items[i])

    def merge(self, A, B):
        a, b = len(A), len(B)
        if a == 0:
            for it in B:
                self.replay(it)
            return
        done = 0
        for i, it in enumerate(A):
            self.replay(it)
            upto = ((i + 1) * b) // a
            while done < upto:
                self.replay(B[done])
                done += 1
        while done < b:
            self.replay(B[done])
            done += 1

    def region(self, fn):
        self.schedule(self.record(fn))

    def _newsem(self, name):
        self.nsem += 1
        return self.es.enter_context(self.nc.semaphore(f"{name}_{self.nsem}"))

    def sb(self, shape, dt=F32, name=None):
        self.ntensors += 1
        return self.stack[-1].enter_context(self.nc.sbuf_tensor(f"{name or 'sb'}_{self.ntensors}", list(shape), dt))

    def ps(self, shape, dt=F32, name=None):
        self.ntensors += 1
        return self.stack[-1].enter_context(self.nc.psum_tensor(f"{name or 'ps'}_{self.ntensors}", list(shape), dt))

    @contextmanager
    def scope(self):
        st = ExitStack()
        self.stack.append(st)
        try:
            yield
        finally:
            self.barrier()
            self.stack.pop()
            st.close()

    @staticmethod
    def _keys(x):
        if isinstance(x, (str, tuple)):
            return [x]
        t = getattr(x, "tensor", None)
        if t is None:
            return [x.name]
        name = t.name
        if name.startswith("bankw"):
            apl = x.ap
            rs = apl[0][0]
            col0 = x.offset % rs
            ext = 1 + sum((n - 1) * st for st, n in apl[1:])
            half = rs // 2
            ks = []
            if col0 < half:
                ks.append(name + "_lo")
            if col0 + ext > half:
                ks.append(name + "_hi")
            return ks
        return [name]

    def _need(self, eng, ev, waits):
        if ev is None:
            return
        _, sem, val = ev
        k = self.known[eng]
        if k.get(sem, 0) >= val:
            return
        k[sem] = val
        waits[sem] = max(waits.get(sem, 0), val)

    def _deps(self, eng, reads, writes):
        waits = {}
        for r in reads:
            for k in self._keys(r):
                ev = self.lastw.get(k)
                if ev is not None and not (ev[0] == eng and eng == "tensor"):
                    self._need(eng, ev, waits)
                if k.startswith("bank"):
                    for rv in self.reads.get(k, ()):
                        if rv[0] != eng:
                            self._need(eng, rv, waits)
        for w in writes:
            for k in self._keys(w):
                ev = self.lastw.get(k)
                if ev is not None and ev[0] != eng:
                    self._need(eng, ev, waits)
                for rv in self.reads.get(k, ()):
                    if rv[0] != eng:
                        self._need(eng, rv, waits)
        return waits

    def _commit(self, ev, reads, writes):
        for r in reads:
            for k in self._keys(r):
                self.reads.setdefault(k, []).append(ev)
        for w in writes:
            for k in self._keys(w):
                self.lastw[k] = ev
                self.reads[k] = []

    def op(self, eng, fn, reads=(), writes=(), cost=None):
        if self.rec is not None:
            if cost is None:
                cost = self._cost(eng, reads, writes)
            self.rec.append(("op", eng, fn, list(reads), list(writes), cost))
            return None
        waits = self._deps(eng, reads, writes)
        if self.cnt[eng] >= EPOCH:
            self.esem[eng] = self._newsem(f"e_{eng}")
            self.cnt[eng] = 0
        self.cnt[eng] += 1
        sem = self.esem[eng]
        val = self.cnt[eng]
        wl = list(waits.items())

        def run(e, fn=fn, wl=wl, sem=sem):
            for s, v in wl:
                e.wait_ge(s, v)
            fn(e).then_inc(sem, 1)
        self.q[eng].append(run)
        ev = (eng, sem, val)
        self._commit(ev, reads, writes)
        return ev

    def dma(self, eng, out, in_, reads=None, writes=None, **kw):
        reads = [in_] if reads is None else reads
        writes = [out] if writes is None else writes
        if self.rec is not None:
            self.rec.append(("dma", eng, (out, in_), list(reads), list(writes), kw))
            return None
        waits = self._deps(eng, reads, writes)
        i = self.dnext[eng]
        self.dnext[eng] = (i + 1) % NDMA
        sem = self.dsem[eng][i]
        prev = self.dcount[eng][i]
        if prev > 0 and self.known[eng].get(sem, 0) < 16 * prev:
            waits[sem] = 16 * prev
            self.known[eng][sem] = 16 * prev
        self.dcount[eng][i] = prev + 1
        val = 16 * (prev + 1)
        wl = list(waits.items())

        def run(e, wl=wl, sem=sem, out=out, in_=in_, kw=kw):
            for s, v in wl:
                e.wait_ge(s, v)
            e.dma_start(out=out, in_=in_, **kw).then_inc(sem, 16)
        self.q[eng].append(run)
        ev = ("dma_" + eng, sem, val)
        self._commit(ev, reads, writes)
        return ev

    def barrier(self):
        evs = []
        for e in ENGS:
            if self.cnt[e] > 0:
                evs.append((e, self.esem[e], self.cnt[e]))
        for e in self.dsem:
            for i in range(NDMA):
                if self.dcount[e][i] > 0:
                    evs.append(("dma_" + e, self.dsem[e][i], 16 * self.dcount[e][i]))
        for eng in ENGS:
            waits = {}
            for ev in evs:
                if ev[0] != eng:
                    self._need(eng, ev, waits)
            wl = list(waits.items())
            if wl:
                def run(e, wl=wl):
                    for s, v in wl:
                        e.wait_ge(s, v)
                self.q[eng].append(run)

    def emit(self):
        nc = self.nc
        with nc.Block() as block:
            @block.tensor
            def _(e):
                for f in self.q["tensor"]:
                    f(e)

            @block.vector
            def _(e):
                for f in self.q["vector"]:
                    f(e)

            @block.scalar
            def _(e):
                for f in self.q["scalar"]:
                    f(e)

            @block.gpsimd
            def _(e):
                for f in self.q["gpsimd"]:
                    f(e)

            @block.sync
            def _(e):
                for f in self.q["sync"]:
                    f(e)
        self.es.close()

    def mm(self, out, lhsT, rhs, start=True, stop=True):
        return self.op("tensor", lambda e: e.matmul(out, lhsT, rhs, start=start, stop=stop),
                       reads=[lhsT, rhs], writes=[out])

    def tr(self, out, in_, ident):
        return self.op("tensor", lambda e: e.transpose(out, in_, ident), reads=[in_, ident], writes=[out])

    def act(self, out, in_, func, bias=None, scale=None, accum_out=None):
        kw = {}
        reads = [in_]
        writes = [out]
        if bias is not None:
            kw["bias"] = bias
            if not isinstance(bias, (int, float)):
                reads.append(bias)
        if scale is not None:
            kw["scale"] = scale
            if not isinstance(scale, (int, float)):
                reads.append(scale)
        if accum_out is not None:
            kw["accum_out"] = accum_out
            writes.append(accum_out)
        return self.op("scalar", lambda e: e.activation(out, in_, func, **kw), reads=reads, writes=writes)

    def tt(self, out, in0, in1, op, eng="vector"):
        return self.op(eng, lambda e: e.tensor_tensor(out, in0, in1, op), reads=[in0, in1], writes=[out])

    def ts(self, out, in0, s1, s2=None, op0=ALU.mult, op1=None, eng="vector"):
        reads = [in0] + [s for s in (s1, s2) if s is not None and not isinstance(s, (int, float))]
        kw = {}
        if op1 is not None:
            kw["op1"] = op1
        return self.op(eng, lambda e: e.tensor_scalar(out, in0, s1, s2, op0, **kw), reads=reads, writes=[out])

    def stt(self, out, in0, scalar, in1, op0, op1):
        reads = [in0, in1] + ([scalar] if not isinstance(scalar, (int, float)) else [])
        return self.op("vector", lambda e: e.scalar_tensor_tensor(out, in0, scalar, in1, op0, op1),
                       reads=reads, writes=[out])

    def copy(self, out, in_, eng="vector"):
        if eng == "scalar":
            return self.op(eng, lambda e: e.copy(out, in_), reads=[in_], writes=[out])
        return self.op(eng, lambda e: e.tensor_copy(out, in_), reads=[in_], writes=[out])

    def memset(self, ap, val, eng="vector"):
        return self.op(eng, lambda e: e.memset(ap, val), reads=[], writes=[ap])

    def recip(self, out, in_):
        return self.op("vector", lambda e: e.reciprocal(out, in_), reads=[in_], writes=[out])

    def reduce(self, out, in_, op=ALU.add, axis=AX.X):
        return self.op("vector", lambda e: e.tensor_reduce(out, in_, axis, op), reads=[in_], writes=[out])


def build_program(stop_after=99):
    nc = bass.Bass("TRN2", target_bir_lowering=False)

    declared = []

    def din(name, shape, dt=F32, need=0):
        if stop_after < need:
            return None
        declared.append(name)
        return nc.dram_tensor(name, list(shape), dt, kind="ExternalInput").ap()

    x = din("x", [T, DM])
    g1T_d = din("g1T", [128, 16])
    w_lora = din("w_lora", [DM, 448])
    w_rw = din("w_rw", [8, DM, 384])
    w_ret = din("w_ret", [8, DM, 768], need=2)
    w_in = din("w_in", [DM, IN_COLS], need=3)
    lprm_d = din("lprm", [128, 4])
    rwprm_d = din("rwprm", [128, 8, 8])
    rw_w2 = din("rw_w2", [96, 1024])
    rw_a2 = din("rw_a2", [96, 1024])
    rw_g2 = din("rw_g2", [256, 1024])
    lnx_g = din("lnx_g", [1, 1024])
    lnx_b = din("lnx_b", [1, 1024])
    ret_g = din("ret_g", [1, 2048], need=2)
    w_up_a = din("w_up_a", [1024, DM], need=3)
    w_up_b = din("w_up_b", [2048, DM], need=3)
    w_out = din("w_out", [DM, DM], need=4)
    g2_d = din("g2", [1, DM], need=4)
    gf_d = din("gf", [1, DM], need=4)
    w_rt = din("w_rt", [DM, 36], need=4)
    b_rt = din("b_rt", [1, 36], need=4)
    e_gate = din("e_gate", [NEXP, DM, EFF], need=4)
    e_up = din("e_up", [NEXP, DM, EFF], need=4)
    e_down = din("e_down", [NEXP, EFF, DM], need=4)
    c_identb = din("c_identb", [128, 128], BF16)
    c_identf = din("c_identf", [128, 128])
    c_maskS = din("c_maskS", [128, 384])
    c_maskL = din("c_maskL", [128, 128])
    c_bones = din("c_bones", [128, 128], BF16)
    c_hmask = din("c_hmask", [128, 2])
    c_reset = din("c_reset", [128, 1024], BF16)
    c_cs = din("c_cs", [128, 16, 4, 64], need=2)
    c_rmask = din("c_rmask", [128, 8, 128], need=2)
    c_rdec = din("c_rdec", [128, 16], need=2)
    c_tri = din("c_tri", [128, 128], BF16, need=4)
    c_iota = din("c_iota", [128, 128], need=4)
    out = nc.dram_tensor("out", [TO, DM], F32, kind="ExternalOutput").ap()
    oaT_d = nc.dram_tensor("oaT_scr", [8, 128, TO], BF16, kind="Internal").ap()
    obT_d = nc.dram_tensor("obT_scr", [16, 128, TO], BF16, kind="Internal").ap()
    mT_d = nc.dram_tensor("mT_scr", [16, 128, TO], BF16, kind="Internal").ap()
    dbg = None
    if stop_after < 99:
        dbg = nc.dram_tensor("dbg", [24, 128, TO], BF16, kind="ExternalOutput").ap()

    P = Prog(nc)
    P.es.enter_context(nc.allow_low_precision("bf16 PE transposes write bf16 PSUM"))
    pb = [P.ps([128, 512], F32, f"bank{i}") for i in range(6)]
    pbw = P.ps([128, 1024], F32, "bankw")
    pb.append(pbw[:, 0:512])
    pb.append(pbw[:, 512:1024])

    def pbb(i):
        return pb[i][:].bitcast(BF16)

    identb = P.sb([128, 128], BF16, "identb")
    identf = P.sb([128, 128], F32, "identf")
    P.dma("sync", identb[:], c_identb)
    P.dma("sync", identf[:], c_identf)

    with P.scope():
        hT_hi = P.sb([128, 16, 1024], BF16, "hT_hi")
        with P.scope():
            hT_lo = P.sb([128, 16, 1024], BF16, "hT_lo")

            def hTs(c, t0, n):
                if t0 < 1024:
                    return hT_lo[:, c, t0:t0 + n]
                return hT_hi[:, c, t0 - 1024:t0 - 1024 + n]

            g1T = P.sb([128, 16], F32, "g1T")
            P.dma("sync", g1T[:], g1T_d)
            with P.scope():
                xin = [P.sb([128, DM], F32, f"xin{i}") for i in range(4)]
                xnb = [P.sb([128, DM], BF16, f"xnb{i}") for i in range(4)]
                junk = P.sb([128, DM], BF16, "junk")
                st = [P.sb([128, 4], F32, f"st{i}") for i in range(4)]
                def _phase0():
                    for i in range(16):
                        xi = xin[i % 4]
                        xb = xnb[i % 4]
                        s = st[i % 4]
                        P.dma("sync", xi[:], x[i * 128:(i + 1) * 128, :])
                        P.act(junk[:], xi[:], AF.Square, accum_out=s[:, 0:1])
                        P.ts(s[:, 1:2], s[:, 0:1], 1.0 / DM, EPS, op0=ALU.mult, op1=ALU.add)
                        P.act(s[:, 2:3], s[:, 1:2], AF.Sqrt)
                        P.recip(s[:, 3:4], s[:, 2:3])
                        P.ts(xb[:], xi[:], s[:, 3:4], None, op0=ALU.mult)
                        for c4 in range(4):
                            bk = pbb(c4 % 2)
                            for k in range(4):
                                c = c4 * 4 + k
                                P.tr(bk[:, k * 128:(k + 1) * 128], xb[:, c * 128:(c + 1) * 128], identb[:])
                            for k in range(4):
                                c = c4 * 4 + k
                                dst = hTs(c, i * 128, 128)
                                if c4 % 2 == 0:
                                    P.ts(dst, bk[:, k * 128:(k + 1) * 128], g1T[:, c:c + 1], None, op0=ALU.mult)
                                else:
                                    P.act(dst, bk[:, k * 128:(k + 1) * 128], AF.Copy, scale=g1T[:, c:c + 1])

                P.region(_phase0)

            rw_phase(P, nc, locals())
            if stop_after >= 2:
                ret_phase(P, nc, locals())
        if stop_after < 3:
            with P.scope():
                dt_ = P.sb([128, 24, TO], BF16, "dbgt")
                P.dma("sync", dt_[:, 0:8, :], oaT_d.rearrange("c p t -> p c t"))
                if stop_after >= 2:
                    P.dma("sync", dt_[:, 8:24, :], obT_d.rearrange("c p t -> p c t"))
                else:
                    P.memset(dt_[:, 8:24, :], 0.0)
                P.dma("sync", dbg.rearrange("c p t -> p c t"), dt_[:])
        else:
            gate_phase(P, nc, locals())
    if stop_after >= 4:
        tail_phase(P, nc, locals())
    else:
        with P.scope():
            zt = P.sb([128, DM], F32, "zt")
            P.memset(zt[:], 0.0)
            for i in range(8):
                P.dma("sync", out[i * 128:(i + 1) * 128, :], zt[:])
    P.barrier()
    P.emit()
    return nc, declared


def rw_phase(P, nc, L):
    pb = L["pb"]; pbb = L["pbb"]; hTs = L["hTs"]; identb = L["identb"]; pbw = L["pbw"]
    with P.scope():
        lprm = P.sb([128, 8], F32, "lprm")
        P.dma("sync", lprm[:, 0:4], L["lprm_d"])
        P.ts(lprm[:, 4:8], lprm[:, 0:4], -1.0, 1.0, op0=ALU.mult, op1=ALU.add)
        rwprm = P.sb([128, 8, 14], F32, "rwprm")
        onec = P.sb([128, 2], F32, "onec")
        P.memset(onec[:], 1.0)
        P.dma("sync", rwprm[:, :, 0:8], L["rwprm_d"])
        P.ts(rwprm[:, :, 8:11], rwprm[:, :, 0:3], -1.0, 1.0, op0=ALU.mult, op1=ALU.add)
        P.ts(rwprm[:, :, 11:12], rwprm[:, :, 6:7], -1.0, 1.0, op0=ALU.mult, op1=ALU.add)
        P.ts(rwprm[:, :, 12:14], rwprm[:, :, 3:5], -1.0, None, op0=ALU.mult)
        maskL = P.sb([128, 128], F32, "maskL")
        bones = P.sb([128, 128], BF16, "bones")
        hmask = P.sb([128, 2], F32, "hmask")
        reset = P.sb([128, 1024], BF16, "reset")
        P.dma("sync", maskL[:], L["c_maskL"])
        P.dma("sync", bones[:], L["c_bones"])
        P.dma("sync", hmask[:], L["c_hmask"])
        P.dma("sync", reset[:], L["c_reset"])
        w2 = P.sb([96, 1024], BF16, "w2")
        a2 = P.sb([96, 1024], BF16, "a2")
        g2 = P.sb([128, 2, 1024], BF16, "g2")
        P.dma("gpsimd", w2[:], L["rw_w2"])
        P.dma("gpsimd", a2[:], L["rw_a2"])
        P.dma("gpsimd", g2[:], L["rw_g2"].rearrange("(c p) n -> p c n", p=128))
        lgb = P.sb([64, 256], F32, "lgb")

        txw = P.sb([96, T], BF16, "txw")
        xab = P.sb([96, T], BF16, "xab")
        sgx = P.sb([128, 2, TO], BF16, "sgx")
        saved = P.sb([128, 4], F32, "saved")

        with P.scope():
            wl = P.sb([128, 16, 448], BF16, "wl")
            raw = P.sb([128, 1025], F32, "raw")
            tmpF = P.sb([128, 1024], F32, "tmpF")

            def project(wt, c0, M, t0):
                for blk in range(2):
                    ps = pb[blk % 2]
                    for c in range(16):
                        P.mm(ps[0:M, :], wt[:, c, c0:c0 + M], hTs(c, t0 + blk * 512, 512),
                             start=(c == 0), stop=(c == 15))
                    P.act(raw[0:M, 1 + blk * 512:1 + (blk + 1) * 512], ps[0:M, :], AF.Copy)
            P.dma("gpsimd", wl[:], L["w_lora"].rearrange("(c p) n -> p c n", p=128))
            svl = P.sb([128, 4], F32, "svl")
            def _lora():
                for half in range(2):
                    t0 = half * 1024
                    for gi, (c0, M) in enumerate([(0, 96), (96, 96), (192, 128), (320, 128)]):
                        if half == 0:
                            P.memset(raw[0:M, 0:1], 0.0)
                        else:
                            P.copy(raw[0:M, 0:1], svl[0:M, gi:gi + 1])
                        project(wl, c0, M, t0)
                        tmp = tmpF[0:M, :]
                        P.ts(tmp, raw[0:M, 1:1025], lprm[0:M, 4 + gi:5 + gi], None, op0=ALU.mult)
                        P.stt(tmp, raw[0:M, 0:1024], lprm[0:M, gi:gi + 1], tmp, ALU.mult, ALU.add)
                        P.copy(svl[0:M, gi:gi + 1], raw[0:M, 1024:1025])
                        if gi == 0:
                            P.act(txw[:, t0:t0 + 1024], tmp, AF.Tanh)
                        elif gi == 1:
                            P.act(xab[:, t0:t0 + 1024], tmp, AF.Copy)
                        elif half == 1:
                            P.act(sgx[:, gi - 2, :], tmp, AF.Sigmoid)

            P.region(_lora)

        UL = 512
        NCU = 8
        ARx = [P.sb([128, NCU, 192], BF16, f"ARx{i}") for i in range(2)]
        BKx = [P.sb([128, NCU, 2, 2, 64], BF16, f"BKx{i}") for i in range(2)]
        BHx = [P.sb([128, NCU, 2, 64], BF16, f"BHx{i}") for i in range(2)]
        KHx = [P.sb([128, NCU, 2, 64], BF16, f"KHx{i}") for i in range(2)]
        Vx = [P.sb([128, NCU, 2, 64], BF16, f"Vx{i}") for i in range(2)]
        Vc = [P.sb([128, UL], BF16, f"Vc{i}") for i in range(2)]
        RKb = [P.sb([128, UL], BF16, f"RKb{i}") for i in range(2)]
        Pall = [P.sb([128, NCU, 128], BF16, f"Pall{i}") for i in range(2)]
        WC = [P.sb([128, NCU], F32, f"WC{i}") for i in range(2)]
        for lst in (ARx, BKx, BHx, KHx, Vx):
            for tl in lst:
                P.memset(tl[:], 0.0, eng="gpsimd")
        Hst = P.sb([128, 128], F32, "Hst")
        Hbf = P.sb([128, 128], BF16, "Hbf")
        wts = [P.sb([128, 16, 128], BF16, f"wts{i}") for i in range(3)]
        rksel = P.sb([128, 2], BF16, "rksel")
        maskS = P.sb([128, 384], F32, "maskS")
        P.dma("sync", maskS[:], L["c_maskS"])
        SCm = [P.sb([128, 384], BF16, f"SCm{i}") for i in range(2)]
        TM = [P.sb([128, 384], BF16, f"TM{i}") for i in range(2)]
        TMc = [P.sb([64, 2, 128], BF16, f"TMc{i}") for i in range(2)]
        Zs = [P.sb([128, 128], BF16, f"Zs{i}") for i in range(2)]
        Us = [P.sb([128, 128], BF16, f"Us{i}") for i in range(2)]
        XXg = [[P.sb([128, 1024], BF16, f"XX{g_}_{i}") for i in range(2)] for g_ in range(2)]
        Pmg = [P.sb([128, 512], BF16, f"Pm{g_}") for g_ in range(2)]
        ep = [P.sb([64, 32], F32, f"ep{i}") for i in range(2)]
        ey = [P.sb([64, 256], F32, f"ey{i}") for i in range(2)]
        ey2 = [P.sb([64, 256], F32, f"eyb{i}") for i in range(2)]
        eg = [P.sb([64, 256], F32, f"eg{i}") for i in range(2)]
        eo = [P.sb([64, 256], BF16, f"eo{i}") for i in range(2)]
        oaT_st = P.sb([128, 1024], BF16, "oaT_st")
        mS = maskS[:, 0:128]
        Fq = [P.sb([128, UL], F32, f"Fq{i}") for i in range(6)]
        Bq = [P.sb([128, UL], BF16, f"Bq{i}") for i in range(4)]
        rawq = P.sb([128, UL + 1], F32, "rawq")

        def v3(t_, hs):
            return t_[hs, :].rearrange("p (c t) -> p c t", t=64)

        def wload(p, xi):
            P.dma("gpsimd", wts[xi][:], L["w_rw"][p].rearrange("(c p) n -> p c n", p=128)[:, :, xi * 128:(xi + 1) * 128])

        def prep(p, q, ub):
            t0 = q * UL
            need_y = q >= 2
            prm = rwprm[:, p, :]
            cs = slice(p * 128, (p + 1) * 128)
            Rf, Kf, T1, SGb, Epos, CUMb = Fq
            ALb, KKb, RNb, KK2b = Bq
            arx, bkx, bhx, khx, vx, vc, rkb, pall, wc = ARx[ub], BKx[ub], BHx[ub], KHx[ub], Vx[ub], Vc[ub], RKb[ub], Pall[ub], WC[ub]
            for xi, dst in enumerate((Rf, Kf, T1)):
                if xi == 0 and not need_y:
                    if q == 1:
                        for c in range(16):
                            P.mm(pb[0][:, 0:2], wts[0][:, c, :], hTs(c, t0 + UL - 2, 2), start=(c == 0), stop=(c == 15))
                        P.act(saved[:, 0:1], pb[0][:, 1:2], AF.Copy)
                    continue
                if q == 0:
                    P.memset(rawq[:, 0:1], 0.0, eng="gpsimd")
                else:
                    P.copy(rawq[:, 0:1], saved[:, xi:xi + 1])
                ps = pb[xi % 2]
                for c in range(16):
                    P.mm(ps[:], wts[xi][:, c, :], hTs(c, t0, UL), start=(c == 0), stop=(c == 15))
                if q == 3 and p + 1 < 8:
                    wload(p + 1, xi)
                P.act(rawq[:, 1:UL + 1], ps[:], AF.Copy)
                P.act(dst[:], ps[:], AF.Copy, scale=prm[:, 8 + xi:9 + xi])
                P.stt(dst[:], rawq[:, 0:UL], prm[:, xi:xi + 1], dst[:], ALU.mult, ALU.add)
                P.copy(saved[:, xi:xi + 1], rawq[:, UL:UL + 1])
            for h in range(2):
                hs = slice(h * 64, (h + 1) * 64)
                P.act(vx[hs, :, h, :], v3(T1, hs), AF.Copy)
            if need_y:
                P.act(vc[:], T1[:], AF.Copy)
            P.mm(pb[0][:], w2[:, cs], txw[:, t0:t0 + UL])
            P.act(SGb[:], pb[0][:], AF.Exp, scale=-1.0, bias=prm[:, 12:13])
            P.act(SGb[:], SGb[:], AF.Ln, bias=onec[:, 0:1])
            P.act(SGb[:], SGb[:], AF.Exp, scale=-1.0)
            P.mm(pb[1][:], a2[:, cs], xab[:, t0:t0 + UL])
            P.act(ALb[:], pb[1][:], AF.Exp, scale=-1.0, bias=prm[:, 13:14])
            P.act(ALb[:], ALb[:], AF.Ln, bias=onec[:, 0:1])
            P.act(ALb[:], ALb[:], AF.Exp, scale=-1.0)
            P.op("vector", lambda e, o=CUMb, r_=reset, s_=SGb: e.tensor_tensor_scan(o[:], r_[:, 0:UL], s_[:], 0.0, ALU.mult, ALU.add),
                 reads=[reset, SGb], writes=[CUMb])
            P.tt(T1[:], CUMb[:], SGb[:], ALU.subtract)
            P.act(T1[:], T1[:], AF.Exp, scale=CDEC)
            cv = CUMb[:].rearrange("p (c t) -> p c t", t=64)
            P.tt(SGb[:].rearrange("p (c t) -> p c t", t=64), cv[:, :, 63:64].to_broadcast([128, NCU, 64]), cv, ALU.subtract)
            P.act(SGb[:], SGb[:], AF.Exp, scale=CDEC)
            P.act(Epos[:], CUMb[:], AF.Exp, scale=CDEC)
            P.act(CUMb[:], CUMb[:], AF.Exp, scale=-CDEC)
            Eprev, Eend, Eneg = T1, SGb, CUMb
            P.copy(wc[:], Epos[:].rearrange("p (c t) -> p c t", t=64)[:, :, 63])
            P.act(KKb[:], Kf[:], AF.Copy, scale=prm[:, 5:6])
            P.tt(KK2b[:], KKb[:], KKb[:], ALU.mult)
            P.mm(pb[0][:], bones[:], KK2b[:])
            P.ts(RNb[:], pb[0][:], 1e-18, None, op0=ALU.max)
            P.act(RNb[:], RNb[:], AF.Ln)
            P.act(RNb[:], RNb[:], AF.Exp, scale=-0.5)
            P.tt(KKb[:], KKb[:], RNb[:], ALU.mult)
            for h in range(2):
                hs = slice(h * 64, (h + 1) * 64)
                P.tt(arx[hs, :, h * 64:(h + 1) * 64], v3(KKb, hs), v3(Eprev, hs), ALU.mult)
            P.tt(KKb[:], KKb[:], ALb[:], ALU.mult)
            P.ts(RNb[:], ALb[:], prm[:, 6:7], prm[:, 11:12], op0=ALU.mult, op1=ALU.add)
            P.tt(Kf[:], Kf[:], RNb[:], ALU.mult)
            for h in range(2):
                hs = slice(h * 64, (h + 1) * 64)
                e1 = "vector"
                e2 = "vector"
                P.tt(bkx[hs, :, 0, h, :], v3(KKb, hs), v3(Eneg, hs), ALU.mult, eng=e1)
                P.tt(bkx[hs, :, 1, h, :], v3(Kf, hs), v3(Eneg, hs), ALU.mult, eng=e2)
                P.tt(bhx[hs, :, h, :], v3(KKb, hs), v3(Eend, hs), ALU.mult, eng=e1)
                P.tt(khx[hs, :, h, :], v3(Kf, hs), v3(Eend, hs), ALU.mult, eng=e2)
            if need_y:
                P.tt(arx[:, :, 128:192], Rf[:].rearrange("p (c t) -> p c t", t=64),
                     Epos[:].rearrange("p (c t) -> p c t", t=64), ALU.mult)
                P.tt(rkb[:], Rf[:], Kf[:], ALU.mult)
            r4 = lambda t_: t_.rearrange("p (k n) -> p k n", n=128)
            for g in range(NCU // 4):
                XX = XXg[g % 2]
                Pm = Pmg[g % 2]
                xx = XX[0]
                for k in range(4):
                    c = g * 4 + k
                    bcols = bkx[:, c, 0, :, :].rearrange("p a b -> p (a b)")
                    acols = arx[:, c, 0:128]
                    P.mm(pbw[:, k * 128:(k + 1) * 128], bcols, acols)
                    P.mm(pbw[:, 512 + k * 128:512 + (k + 1) * 128], acols, bcols)
                P.tt(r4(xx[:, 0:512]), r4(pbw[:, 0:512]), mS.unsqueeze(1).to_broadcast([128, 4, 128]), ALU.mult)
                P.tt(r4(xx[:, 512:1024]), r4(pbw[:, 512:1024]), maskL[:].unsqueeze(1).to_broadcast([128, 4, 128]), ALU.mult)
                P.tt(r4(Pm[:]), L["identf"][:].unsqueeze(1).to_broadcast([128, 4, 128]), r4(xx[:, 0:512]), ALU.subtract)
                for lvl in range(6):
                    if lvl < 5:
                        for k in range(4):
                            ks = slice(k * 128, (k + 1) * 128)
                            kt = slice(512 + k * 128, 512 + (k + 1) * 128)
                            P.mm(pbw[:, ks], xx[:, kt], xx[:, ks])
                            P.mm(pbw[:, kt], xx[:, ks], xx[:, kt])
                    if lvl >= 1:
                        for k in range(4):
                            ks = slice(k * 128, (k + 1) * 128)
                            kt = slice(512 + k * 128, 512 + (k + 1) * 128)
                            P.mm(pb[1][:, ks], xx[:, kt], Pm[:, ks])
                    if lvl < 5:
                        xn = XX[(lvl + 1) % 2]
                        P.act(xn[:], pbw[:], AF.Copy)
                    if lvl >= 1:
                        if lvl == 5:
                            P.tt(pall[:, g * 4:(g + 1) * 4, :], r4(Pm[:]), r4(pb[1][:]), ALU.add)
                        else:
                            P.tt(Pm[:], Pm[:], pb[1][:], ALU.add)
                    if lvl < 5:
                        xx = xn

        def rec(p, q, ub):
            t0 = q * UL
            need_y = q >= 2
            prm = rwprm[:, p, :]
            cs = slice(p * 128, (p + 1) * 128)
            arx, bkx, bhx, khx, vx, vc, rkb, pall, wc = ARx[ub], BKx[ub], BHx[ub], KHx[ub], Vx[ub], Vc[ub], RKb[ub], Pall[ub], WC[ub]
            if q == 0:
                P.memset(Hst[:], 0.0)
                P.memset(Hbf[:], 0.0)
            if q == 2:
                P.ts(rksel[:], hmask[:], prm[:, 7:8], None, op0=ALU.mult)
                P.dma("sync", lgb[:, 0:128], L["lnx_g"][:, cs].partition_broadcast(64))
                P.dma("sync", lgb[:, 128:256], L["lnx_b"][:, cs].partition_broadcast(64))
            for c in range(NCU):
                par = c % 2
                sc = SCm[par]; tm = TM[par]; zs = Zs[par]; us = Us[par]
                ppar = (c // 2) % 2
                tmc = TMc[ppar]
                bcols = bkx[:, c, 0, :, :].rearrange("p a b -> p (a b)")
                kcols = bkx[:, c, 1, :, :].rearrange("p a b -> p (a b)")
                acols = arx[:, c, 0:128]
                rcols = arx[:, c, 128:192]
                nar = 192 if need_y else 128
                P.mm(pb[3][:, 0:nar], bcols, arx[:, c, 0:nar])
                P.mm(pb[3][:, 192:192 + nar], kcols, arx[:, c, 0:nar])
                s3 = lambda t_: t_[:, 0:384].rearrange("p (a n) -> p a n", n=192)[:, :, 0:nar]
                P.tt(s3(sc), s3(pb[3]), s3(maskS), ALU.mult)
                nabT, nrbT, nakT, nrkT = sc[:, 0:128], sc[:, 128:192], sc[:, 192:320], sc[:, 320:384]
                bT = pbb(2)
                P.tr(bT[:, 0:128], bhx[:, c, :, :].rearrange("p a b -> p (a b)"), identb[:])
                P.tr(bT[:, 128:256], khx[:, c, :, :].rearrange("p a b -> p (a b)"), identb[:])
                P.tr(bT[:, 256:384], vx[:, c, :, :].rearrange("p a b -> p (a b)"), identb[:])
                P.act(tm[:, 0:384], bT[:, 0:384], AF.Copy)
                if need_y:
                    P.tr(bT[0:64, 384:512], vc[:, c * 64:(c + 1) * 64], identb[:])
                    P.act(tmc[:, c % 2, :], bT[0:64, 384:512], AF.Copy)
                bht, kht, vt = tm[:, 0:128], tm[:, 128:256], tm[:, 256:384]
                P.mm(pb[4][:, 0:128], acols, Hbf[:], start=True, stop=False)
                P.mm(pb[4][:, 0:128], nakT, vt, start=False, stop=True)
                P.copy(zs[:], pb[4][:, 0:128])
                P.mm(pb[4][:, 128:256], pall[:, c, :], zs[:])
                P.ts(us[:], pb[4][:, 128:256], -1.0, None, op0=ALU.mult)
                if need_y:
                    yc = slice((c % 2) * 128, (c % 2 + 1) * 128)
                    P.mm(pb[5][0:64, yc], rcols, Hbf[:], start=True, stop=False)
                    P.mm(pb[5][0:64, yc], nrbT, us[:], start=False, stop=False)
                    P.mm(pb[5][0:64, yc], nrkT, vt, start=False, stop=True)
                P.mm(pb[4][:, 256:384], bht, us[:], start=True, stop=False)
                P.mm(pb[4][:, 256:384], kht, vt, start=False, stop=True)
                P.stt(Hbf[:], Hst[:], wc[:, c:c + 1], pb[4][:, 256:384], ALU.mult, ALU.add)
                P.stt(Hst[:], Hst[:], wc[:, c:c + 1], pb[4][:, 256:384], ALU.mult, ALU.add)
                if need_y and c % 2 == 1:
                    e = ep[ppar]; y = ey[ppar]; y2 = ey2[ppar]; o = eo[ppar]; gg = eg[ppar]
                    to0 = (q - 2) * UL + (c - 1) * 64
                    tu0 = (c - 1) * 64
                    P.copy(y[:], pb[5][0:64, 0:256])
                    for j in range(2):
                        for kc in range(2):
                            P.mm(pb[5][0:64, 256 + j * 128:256 + (j + 1) * 128], sgx[:, kc, to0 + j * 64:to0 + (j + 1) * 64],
                                 g2[:, kc, cs], start=(kc == 0), stop=(kc == 1))
                    P.act(gg[:], pb[5][0:64, 256:512], AF.Copy)
                    for j in range(2):
                        P.mm(pb[3][0:64, 384 + 2 * j:386 + 2 * j], rkb[:, tu0 + j * 64:tu0 + (j + 1) * 64], rksel[:])
                    P.act(e[:, 24:28], pb[3][0:64, 384:388], AF.Copy)
                    y4 = y[:].rearrange("p (a v) -> p a v", v=64)
                    P.reduce(e[:, 0:4], y4)
                    P.tt(y2[:], y[:], y[:], ALU.mult)
                    P.reduce(e[:, 4:8], y2[:].rearrange("p (a v) -> p a v", v=64))
                    P.ts(e[:, 8:12], e[:, 0:4], 1.0 / 64, None, op0=ALU.mult)
                    P.tt(e[:, 12:16], e[:, 8:12], e[:, 8:12], ALU.mult)
                    P.ts(e[:, 16:20], e[:, 4:8], 1.0 / 64, RW_LN_EPS, op0=ALU.mult, op1=ALU.add)
                    P.tt(e[:, 16:20], e[:, 16:20], e[:, 12:16], ALU.subtract)
                    P.act(e[:, 16:20], e[:, 16:20], AF.Ln)
                    P.act(e[:, 20:24], e[:, 16:20], AF.Exp, scale=-0.5)
                    P.tt(y4, y4, e[:, 8:12].unsqueeze(2).to_broadcast([64, 4, 64]), ALU.subtract)
                    P.tt(y4, y4, e[:, 20:24].unsqueeze(2).to_broadcast([64, 4, 64]), ALU.mult)
                    y3 = y[:].rearrange("p (j n) -> p j n", n=128)
                    P.tt(y3, y3, lgb[:, 0:128].unsqueeze(1).to_broadcast([64, 2, 128]), ALU.mult)
                    P.tt(y3, y3, lgb[:, 128:256].unsqueeze(1).to_broadcast([64, 2, 128]), ALU.add)
                    P.tt(y2[:].rearrange("p (a v) -> p a v", v=64), tmc[:].rearrange("p j (h v) -> p (j h) v", v=64),
                         e[:, 24:28].unsqueeze(2).to_broadcast([64, 4, 64]), ALU.mult)
                    P.tt(y[:], y[:], y2[:], ALU.add)
                    P.tt(o[:], y[:], gg[:], ALU.mult)
                    for j in range(2):
                        P.tr(bT[:, 512 + j * 64:512 + (j + 1) * 64], o[:, j * 128:(j + 1) * 128], identb[0:64, 0:64])
                    P.act(oaT_st[:, to0:to0 + 128], bT[:, 512:640], AF.Copy)
            if q == 3:
                P.dma("sync", L["oaT_d"][p], oaT_st[:])

        units = [(p, q) for p in range(8) for q in range(4)]
        for xi in range(3):
            wload(0, xi)
        prep(0, 0, 0)
        WIN = 8
        for u0 in range(0, len(units), WIN):
            items = []
            for u in range(u0, min(u0 + WIN, len(units))):
                p, q = units[u]
                items += P.record(lambda p=p, q=q, u=u: rec(p, q, u % 2))
                if u + 1 < len(units):
                    pn, qn = units[u + 1]
                    items += P.record(lambda pn=pn, qn=qn, u=u: prep(pn, qn, (u + 1) % 2))
            P.schedule(items)


def ret_phase(P, nc, L):
    pb = L["pb"]; pbb = L["pbb"]; hTs = L["hTs"]; identb = L["identb"]
    with P.scope():
        cst = P.sb([128, 16, 4, 64], F32, "cst")
        rmask = P.sb([128, 8, 128], F32, "rmask")
        rdec = P.sb([128, 16], F32, "rdec")
        P.dma("sync", cst[:], L["c_cs"])
        P.dma("sync", rmask[:], L["c_rmask"])
        P.dma("sync", rdec[:], L["c_rdec"])
        retg = P.sb([128, 256], F32, "retg")
        wr = [P.sb([128, 16, 768], BF16, f"wr{i}") for i in range(2)]
        S = P.sb([128, 256], F32, "S")
        Sbf = P.sb([128, 256], BF16, "Sbf")
        obT_st = P.sb([128, 2, 1024], BF16, "obT_st")
        A_ = [P.sb([128, 2, 128], F32, f"rA{i}") for i in range(2)]
        B_ = [P.sb([128, 2, 128], F32, f"rB{i}") for i in range(2)]
        rot = [P.sb([128, 2, 128], F32, f"rot{i}") for i in range(2)]
        qkb = [P.sb([128, 3, 128], BF16, f"qkb{i}") for i in range(2)]
        qkT = [P.sb([128, 2, 128], BF16, f"qkT{i}") for i in range(2)]
        vb = [P.sb([128, 256], BF16, f"vb{i}") for i in range(2)]
        scm = [P.sb([128, 128], BF16, f"scm{i}") for i in range(2)]
        sg = [P.sb([128, 256], F32, f"sg{i}") for i in range(2)]
        ob = [P.sb([128, 256], BF16, f"ob{i}") for i in range(2)]
        rs = [P.sb([128, 4], F32, f"rs{i}") for i in range(2)]
        junk = P.sb([128, 256], BF16, "rjunk")
        onec2 = P.sb([128, 2], F32, "onec2")
        P.memset(onec2[:], 1.0)
        lgam = [math.log1p(-2.0 ** (-5.0 - h)) for h in range(8)]
        P.dma("gpsimd", wr[0][:], L["w_ret"][0].rearrange("(c p) n -> p c n", p=128))
        def _head(h):
            wt = wr[h % 2]
            if h + 1 < 8:
                P.dma("gpsimd", wr[(h + 1) % 2][:], L["w_ret"][h + 1].rearrange("(c p) n -> p c n", p=128))
            P.dma("sync", retg[:], L["ret_g"][:, h * 256:(h + 1) * 256].partition_broadcast(128))
            P.memset(S[:], 0.0)
            P.memset(Sbf[:], 0.0)
            g128 = math.exp(128.0 * lgam[h])
            def proj(i):
                par = i % 2
                need_o = i >= 8
                t0 = i * 128
                pa = pb[par]
                lo = 0 if need_o else 128
                for c in range(16):
                    P.mm(pa[:, lo:512], hTs(c, t0, 128), wt[:, c, lo:512], start=(c == 0), stop=(c == 15))
                if need_o:
                    pg = pb[6] if par == 0 else pb[7]
                    for c in range(16):
                        P.mm(pg[:, 0:256], hTs(c, t0, 128), wt[:, c, 512:768], start=(c == 0), stop=(c == 15))

            proj(0)
            for i in range(16):
                par = i % 2
                need_o = i >= 8
                t0 = i * 128
                pa = pb[par]
                pg = pb[6] if par == 0 else pb[7]
                A = A_[par]; Bm = B_[par]; ro = rot[par]; qk = qkb[par]; qT = qkT[par]
                for xi in ([0, 1] if need_o else [1]):
                    xs = pa[:, xi * 128:(xi + 1) * 128].rearrange("p (a d) -> p a d", d=64)
                    ci = 0 if xi == 0 else 2
                    cosb = cst[:, i, ci, :].unsqueeze(1).to_broadcast([128, 2, 64])
                    sinb = cst[:, i, ci + 1, :].unsqueeze(1).to_broadcast([128, 2, 64])
                    Av = A[:, xi, :].rearrange("p (a d) -> p a d", d=64)
                    Bv = Bm[:, xi, :].rearrange("p (a d) -> p a d", d=64)
                    P.tt(Av, xs, cosb, ALU.mult)
                    P.tt(Bv, xs, sinb, ALU.mult)
                    P.tt(ro[:, xi, 0:64], A[:, xi, 0:64], Bm[:, xi, 64:128], ALU.subtract)
                    P.tt(ro[:, xi, 64:128], Bm[:, xi, 0:64], A[:, xi, 64:128], ALU.add)
                if need_o:
                    P.act(qk[:, 0, :], ro[:, 0, :], AF.Copy, scale=rdec[:, h:h + 1])
                    P.act(qk[:, 1, :], ro[:, 1, :], AF.Copy)
                P.act(qk[:, 2, :], ro[:, 1, :], AF.Copy, scale=rdec[:, 8 + h:9 + h])
                P.act(vb[par][:], pa[:, 256:512], AF.Copy)
                if i + 1 < 16:
                    proj(i + 1)
                if need_o:
                    bT = pbb(5)
                    P.tr(bT[:, 0:128], qk[:, 0, :], identb[:])
                    P.tr(bT[:, 128:256], qk[:, 1, :], identb[:])
                    P.copy(qT[:].rearrange("p a d -> p (a d)"), bT[:, 0:256])
                    P.mm(pb[2][:, 0:128], qT[:, 1, :], qT[:, 0, :])
                    P.tt(scm[par][:], pb[2][:, 0:128], rmask[:, h, :], ALU.mult)
                    P.mm(pb[3][:, 0:256], scm[par][:], vb[par][:], start=True, stop=False)
                    P.mm(pb[3][:, 0:256], qT[:, 0, :], Sbf[:], start=False, stop=True)
                P.mm(pb[4][:, 0:256], qk[:, 2, :], vb[par][:])
                P.stt(S[:], S[:], g128, pb[4][:, 0:256], ALU.mult, ALU.add)
                P.act(Sbf[:], S[:], AF.Copy)
                if need_o:
                    r = rs[par]
                    P.act(junk[:], pb[3][:, 0:256], AF.Square, accum_out=r[:, 0:1])
                    P.ts(r[:, 1:2], r[:, 0:1], 1.0 / 256, EPS, op0=ALU.mult, op1=ALU.add)
                    P.act(r[:, 2:3], r[:, 1:2], AF.Ln)
                    P.act(r[:, 3:4], r[:, 2:3], AF.Exp, scale=-0.5)
                    P.act(sg[par][:], pg[:, 0:256], AF.Exp, scale=-1.0)
                    P.act(sg[par][:], sg[par][:], AF.Ln, bias=onec2[:, 0:1])
                    P.act(sg[par][:], sg[par][:], AF.Exp, scale=-1.0)
                    P.tt(sg[par][:], sg[par][:], pg[:, 0:256], ALU.mult)
                    P.tt(sg[par][:], sg[par][:], retg[:], ALU.mult)
                    P.stt(ob[par][:], pb[3][:, 0:256], r[:, 3:4], sg[par][:], ALU.mult, ALU.mult)
                    bT2 = pbb(5)
                    for ec in range(2):
                        P.tr(bT2[:, 256 + ec * 128:256 + (ec + 1) * 128], ob[par][:, ec * 128:(ec + 1) * 128], identb[:])
                    P.act(obT_st[:, :, (i - 8) * 128:(i - 7) * 128], bT2[:, 256:512].rearrange("p (a d) -> p a d", d=128), AF.Copy)
            for ec in range(2):
                P.dma("sync", L["obT_d"][h * 2 + ec], obT_st[:, ec, :])


        def _allheads():
            for h in range(8):
                _head(h)
        P.region(_allheads)


def gate_phase(P, nc, L):
    pb = L["pb"]; hT_hi = L["hT_hi"]; w_in = L["w_in"]
    with P.scope():
        oaT = P.sb([128, 8, TO], BF16, "oaT")
        obT = P.sb([128, 16, TO], BF16, "obT")
        P.dma("sync", oaT[:], L["oaT_d"].rearrange("c p t -> p c t"))
        P.dma("sync", obT[:], L["obT_d"].rearrange("c p t -> p c t"))
        GA_ = [P.sb([128, 16, 512], BF16, f"GA{i}") for i in range(2)]
        GB_ = [P.sb([128, 16, 512], BF16, f"GB{i}") for i in range(2)]
        UA_ = [P.sb([128, 8, 512], BF16, f"UA{i}") for i in range(2)]
        UB_ = [P.sb([128, 16, 512], BF16, f"UB{i}") for i in range(2)]
        sA = [P.sb([128, 512], F32, f"sA{i}") for i in range(2)]
        sB = [P.sb([128, 512], F32, f"sB{i}") for i in range(2)]
        mT = [P.sb([128, TO], BF16, f"mT{i}") for i in range(2)]
        wv = w_in.rearrange("(c p) n -> p c n", p=128)
        def _quad(q):
            GA, GB, UA, UB = GA_[q % 2], GB_[q % 2], UA_[q % 2], UB_[q % 2]
            P.dma("gpsimd", GA[:], wv[:, :, GATE_OFF + q * 512:GATE_OFF + (q + 1) * 512])
            P.dma("gpsimd", GB[:], wv[:, :, GATE_OFF + 2048 + q * 512:GATE_OFF + 2048 + (q + 1) * 512])
            P.dma("gpsimd", UA[:], L["w_up_a"].rearrange("(c p) n -> p c n", p=128)[:, :, q * 512:(q + 1) * 512])
            P.dma("gpsimd", UB[:], L["w_up_b"].rearrange("(c p) n -> p c n", p=128)[:, :, q * 512:(q + 1) * 512])
            for jj in range(4):
                j = q * 4 + jj
                cols = slice(jj * 128, (jj + 1) * 128)
                mt = mT[j % 2]
                for blk in range(2):
                    par = blk
                    bs = slice(blk * 512, (blk + 1) * 512)
                    b0, b1, b2, b3 = pb[par * 4], pb[par * 4 + 1], pb[par * 4 + 2], pb[par * 4 + 3]
                    for c in range(16):
                        P.mm(b0[:], GA[:, c, cols], hT_hi[:, c, bs], start=(c == 0), stop=(c == 15))
                    for c in range(16):
                        P.mm(b1[:], GB[:, c, cols], hT_hi[:, c, bs], start=(c == 0), stop=(c == 15))
                    for c in range(8):
                        P.mm(b2[:], UA[:, c, cols], oaT[:, c, bs], start=(c == 0), stop=(c == 7))
                    for c in range(16):
                        P.mm(b3[:], UB[:, c, cols], obT[:, c, bs], start=(c == 0), stop=(c == 15))
                    P.act(sA[par][:], b0[:], AF.Sigmoid)
                    P.act(sB[par][:], b1[:], AF.Sigmoid)
                    P.tt(sA[par][:], sA[par][:], b2[:], ALU.mult)
                    P.tt(sB[par][:], sB[par][:], b3[:], ALU.mult)
                    P.tt(mt[:, bs], sA[par][:], sB[par][:], ALU.add)
                P.dma("sync", L["mT_d"][j], mt[:])


        def _allq():
            for q in range(4):
                _quad(q)
        P.region(_allq)


def tail_phase(P, nc, L):
    pb = L["pb"]; pbb = L["pbb"]; identb = L["identb"]; identf = L["identf"]
    x = L["x"]; out = L["out"]
    with P.scope():
        x2 = P.sb([128, 8, DM], F32, "x2")
        with P.scope():
            mg = P.sb([128, 16, TO], BF16, "mg")
            P.dma("sync", mg[:], L["mT_d"].rearrange("c p t -> p c t"))
            Wo = [P.sb([128, 16, 512], BF16, f"Wo{i}") for i in range(2)]
            xin = [P.sb([128, 512], F32, f"xin3_{i}") for i in range(2)]
            wov = L["w_out"].rearrange("(c p) n -> p c n", p=128)
            P.dma("gpsimd", Wo[0][:], wov[:, :, 0:512])
            def _wout():
                k = 0
                for nb in range(4):
                    if nb + 1 < 4:
                        P.dma("gpsimd", Wo[(nb + 1) % 2][:], wov[:, :, (nb + 1) * 512:(nb + 2) * 512])
                    for i in range(8):
                        bk = pb[k % 4]
                        xi = xin[k % 2]
                        k += 1
                        P.dma("sync", xi[:], x[1024 + i * 128:1024 + (i + 1) * 128, nb * 512:(nb + 1) * 512])
                        for c in range(16):
                            P.mm(bk[:], mg[:, c, i * 128:(i + 1) * 128], Wo[nb % 2][:, c, :], start=(c == 0), stop=(c == 15))
                        P.tt(x2[:, i, nb * 512:(nb + 1) * 512], bk[:], xi[:], ALU.add)

            P.region(_wout)

        with P.scope():
            h2 = P.sb([128, 8, DM], BF16, "h2")
            asg = P.sb([128, 8, 32], F32, "asg")
            asgb = P.sb([128, 8, 32], BF16, "asgb")
            wmat = P.sb([128, 8, 32], F32, "wmat")
            pos = P.sb([128, 8, 32], F32, "pos")
            iota = P.sb([128, 128], F32, "iota")
            tri = P.sb([128, 128], BF16, "tri")
            onesb = P.sb([128, 128], BF16, "onesb")
            P.dma("sync", iota[:], L["c_iota"])
            P.dma("sync", tri[:], L["c_tri"])
            P.memset(onesb[:], 1.0)
            Wg = [P.sb([128, 16, EFF], BF16, "Wg0")]
            Wu = [P.sb([128, 16, EFF], BF16, "Wu0")]
            P.dma("gpsimd", Wg[0][:], L["e_gate"][0].rearrange("(c p) f -> p c f", p=128))
            P.dma("gpsimd", Wu[0][:], L["e_up"][0].rearrange("(c p) f -> p c f", p=128))
            with P.scope():
                g2b = P.sb([128, DM], F32, "g2b")
                P.dma("sync", g2b[:], L["g2_d"].partition_broadcast(128))
                wrt = P.sb([128, 16, 36], F32, "wrt")
                P.dma("sync", wrt[:], L["w_rt"].rearrange("(c p) n -> p c n", p=128))
                brt = P.sb([128, 36], F32, "brt")
                P.dma("sync", brt[:], L["b_rt"].partition_broadcast(128))
                h2f = [P.sb([128, DM], F32, f"h2f{i}") for i in range(2)]
                h2T = [P.sb([128, 16, 128], F32, f"h2T{i}") for i in range(2)]
                junk = P.sb([128, DM], BF16, "junk4")
                rr = [P.sb([128, 24], F32, f"rr{i}") for i in range(2)]
                lgt = [P.sb([128, 36], F32, f"lgt{i}") for i in range(2)]
                em = [P.sb([128, 3, 32], F32, f"em{i}") for i in range(2)]
                def _router():
                    for i in range(8):
                        par = i % 2
                        r = rr[par]; hf = h2f[par]; hT_ = h2T[par]; lg = lgt[par]; e_ = em[par]
                        P.act(junk[:], x2[:, i, :], AF.Square, accum_out=r[:, 0:1])
                        P.ts(r[:, 1:2], r[:, 0:1], 1.0 / DM, EPS, op0=ALU.mult, op1=ALU.add)
                        P.act(r[:, 2:3], r[:, 1:2], AF.Ln)
                        P.act(r[:, 3:4], r[:, 2:3], AF.Exp, scale=-0.5)
                        P.stt(hf[:], x2[:, i, :], r[:, 3:4], g2b[:], ALU.mult, ALU.mult)
                        P.act(h2[:, i, :], hf[:], AF.Copy)
                        for c4 in range(4):
                            bk = pb[c4 % 2]
                            for kk in range(4):
                                c = c4 * 4 + kk
                                P.tr(bk[:, kk * 128:(kk + 1) * 128], hf[:, c * 128:(c + 1) * 128], identf[:])
                            P.act(hT_[:, c4 * 4:(c4 + 1) * 4, :], bk[:].rearrange("p (a d) -> p a d", d=128), AF.Copy)
                        for c in range(16):
                            P.mm(pb[2][:, 0:36], hT_[:, c, :], wrt[:, c, :], start=(c == 0), stop=(c == 15))
                        P.tt(lg[:], pb[2][:, 0:36], brt[:], ALU.add)
                        P.reduce(r[:, 4:5], lg[:, 0:4], op=ALU.max)
                        P.ts(r[:, 8:12], lg[:, 0:4], r[:, 4:5], None, op0=ALU.is_equal)
                        P.ts(r[:, 5:6], r[:, 4:5], -1.0, None, op0=ALU.mult)
                        P.act(r[:, 12:16], lg[:, 0:4], AF.Exp, bias=r[:, 5:6], accum_out=r[:, 6:7])
                        P.recip(r[:, 7:8], r[:, 6:7])
                        P.ts(r[:, 16:20], r[:, 8:12], 1e30, -1e30, op0=ALU.mult, op1=ALU.add)
                        ev = e_[:, 0, :].rearrange("p (g k) -> p g k", k=8)
                        P.tt(ev, lg[:, 4:36].rearrange("p (g k) -> p g k", k=8),
                             r[:, 16:20].unsqueeze(2).to_broadcast([128, 4, 8]), ALU.add)
                        P.reduce(r[:, 20:21], e_[:, 0, :], op=ALU.max)
                        P.ts(e_[:, 1, :], e_[:, 0, :], r[:, 20:21], None, op0=ALU.is_equal)
                        P.stt(e_[:, 0, :], e_[:, 1, :], -1e30, e_[:, 0, :], ALU.mult, ALU.add)
                        P.reduce(r[:, 21:22], e_[:, 0, :], op=ALU.max)
                        P.ts(e_[:, 2, :], e_[:, 0, :], r[:, 21:22], None, op0=ALU.is_equal)
                        P.tt(r[:, 22:23], r[:, 20:21], r[:, 21:22], ALU.subtract)
                        P.act(r[:, 22:23], r[:, 22:23], AF.Exp, scale=-1.0)
                        P.ts(r[:, 22:23], r[:, 22:23], 1.0, None, op0=ALU.add)
                        P.recip(r[:, 22:23], r[:, 22:23])
                        P.tt(r[:, 22:23], r[:, 22:23], r[:, 7:8], ALU.mult)
                        P.tt(r[:, 23:24], r[:, 7:8], r[:, 22:23], ALU.subtract)
                        P.tt(asg[:, i, :], e_[:, 1, :], e_[:, 2, :], ALU.add)
                        P.copy(asgb[:, i, :], asg[:, i, :])
                        P.ts(wmat[:, i, :], e_[:, 1, :], r[:, 22:23], None, op0=ALU.mult)
                        P.stt(wmat[:, i, :], e_[:, 2, :], r[:, 23:24], wmat[:, i, :], ALU.mult, ALU.add)
                    for i in range(8):
                        for j in range(i):
                            P.mm(pb[3][:, 0:32], onesb[:], asgb[:, j, :], start=(j == 0), stop=False)
                        P.mm(pb[3][:, 0:32], tri[:], asgb[:, i, :], start=(i == 0), stop=True)
                        P.copy(pos[:, i, :], pb[3][:, 0:32])

                P.region(_router)

            Wg.append(P.sb([128, 16, EFF], BF16, "Wg1"))
            Wu.append(P.sb([128, 16, EFF], BF16, "Wu1"))
            Wd = [P.sb([128, 4, DM], BF16, f"Wd{i}") for i in range(1)]
            sel = [P.sb([128, 8, 128], BF16, f"sel{i}") for i in range(2)]
            selw = [P.sb([128, 8, 128], BF16, f"selw{i}") for i in range(2)]
            selwT = P.sb([128, 8, 128], BF16, "selwT")
            XeT = P.sb([128, 16, 128], BF16, "XeT")
            sgt = P.sb([128, 512], F32, "sgt")
            hidT = [P.sb([128, 4, 128], BF16, f"hidT{i}") for i in range(2)]
            ye = P.sb([128, DM], BF16, "ye")

            def load_e(e):
                P.dma("gpsimd", Wg[e % 2][:], L["e_gate"][e].rearrange("(c p) f -> p c f", p=128))
                P.dma("gpsimd", Wu[e % 2][:], L["e_up"][e].rearrange("(c p) f -> p c f", p=128))

            def stageA(e):
                par = e % 2
                if e + 1 < NEXP:
                    load_e(e + 1)
                sl = sel[par]; sw = selw[par]
                for i in range(8):
                    P.ts(sl[:, i, :], iota[:], pos[:, i, e:e + 1], asg[:, i, e:e + 1], op0=ALU.is_equal, op1=ALU.mult)
                    P.ts(sw[:, i, :], iota[:], pos[:, i, e:e + 1], wmat[:, i, e:e + 1], op0=ALU.is_equal, op1=ALU.mult)
                for c4 in range(4):
                    bk = pb[c4 % 2]
                    for kk in range(4):
                        c = c4 * 4 + kk
                        for i in range(8):
                            P.mm(bk[:, kk * 128:(kk + 1) * 128], h2[:, i, c * 128:(c + 1) * 128], sl[:, i, :],
                                 start=(i == 0), stop=(i == 7))
                    P.act(XeT[:, c4 * 4:(c4 + 1) * 4, :], bk[:].rearrange("p (a d) -> p a d", d=128), AF.Copy)
                for fc in range(4):
                    for c in range(16):
                        P.mm(pb[6][:, fc * 128:(fc + 1) * 128], Wg[par][:, c, fc * 128:(fc + 1) * 128], XeT[:, c, :],
                             start=(c == 0), stop=(c == 15))
                for fc in range(4):
                    for c in range(16):
                        P.mm(pb[7][:, fc * 128:(fc + 1) * 128], Wu[par][:, c, fc * 128:(fc + 1) * 128], XeT[:, c, :],
                             start=(c == 0), stop=(c == 15))
                P.act(sgt[:], pb[6][:], AF.Silu)
                P.tt(hidT[par][:].rearrange("p a d -> p (a d)"), sgt[:], pb[7][:], ALU.mult)

            def stageB(e):
                par = e % 2
                sw = selw[par]
                P.dma("gpsimd", Wd[0][:], L["e_down"][e].rearrange("(c p) n -> p c n", p=128))
                bT = pbb(5)
                for i in range(8):
                    P.tr(bT[:, i * 128:(i + 1) * 128], sw[:, i, :], identb[:])
                P.act(selwT[:].rearrange("p a d -> p (a d)"), bT[:, 0:1024], AF.Copy)
                for nb in range(4):
                    bk = pb[2 + nb % 2]
                    for fc in range(4):
                        P.mm(bk[:], hidT[par][:, fc, :], Wd[0][:, fc, nb * 512:(nb + 1) * 512], start=(fc == 0), stop=(fc == 3))
                    P.act(ye[:, nb * 512:(nb + 1) * 512], bk[:], AF.Copy)
                k = 0
                for i in range(8):
                    for nb in range(4):
                        bk = pb[2 + (k % 3)]
                        k += 1
                        P.mm(bk[:], selwT[:, i, :], ye[:, nb * 512:(nb + 1) * 512])
                        P.tt(x2[:, i, nb * 512:(nb + 1) * 512], x2[:, i, nb * 512:(nb + 1) * 512], bk[:], ALU.add)

            P.region(lambda: stageA(0))
            EW = 8
            for e0 in range(0, NEXP, EW):
                items = []
                for e in range(e0, min(e0 + EW, NEXP)):
                    items += P.record(lambda e=e: stageB(e))
                    if e + 1 < NEXP:
                        items += P.record(lambda e=e: stageA(e + 1))
                P.schedule(items)

        with P.scope():
            gfb = P.sb([128, DM], F32, "gfb")
            P.dma("sync", gfb[:], L["gf_d"].partition_broadcast(128))
            ot = [P.sb([128, DM], F32, f"ot{i}") for i in range(2)]
            junk = P.sb([128, DM], BF16, "junk5")
            rr = [P.sb([128, 4], F32, f"rf{i}") for i in range(2)]
            def _final():
                for i in range(8):
                    r = rr[i % 2]
                    P.act(junk[:], x2[:, i, :], AF.Square, accum_out=r[:, 0:1])
                    P.ts(r[:, 1:2], r[:, 0:1], 1.0 / DM, EPS, op0=ALU.mult, op1=ALU.add)
                    P.act(r[:, 2:3], r[:, 1:2], AF.Sqrt)
                    P.recip(r[:, 3:4], r[:, 2:3])
                    P.stt(ot[i % 2][:], x2[:, i, :], r[:, 3:4], gfb[:], ALU.mult, ALU.mult)
                    P.dma("sync", out[i * 128:(i + 1) * 128, :], ot[i % 2][:])

            P.region(_final)


def _constants(core_half):
    bf = ml_dtypes.bfloat16
    c = {}
    c["c_identb"] = np.eye(128, dtype=np.float32).astype(bf)
    c["c_identf"] = np.eye(128, dtype=np.float32)
    hp = np.arange(128) // 64
    sp = np.arange(128) % 64
    same = (hp[:, None] == hp[None, :])
    strict_bd = (same & (sp[:, None] < sp[None, :])).astype(np.float32)
    incl_c = (sp[:, None] <= np.arange(64)[None, :]).astype(np.float32)
    c["c_maskS"] = np.concatenate([strict_bd, incl_c, strict_bd, incl_c], axis=1)
    c["c_maskL"] = (same & (sp[None, :] < sp[:, None])).astype(np.float32)
    c["c_bones"] = same.astype(np.float32).astype(bf)
    c["c_hmask"] = np.stack([(hp == 0), (hp == 1)], axis=1).astype(np.float32)
    rs = np.ones((128, 1024), np.float32)
    rs[:, ::64] = 0.0
    c["c_reset"] = rs.astype(bf)
    pos = np.arange(T, dtype=np.float32) - (1024.0 if core_half == 0 else 0.0)
    inv = (10000.0 ** (-np.arange(64, dtype=np.float32) / 64)).astype(np.float32)
    ang = pos[:, None] * inv[None, :]
    cos, sin = np.cos(ang).astype(np.float32), np.sin(ang).astype(np.float32)
    sc = np.float32(128 ** -0.5)
    cs = np.stack([cos, sin, cos * sc, sin * sc], axis=1)
    c["c_cs"] = np.ascontiguousarray(cs.reshape(16, 128, 4, 64).transpose(1, 0, 2, 3))
    lg = np.log1p(-np.exp2(-5.0 - np.arange(8, dtype=np.float64)))
    idx = np.arange(128, dtype=np.float64)
    j = idx[:, None]
    i = idx[None, :]
    same_c = (j // 64 == i // 64)
    rm = np.zeros((128, 8, 128), np.float64)
    for h in range(8):
        m = np.where(same_c, np.exp(np.abs(i - j) * lg[h]), np.where(j < i, np.exp((i - j) * lg[h]), 0.0))
        rm[:, h, :] = m * np.exp(-(i + 1) * lg[h])
    c["c_rmask"] = rm.astype(np.float32)
    rd = np.zeros((128, 16), np.float64)
    for h in range(8):
        rd[:, h] = np.exp((idx + 1) * lg[h])
        rd[:, 8 + h] = np.exp((127 - idx) * lg[h])
    c["c_rdec"] = rd.astype(np.float32)
    c["c_tri"] = (idx[:, None] < idx[None, :]).astype(np.float32).astype(bf)
    c["c_iota"] = np.tile(np.arange(128, dtype=np.float32)[None, :], (128, 1))
    return c


def _prep_shared(inp):
    f = np.float32
    w_in = np.asarray(inp["w_in"][0], f)
    d = {}
    d["g1T"] = np.ascontiguousarray(np.asarray(inp["norm1_g"][0], f).reshape(16, 128).T)
    d["w_lora"] = np.ascontiguousarray(w_in[:, 3072:3520])
    d["w_rw"] = np.ascontiguousarray(np.stack(
        [np.concatenate([w_in[:, k * 1024 + p * 128:k * 1024 + (p + 1) * 128] for k in range(3)], axis=1) for p in range(8)]))
    ro = RET_OFF
    d["w_ret"] = np.ascontiguousarray(np.stack(
        [np.concatenate([w_in[:, ro + h * 128:ro + (h + 1) * 128],
                         w_in[:, ro + 1024 + h * 128:ro + 1024 + (h + 1) * 128],
                         w_in[:, ro + 2048 + h * 256:ro + 2048 + (h + 1) * 256],
                         w_in[:, ro + 4096 + h * 256:ro + 4096 + (h + 1) * 256]], axis=1) for h in range(8)]))
    d["w_in"] = w_in
    mu = np.asarray(inp["mu_shift"][0], f)
    lp = np.zeros((128, 4), f)
    lp[:96, 0] = mu[3072:3168]
    lp[:96, 1] = mu[3168:3264]
    lp[:, 2] = mu[3264:3392]
    lp[:, 3] = mu[3392:3520]
    d["lprm"] = lp
    vecs = [mu[0:1024], mu[1024:2048], mu[2048:3072], np.asarray(inp["rw_w0"][0], f), np.asarray(inp["rw_a0"][0], f),
            np.asarray(inp["rw_k_k"][0], f), np.asarray(inp["rw_k_a"][0], f), np.asarray(inp["rw_r_k"][0], f)]
    d["rwprm"] = np.ascontiguousarray(np.stack([v.reshape(8, 128).T for v in vecs], axis=2))
    d["rw_w2"] = np.asarray(inp["rw_w2"][0], f)
    d["rw_a2"] = np.asarray(inp["rw_a2"][0], f)
    d["rw_g2"] = np.asarray(inp["rw_g2"][0], f)
    d["lnx_g"] = np.asarray(inp["rw_lnx_g"][0], f).reshape(1, 1024)
    d["lnx_b"] = np.asarray(inp["rw_lnx_b"][0], f).reshape(1, 1024)
    d["ret_g"] = np.asarray(inp["ret_norm_g"][0], f).reshape(1, 2048)
    d["w_up_a"] = np.asarray(inp["w_up_a"][0], f)
    d["w_up_b"] = np.asarray(inp["w_up_b"][0], f)
    d["w_out"] = np.asarray(inp["w_out"][0], f)
    d["g2"] = np.asarray(inp["norm2_g"][0], f).reshape(1, DM)
    d["gf"] = np.asarray(inp["final_norm_g"], f).reshape(1, DM)
    d["w_rt"] = np.ascontiguousarray(np.concatenate([np.asarray(inp["w_group"][0], f), np.asarray(inp["w_expert"][0], f)], axis=1))
    d["b_rt"] = np.concatenate([np.asarray(inp["b_group"][0], f), np.asarray(inp["b_expert"][0], f)]).reshape(1, 36)
    d["e_gate"] = np.asarray(inp["e_gate"][0], f)
    d["e_up"] = np.asarray(inp["e_up"][0], f)
    d["e_down"] = np.asarray(inp["e_down"][0], f)
    return d


_CACHE = {}


def run(inp, stop_after=99, cores=None, trace=False):
    key = stop_after
    if key not in _CACHE:
        _CACHE[key] = build_program(stop_after)
    nc, declared = _CACHE[key]
    shared = _prep_shared(inp)
    consts = [_constants(0), _constants(1)]
    xfull = np.asarray(inp["x"], np.float32)
    cores = list(range(8)) if cores is None else cores
    in_maps = []
    for c in cores:
        b, half = c // 2, c % 2
        if half == 0:
            xs = np.concatenate([np.zeros((1024, DM), np.float32), xfull[b, :1024]], axis=0)
        else:
            xs = xfull[b]
        m = {"x": np.ascontiguousarray(xs)}
        m.update(shared)
        m.update(consts[half])
        in_maps.append({k: m[k] for k in declared})
    res = run_bass_kernel_spmd(nc, in_maps, core_ids=list(range(len(cores))), trace=trace)
    return res


def kernel(**inputs):
    res = run(inputs)
    outp = np.zeros((4, 2048, DM), np.float32)
    for c in range(8):
        b, half = c // 2, c % 2
        outp[b, half * 1024:(half + 1) * 1024] = res.results[c]["out"]
    return outp
```

```python
from contextlib import ExitStack, contextmanager
import math
import numpy as np
import ml_dtypes
import concourse.bass as bass
import concourse.mybir as mybir
from concourse.bass_utils import run_bass_kernel_spmd

F32 = mybir.dt.float32
BF16 = mybir.dt.bfloat16
AF = mybir.ActivationFunctionType
ALU = mybir.AluOpType
AX = mybir.AxisListType

ENGS = ("tensor", "vector", "scalar", "gpsimd", "sync")
EPOCH = 30000
NDMA = 8

DM = 2048
T = 2048
TO = 1024
RW_COLS = 3520
RET_OFF = 3520
GATE_OFF = 3520 + 6144
IN_COLS = 13760
EPS = 1e-6
RW_LN_EPS = 64e-5
CDEC = -math.exp(-0.5)
NEXP = 32
EFF = 512


class Prog:
    def __init__(self, nc):
        self.nc = nc
        self.es = ExitStack()
        self.stack = [self.es]
        self.q = {e: [] for e in ENGS}
        self.cnt = {e: 0 for e in ENGS}
        self.esem = {}
        self.nsem = 0
        for e in ENGS:
            self.esem[e] = self._newsem(f"e_{e}")
        self.dsem = {e: [self._newsem(f"d_{e}{i}") for i in range(NDMA)] for e in ("sync", "scalar", "gpsimd")}
        self.dcount = {e: [0] * NDMA for e in self.dsem}
        self.dnext = {e: 0 for e in self.dsem}
        self.lastw = {}
        self.reads = {}
        self.known = {e: {} for e in ENGS}
        self.ntensors = 0
        self.rec = None

    def record(self, fn):
        assert self.rec is None
        self.rec = []
        try:
            fn()
        finally:
            lst, self.rec = self.rec, None
        return lst

    def replay(self, item):
        kind, eng, a, reads, writes, kw = item
        if kind == "op":
            self.op(eng, a, reads, writes)
        else:
            self.dma(eng, a[0], a[1], reads=reads, writes=writes, **kw)

    @staticmethod
    def _fsize(x):
        sh = getattr(x, "shape", None)
        if sh is None:
            return 64
        n = 1
        for d in sh[1:]:
            n *= d
        return n

    def _cost(self, eng, reads, writes):
        if eng == "tensor":
            n = self._fsize(reads[1]) if len(reads) > 1 else 128
            return 70.0 + n / 2.0
        n = max([self._fsize(w) for w in writes] + [1])
        if eng == "vector":
            return 120.0 + n * 0.9
        if eng == "scalar":
            return 220.0 + n * 0.8
        if eng == "gpsimd":
            return 200.0 + n * 1.5
        return 100.0

    def schedule(self, items, lat=20.0):
        n = len(items)
        if n == 0:
            return
        lastw, readers = {}, {}
        preds = [set() for _ in range(n)]
        succs = [[] for _ in range(n)]
        dur = [0.0] * n
        engs = [None] * n
        for i, it in enumerate(items):
            kind, eng, a, reads, writes, extra = it
            engs[i] = eng
            dur[i] = extra if (kind == "op" and extra is not None) else 100.0
            rk = [k for r in reads for k in self._keys(r)]
            wk = [k for w in writes for k in self._keys(w)]
            for k in rk:
                if k in lastw:
                    preds[i].add(lastw[k])
                if k.startswith("bank"):
                    for j in readers.get(k, ()):
                        if engs[j] != eng:
                            preds[i].add(j)
            for k in wk:
                if k in lastw:
                    preds[i].add(lastw[k])
                for j in readers.get(k, ()):
                    preds[i].add(j)
            for k in rk:
                readers.setdefault(k, []).append(i)
            for k in wk:
                lastw[k] = i
                readers[k] = []
            preds[i].discard(i)
            for j in preds[i]:
                succs[j].append(i)
        dlat = [2000.0 if items[i][0] == "dma" else dur[i] for i in range(n)]
        prio = [0.0] * n
        for i in range(n - 1, -1, -1):
            m = 0.0
            for j in succs[i]:
                if prio[j] > m:
                    m = prio[j]
            prio[i] = dlat[i] + lat + m
        npred = [len(p) for p in preds]
        ready = [i for i in range(n) if npred[i] == 0]
        fin = [0.0] * n
        efree = {}
        order = []
        rdy_t = [0.0] * n
        while ready:
            best = None
            bkey = None
            for i in ready:
                st = max(efree.get(engs[i], 0.0), rdy_t[i])
                key = (st, -prio[i], i)
                if bkey is None or key < bkey:
                    bkey, best = key, i
            ready.remove(best)
            st = bkey[0]
            issue = dur[best] if items[best][0] == "op" else 60.0
            efree[engs[best]] = st + issue
            fin[best] = st + dlat[best]
            order.append(best)
            for j in succs[best]:
                t = fin[best] + (lat if engs[j] != engs[best] or engs[j] != "tensor" else 0.0)
                if t > rdy_t[j]:
                    rdy_t[j] = t
                npred[j] -= 1
                if npred[j] == 0:
                    ready.append(j)
        assert len(order) == n
        for i in order:
            self.replay(items[i])

    def merge(self, A, B):
        a, b = len(A), len(B)
        if a == 0:
            for it in B:
                self.replay(it)
            return
        done = 0
        for i, it in enumerate(A):
            self.replay(it)
            upto = ((i + 1) * b) // a
            while done < upto:
                self.replay(B[done])
                done += 1
        while done < b:
            self.replay(B[done])
            done += 1

    def region(self, fn):
        self.schedule(self.record(fn))

    def _newsem(self, name):
        self.nsem += 1
        return self.es.enter_context(self.nc.semaphore(f"{name}_{self.nsem}"))

    def sb(self, shape, dt=F32, name=None):
        self.ntensors += 1
        return self.stack[-1].enter_context(self.nc.sbuf_tensor(f"{name or 'sb'}_{self.ntensors}", list(shape), dt))

    def ps(self, shape, dt=F32, name=None):
        self.ntensors += 1
        return self.stack[-1].enter_context(self.nc.psum_tensor(f"{name or 'ps'}_{self.ntensors}", list(shape), dt))

    @contextmanager
    def scope(self):
        st = ExitStack()
        self.stack.append(st)
        try:
            yield
        finally:
            self.barrier()
            self.stack.pop()
            st.close()

    @staticmethod
    def _keys(x):
        if isinstance(x, (str, tuple)):
            return [x]
        t = getattr(x, "tensor", None)
        if t is None:
            return [x.name]
        name = t.name
        if name.startswith("bankw"):
            apl = x.ap
            rs = apl[0][0]
            col0 = x.offset % rs
            ext = 1 + sum((n - 1) * st for st, n in apl[1:])
            half = rs // 2
            ks = []
            if col0 < half:
                ks.append(name + "_lo")
            if col0 + ext > half:
                ks.append(name + "_hi")
            return ks
        return [name]

    def _need(self, eng, ev, waits):
        if ev is None:
            return
        _, sem, val = ev
        k = self.known[eng]
        if k.get(sem, 0) >= val:
            return
        k[sem] = val
        waits[sem] = max(waits.get(sem, 0), val)

    def _deps(self, eng, reads, writes):
        waits = {}
        for r in reads:
            for k in self._keys(r):
                ev = self.lastw.get(k)
                if ev is not None and not (ev[0] == eng and eng == "tensor"):
                    self._need(eng, ev, waits)
                if k.startswith("bank"):
                    for rv in self.reads.get(k, ()):
                        if rv[0] != eng:
                            self._need(eng, rv, waits)
        for w in writes:
            for k in self._keys(w):
                ev = self.lastw.get(k)
                if ev is not None and ev[0] != eng:
                    self._need(eng, ev, waits)
                for rv in self.reads.get(k, ()):
                    if rv[0] != eng:
                        self._need(eng, rv, waits)
        return waits

    def _commit(self, ev, reads, writes):
        for r in reads:
            for k in self._keys(r):
                self.reads.setdefault(k, []).append(ev)
        for w in writes:
            for k in self._keys(w):
                self.lastw[k] = ev
                self.reads[k] = []

    def op(self, eng, fn, reads=(), writes=(), cost=None):
        if self.rec is not None:
            if cost is None:
                cost = self._cost(eng, reads, writes)
            self.rec.append(("op", eng, fn, list(reads), list(writes), cost))
            return None
        waits = self._deps(eng, reads, writes)
        if self.cnt[eng] >= EPOCH:
            self.esem[eng] = self._newsem(f"e_{eng}")
            self.cnt[eng] = 0
        self.cnt[eng] += 1
        sem = self.esem[eng]
        val = self.cnt[eng]
        wl = list(waits.items())

        def run(e, fn=fn, wl=wl, sem=sem):
            for s, v in wl:
                e.wait_ge(s, v)
            fn(e).then_inc(sem, 1)
        self.q[eng].append(run)
        ev = (eng, sem, val)
        self._commit(ev, reads, writes)
        return ev

    def dma(self, eng, out, in_, reads=None, writes=None, **kw):
        reads = [in_] if reads is None else reads
        writes = [out] if writes is None else writes
        if self.rec is not None:
            self.rec.append(("dma", eng, (out, in_), list(reads), list(writes), kw))
            return None
        waits = self._deps(eng, reads, writes)
        i = self.dnext[eng]
        self.dnext[eng] = (i + 1) % NDMA
        sem = self.dsem[eng][i]
        prev = self.dcount[eng][i]
        if prev > 0 and self.known[eng].get(sem, 0) < 16 * prev:
            waits[sem] = 16 * prev
            self.known[eng][sem] = 16 * prev
        self.dcount[eng][i] = prev + 1
        val = 16 * (prev + 1)
        wl = list(waits.items())

        def run(e, wl=wl, sem=sem, out=out, in_=in_, kw=kw):
            for s, v in wl:
                e.wait_ge(s, v)
            e.dma_start(out=out, in_=in_, **kw).then_inc(sem, 16)
        self.q[eng].append(run)
        ev = ("dma_" + eng, sem, val)
        self._commit(ev, reads, writes)
        return ev

    def barrier(self):
        evs = []
        for e in ENGS:
            if self.cnt[e] > 0:
                evs.append((e, self.esem[e], self.cnt[e]))
        for e in self.dsem:
            for i in range(NDMA):
                if self.dcount[e][i] > 0:
                    evs.append(("dma_" + e, self.dsem[e][i], 16 * self.dcount[e][i]))
        for eng in ENGS:
            waits = {}
            for ev in evs:
                if ev[0] != eng:
                    self._need(eng, ev, waits)
            wl = list(waits.items())
            if wl:
                def run(e, wl=wl):
                    for s, v in wl:
                        e.wait_ge(s, v)
                self.q[eng].append(run)

    def emit(self):
        nc = self.nc
        with nc.Block() as block:
            @block.tensor
            def _(e):
                for f in self.q["tensor"]:
                    f(e)

            @block.vector
            def _(e):
                for f in self.q["vector"]:
                    f(e)

            @block.scalar
            def _(e):
                for f in self.q["scalar"]:
                    f(e)

            @block.gpsimd
            def _(e):
                for f in self.q["gpsimd"]:
                    f(e)

            @block.sync
            def _(e):
                for f in self.q["sync"]:
                    f(e)
        self.es.close()

    def mm(self, out, lhsT, rhs, start=True, stop=True):
        return self.op("tensor", lambda e: e.matmul(out, lhsT, rhs, start=start, stop=stop),
                       reads=[lhsT, rhs], writes=[out])

    def tr(self, out, in_, ident):
        return self.op("tensor", lambda e: e.transpose(out, in_, ident), reads=[in_, ident], writes=[out])

    def act(self, out, in_, func, bias=None, scale=None, accum_out=None):
        kw = {}
        reads = [in_]
        writes = [out]
        if bias is not None:
            kw["bias"] = bias
            if not isinstance(bias, (int, float)):
                reads.append(bias)
        if scale is not None:
            kw["scale"] = scale
            if not isinstance(scale, (int, float)):
                reads.append(scale)
        if accum_out is not None:
            kw["accum_out"] = accum_out
            writes.append(accum_out)
        return self.op("scalar", lambda e: e.activation(out, in_, func, **kw), reads=reads, writes=writes)

    def tt(self, out, in0, in1, op, eng="vector"):
        return self.op(eng, lambda e: e.tensor_tensor(out, in0, in1, op), reads=[in0, in1], writes=[out])

    def ts(self, out, in0, s1, s2=None, op0=ALU.mult, op1=None, eng="vector"):
        reads = [in0] + [s for s in (s1, s2) if s is not None and not isinstance(s, (int, float))]
        kw = {}
        if op1 is not None:
            kw["op1"] = op1
        return self.op(eng, lambda e: e.tensor_scalar(out, in0, s1, s2, op0, **kw), reads=reads, writes=[out])

    def stt(self, out, in0, scalar, in1, op0, op1):
        reads = [in0, in1] + ([scalar] if not isinstance(scalar, (int, float)) else [])
        return self.op("vector", lambda e: e.scalar_tensor_tensor(out, in0, scalar, in1, op0, op1),
                       reads=reads, writes=[out])

    def copy(self, out, in_, eng="vector"):
        if eng == "scalar":
            return self.op(eng, lambda e: e.copy(out, in_), reads=[in_], writes=[out])
        return self.op(eng, lambda e: e.tensor_copy(out, in_), reads=[in_], writes=[out])

    def memset(self, ap, val, eng="vector"):
        return self.op(eng, lambda e: e.memset(ap, val), reads=[], writes=[ap])

    def recip(self, out, in_):
        return self.op("vector", lambda e: e.reciprocal(out, in_), reads=[in_], writes=[out])

    def reduce(self, out, in_, op=ALU.add, axis=AX.X):
        return self.op("vector", lambda e: e.tensor_reduce(out, in_, axis, op), reads=[in_], writes=[out])


def build_program(stop_after=99):
    nc = bass.Bass("TRN2", target_bir_lowering=False)

    declared = []

    def din(name, shape, dt=F32, need=0):
        if stop_after < need:
            return None
        declared.append(name)
        return nc.dram_tensor(name, list(shape), dt, kind="ExternalInput").ap()

    x = din("x", [T, DM])
    g1T_d = din("g1T", [128, 16])
    w_lora = din("w_lora", [DM, 448])
    w_rw = din("w_rw", [8, DM, 384])
    w_ret = din("w_ret", [8, DM, 768], need=2)
    w_in = din("w_in", [DM, IN_COLS], need=3)
    lprm_d = din("lprm", [128, 4])
    rwprm_d = din("rwprm", [128, 8, 8])
    rw_w2 = din("rw_w2", [96, 1024])
    rw_a2 = din("rw_a2", [96, 1024])
    rw_g2 = din("rw_g2", [256, 1024])
    lnx_g = din("lnx_g", [1, 1024])
    lnx_b = din("lnx_b", [1, 1024])
    ret_g = din("ret_g", [1, 2048], need=2)
    w_up_a = din("w_up_a", [1024, DM], need=3)
    w_up_b = din("w_up_b", [2048, DM], need=3)
    w_out = din("w_out", [DM, DM], need=4)
    g2_d = din("g2", [1, DM], need=4)
    gf_d = din("gf", [1, DM], need=4)
    w_rt = din("w_rt", [DM, 36], need=4)
    b_rt = din("b_rt", [1, 36], need=4)
    e_gate = din("e_gate", [NEXP, DM, EFF], need=4)
    e_up = din("e_up", [NEXP, DM, EFF], need=4)
    e_down = din("e_down", [NEXP, EFF, DM], need=4)
    c_identb = din("c_identb", [128, 128], BF16)
    c_identf = din("c_identf", [128, 128])
    c_maskS = din("c_maskS", [128, 384])
    c_maskL = din("c_maskL", [128, 128])
    c_bones = din("c_bones", [128, 128], BF16)
    c_hmask = din("c_hmask", [128, 2])
    c_reset = din("c_reset", [128, 1024], BF16)
    c_cs = din("c_cs", [128, 16, 4, 64], need=2)
    c_rmask = din("c_rmask", [128, 8, 128], need=2)
    c_rdec = din("c_rdec", [128, 16], need=2)
    c_tri = din("c_tri", [128, 128], BF16, need=4)
    c_iota = din("c_iota", [128, 128], need=4)
    out = nc.dram_tensor("out", [TO, DM], F32, kind="ExternalOutput").ap()
    oaT_d = nc.dram_tensor("oaT_scr", [8, 128, TO], BF16, kind="Internal").ap()
    obT_d = nc.dram_tensor("obT_scr", [16, 128, TO], BF16, kind="Internal").ap()
    mT_d = nc.dram_tensor("mT_scr", [16, 128, TO], BF16, kind="Internal").ap()
    dbg = None
    if stop_after < 99:
        dbg = nc.dram_tensor("dbg", [24, 128, TO], BF16, kind="ExternalOutput").ap()

    P = Prog(nc)
    P.es.enter_context(nc.allow_low_precision("bf16 PE transposes write bf16 PSUM"))
    pb = [P.ps([128, 512], F32, f"bank{i}") for i in range(6)]
    pbw = P.ps([128, 1024], F32, "bankw")
    pb.append(pbw[:, 0:512])
    pb.append(pbw[:, 512:1024])

    def pbb(i):
        return pb[i][:].bitcast(BF16)

    identb = P.sb([128, 128], BF16, "identb")
    identf = P.sb([128, 128], F32, "identf")
    P.dma("sync", identb[:], c_identb)
    P.dma("sync", identf[:], c_identf)

    with P.scope():
        hT_hi = P.sb([128, 16, 1024], BF16, "hT_hi")
        with P.scope():
            hT_lo = P.sb([128, 16, 1024], BF16, "hT_lo")

            def hTs(c, t0, n):
                if t0 < 1024:
                    return hT_lo[:, c, t0:t0 + n]
                return hT_hi[:, c, t0 - 1024:t0 - 1024 + n]

            g1T = P.sb([128, 16], F32, "g1T")
            P.dma("sync", g1T[:], g1T_d)
            with P.scope():
                xin = [P.sb([128, DM], F32, f"xin{i}") for i in range(4)]
                xnb = [P.sb([128, DM], BF16, f"xnb{i}") for i in range(4)]
                junk = P.sb([128, DM], BF16, "junk")
                st = [P.sb([128, 4], F32, f"st{i}") for i in range(4)]
                def _phase0():
                    for i in range(16):
                        xi = xin[i % 4]
                        xb = xnb[i % 4]
                        s = st[i % 4]
                        P.dma("sync", xi[:], x[i * 128:(i + 1) * 128, :])
                        P.act(junk[:], xi[:], AF.Square, accum_out=s[:, 0:1])
                        P.ts(s[:, 1:2], s[:, 0:1], 1.0 / DM, EPS, op0=ALU.mult, op1=ALU.add)
                        P.act(s[:, 2:3], s[:, 1:2], AF.Sqrt)
                        P.recip(s[:, 3:4], s[:, 2:3])
                        P.ts(xb[:], xi[:], s[:, 3:4], None, op0=ALU.mult)
                        for c4 in range(4):
                            bk = pbb(c4 % 2)
                            for k in range(4):
                                c = c4 * 4 + k
                                P.tr(bk[:, k * 128:(k + 1) * 128], xb[:, c * 128:(c + 1) * 128], identb[:])
                            for k in range(4):
                                c = c4 * 4 + k
                                dst = hTs(c, i * 128, 128)
                                if c4 % 2 == 0:
                                    P.ts(dst, bk[:, k * 128:(k + 1) * 128], g1T[:, c:c + 1], None, op0=ALU.mult)
                                else:
                                    P.act(dst, bk[:, k * 128:(k + 1) * 128], AF.Copy, scale=g1T[:, c:c + 1])

                P.region(_phase0)

            rw_phase(P, nc, locals())
            if stop_after >= 2:
                ret_phase(P, nc, locals())
        if stop_after < 3:
            with P.scope():
                dt_ = P.sb([128, 24, TO], BF16, "dbgt")
                P.dma("sync", dt_[:, 0:8, :], oaT_d.rearrange("c p t -> p c t"))
                if stop_after >= 2:
                    P.dma("sync", dt_[:, 8:24, :], obT_d.rearrange("c p t -> p c t"))
                else:
                    P.memset(dt_[:, 8:24, :], 0.0)
                P.dma("sync", dbg.rearrange("c p t -> p c t"), dt_[:])
        else:
            gate_phase(P, nc, locals())
    if stop_after >= 4:
        tail_phase(P, nc, locals())
    else:
        with P.scope():
            zt = P.sb([128, DM], F32, "zt")
            P.memset(zt[:], 0.0)
            for i in range(8):
                P.dma("sync", out[i * 128:(i + 1) * 128, :], zt[:])
    P.barrier()
    P.emit()
    return nc, declared


def rw_phase(P, nc, L):
    pb = L["pb"]; pbb = L["pbb"]; hTs = L["hTs"]; identb = L["identb"]; pbw = L["pbw"]
    with P.scope():
        lprm = P.sb([128, 8], F32, "lprm")
        P.dma("sync", lprm[:, 0:4], L["lprm_d"])
        P.ts(lprm[:, 4:8], lprm[:, 0:4], -1.0, 1.0, op0=ALU.mult, op1=ALU.add)
        rwprm = P.sb([128, 8, 14], F32, "rwprm")
        onec = P.sb([128, 2], F32, "onec")
        P.memset(onec[:], 1.0)
        P.dma("sync", rwprm[:, :, 0:8], L["rwprm_d"])
        P.ts(rwprm[:, :, 8:11], rwprm[:, :, 0:3], -1.0, 1.0, op0=ALU.mult, op1=ALU.add)
        P.ts(rwprm[:, :, 11:12], rwprm[:, :, 6:7], -1.0, 1.0, op0=ALU.mult, op1=ALU.add)
        P.ts(rwprm[:, :, 12:14], rwprm[:, :, 3:5], -1.0, None, op0=ALU.mult)
        maskL = P.sb([128, 128], F32, "maskL")
        bones = P.sb([128, 128], BF16, "bones")
        hmask = P.sb([128, 2], F32, "hmask")
        reset = P.sb([128, 1024], BF16, "reset")
        P.dma("sync", maskL[:], L["c_maskL"])
        P.dma("sync", bones[:], L["c_bones"])
        P.dma("sync", hmask[:], L["c_hmask"])
        P.dma("sync", reset[:], L["c_reset"])
        w2 = P.sb([96, 1024], BF16, "w2")
        a2 = P.sb([96, 1024], BF16, "a2")
        g2 = P.sb([128, 2, 1024], BF16, "g2")
        P.dma("gpsimd", w2[:], L["rw_w2"])
        P.dma("gpsimd", a2[:], L["rw_a2"])
        P.dma("gpsimd", g2[:], L["rw_g2"].rearrange("(c p) n -> p c n", p=128))
        lgb = P.sb([64, 256], F32, "lgb")

        txw = P.sb([96, T], BF16, "txw")
        xab = P.sb([96, T], BF16, "xab")
        sgx = P.sb([128, 2, TO], BF16, "sgx")
        saved = P.sb([128, 4], F32, "saved")

        with P.scope():
            wl = P.sb([128, 16, 448], BF16, "wl")
            raw = P.sb([128, 1025], F32, "raw")
            tmpF = P.sb([128, 1024], F32, "tmpF")

            def project(wt, c0, M, t0):
                for blk in range(2):
                    ps = pb[blk % 2]
                    for c in range(16):
                        P.mm(ps[0:M, :], wt[:, c, c0:c0 + M], hTs(c, t0 + blk * 512, 512),
                             start=(c == 0), stop=(c == 15))
                    P.act(raw[0:M, 1 + blk * 512:1 + (blk + 1) * 512], ps[0:M, :], AF.Copy)
            P.dma("gpsimd", wl[:], L["w_lora"].rearrange("(c p) n -> p c n", p=128))
            svl = P.sb([128, 4], F32, "svl")
            def _lora():
                for half in range(2):
                    t0 = half * 1024
                    for gi, (c0, M) in enumerate([(0, 96), (96, 96), (192, 128), (320, 128)]):
                        if half == 0:
                            P.memset(raw[0:M, 0:1], 0.0)
                        else:
                            P.copy(raw[0:M, 0:1], svl[0:M, gi:gi + 1])
                        project(wl, c0, M, t0)
                        tmp = tmpF[0:M, :]
                        P.ts(tmp, raw[0:M, 1:1025], lprm[0:M, 4 + gi:5 + gi], None, op0=ALU.mult)
                        P.stt(tmp, raw[0:M, 0:1024], lprm[0:M, gi:gi + 1], tmp, ALU.mult, ALU.add)
                        P.copy(svl[0:M, gi:gi + 1], raw[0:M, 1024:1025])
                        if gi == 0:
                            P.act(txw[:, t0:t0 + 1024], tmp, AF.Tanh)
                        elif gi == 1:
                            P.act(xab[:, t0:t0 + 1024], tmp, AF.Copy)
                        elif half == 1:
                            P.act(sgx[:, gi - 2, :], tmp, AF.Sigmoid)

            P.region(_lora)

        UL = 512
        NCU = 8
        ARx = [P.sb([128, NCU, 192], BF16, f"ARx{i}") for i in range(2)]
        BKx = [P.sb([128, NCU, 2, 2, 64], BF16, f"BKx{i}") for i in range(2)]
        BHx = [P.sb([128, NCU, 2, 64], BF16, f"BHx{i}") for i in range(2)]
        KHx = [P.sb([128, NCU, 2, 64], BF16, f"KHx{i}") for i in range(2)]
        Vx = [P.sb([128, NCU, 2, 64], BF16, f"Vx{i}") for i in range(2)]
        Vc = [P.sb([128, UL], BF16, f"Vc{i}") for i in range(2)]
        RKb = [P.sb([128, UL], BF16, f"RKb{i}") for i in range(2)]
        Pall = [P.sb([128, NCU, 128], BF16, f"Pall{i}") for i in range(2)]
        WC = [P.sb([128, NCU], F32, f"WC{i}") for i in range(2)]
        for lst in (ARx, BKx, BHx, KHx, Vx):
            for tl in lst:
                P.memset(tl[:], 0.0, eng="gpsimd")
        Hst = P.sb([128, 128], F32, "Hst")
        Hbf = P.sb([128, 128], BF16, "Hbf")
        wts = [P.sb([128, 16, 128], BF16, f"wts{i}") for i in range(3)]
        rksel = P.sb([128, 2], BF16, "rksel")
        maskS = P.sb([128, 384], F32, "maskS")
        P.dma("sync", maskS[:], L["c_maskS"])
        SCm = [P.sb([128, 384], BF16, f"SCm{i}") for i in range(2)]
        TM = [P.sb([128, 384], BF16, f"TM{i}") for i in range(2)]
        TMc = [P.sb([64, 2, 128], BF16, f"TMc{i}") for i in range(2)]
        Zs = [P.sb([128, 128], BF16, f"Zs{i}") for i in range(2)]
        Us = [P.sb([128, 128], BF16, f"Us{i}") for i in range(2)]
        XXg = [[P.sb([128, 1024], BF16, f"XX{g_}_{i}") for i in range(2)] for g_ in range(2)]
        Pmg = [P.sb([128, 512], BF16, f"Pm{g_}") for g_ in range(2)]
        ep = [P.sb([64, 32], F32, f"ep{i}") for i in range(2)]
        ey = [P.sb([64, 256], F32, f"ey{i}") for i in range(2)]
        ey2 = [P.sb([64, 256], F32, f"eyb{i}") for i in range(2)]
        eg = [P.sb([64, 256], F32, f"eg{i}") for i in range(2)]
        eo = [P.sb([64, 256], BF16, f"eo{i}") for i in range(2)]
        oaT_st = P.sb([128, 1024], BF16, "oaT_st")
        mS = maskS[:, 0:128]
        Fq = [P.sb([128, UL], F32, f"Fq{i}") for i in range(6)]
        Bq = [P.sb([128, UL], BF16, f"Bq{i}") for i in range(4)]
        rawq = P.sb([128, UL + 1], F32, "rawq")

        def v3(t_, hs):
            return t_[hs, :].rearrange("p (c t) -> p c t", t=64)

        def wload(p, xi):
            P.dma("gpsimd", wts[xi][:], L["w_rw"][p].rearrange("(c p) n -> p c n", p=128)[:, :, xi * 128:(xi + 1) * 128])

        def prep(p, q, ub):
            t0 = q * UL
            need_y = q >= 2
            prm = rwprm[:, p, :]
            cs = slice(p * 128, (p + 1) * 128)
            Rf, Kf, T1, SGb, Epos, CUMb = Fq
            ALb, KKb, RNb, KK2b = Bq
            arx, bkx, bhx, khx, vx, vc, rkb, pall, wc = ARx[ub], BKx[ub], BHx[ub], KHx[ub], Vx[ub], Vc[ub], RKb[ub], Pall[ub], WC[ub]
            for xi, dst in enumerate((Rf, Kf, T1)):
                if xi == 0 and not need_y:
                    if q == 1:
                        for c in range(16):
                            P.mm(pb[0][:, 0:2], wts[0][:, c, :], hTs(c, t0 + UL - 2, 2), start=(c == 0), stop=(c == 15))
                        P.act(saved[:, 0:1], pb[0][:, 1:2], AF.Copy)
                    continue
                if q == 0:
                    P.memset(rawq[:, 0:1], 0.0, eng="gpsimd")
                else:
                    P.copy(rawq[:, 0:1], saved[:, xi:xi + 1])
                ps = pb[xi % 2]
                for c in range(16):
                    P.mm(ps[:], wts[xi][:, c, :], hTs(c, t0, UL), start=(c == 0), stop=(c == 15))
                if q == 3 and p + 1 < 8:
                    wload(p + 1, xi)
                P.act(rawq[:, 1:UL + 1], ps[:], AF.Copy)
                P.act(dst[:], ps[:], AF.Copy, scale=prm[:, 8 + xi:9 + xi])
                P.stt(dst[:], rawq[:, 0:UL], prm[:, xi:xi + 1], dst[:], ALU.mult, ALU.add)
                P.copy(saved[:, xi:xi + 1], rawq[:, UL:UL + 1])
            for h in range(2):
                hs = slice(h * 64, (h + 1) * 64)
                P.act(vx[hs, :, h, :], v3(T1, hs), AF.Copy)
            if need_y:
                P.act(vc[:], T1[:], AF.Copy)
            P.mm(pb[0][:], w2[:, cs], txw[:, t0:t0 + UL])
            P.act(SGb[:], pb[0][:], AF.Exp, scale=-1.0, bias=prm[:, 12:13])
            P.act(SGb[:], SGb[:], AF.Ln, bias=onec[:, 0:1])
            P.act(SGb[:], SGb[:], AF.Exp, scale=-1.0)
            P.mm(pb[1][:], a2[:, cs], xab[:, t0:t0 + UL])
            P.act(ALb[:], pb[1][:], AF.Exp, scale=-1.0, bias=prm[:, 13:14])
            P.act(ALb[:], ALb[:], AF.Ln, bias=onec[:, 0:1])
            P.act(ALb[:], ALb[:], AF.Exp, scale=-1.0)
            P.op("vector", lambda e, o=CUMb, r_=reset, s_=SGb: e.tensor_tensor_scan(o[:], r_[:, 0:UL], s_[:], 0.0, ALU.mult, ALU.add),
                 reads=[reset, SGb], writes=[CUMb])
            P.tt(T1[:], CUMb[:], SGb[:], ALU.subtract)
            P.act(T1[:], T1[:], AF.Exp, scale=CDEC)
            cv = CUMb[:].rearrange("p (c t) -> p c t", t=64)
            P.act(Epos[:], CUMb[:], AF.Exp, scale=CDEC)
            P.act(CUMb[:], CUMb[:], AF.Exp, scale=-CDEC)
            P.tt(SGb[:].rearrange("p (c t) -> p c t", t=64), cv,
                 Epos[:].rearrange("p (c t) -> p c t", t=64)[:, :, 63:64].to_broadcast([128, NCU, 64]), ALU.mult)
            Eprev, Eend, Eneg = T1, SGb, CUMb
            P.copy(wc[:], Epos[:].rearrange("p (c t) -> p c t", t=64)[:, :, 63])
            P.act(KKb[:], Kf[:], AF.Copy, scale=prm[:, 5:6])
            P.act(KK2b[:], Kf[:], AF.Square, scale=prm[:, 5:6])
            P.mm(pb[0][:], bones[:], KK2b[:])
            P.ts(RNb[:], pb[0][:], 1e-18, None, op0=ALU.max)
            P.act(RNb[:], RNb[:], AF.Ln)
            P.act(RNb[:], RNb[:], AF.Exp, scale=-0.5)
            P.tt(KKb[:], KKb[:], RNb[:], ALU.mult)
            for h in range(2):
                hs = slice(h * 64, (h + 1) * 64)
                P.tt(arx[hs, :, h * 64:(h + 1) * 64], v3(KKb, hs), v3(Eprev, hs), ALU.mult)
            P.tt(KKb[:], KKb[:], ALb[:], ALU.mult)
            P.ts(RNb[:], ALb[:], prm[:, 6:7], prm[:, 11:12], op0=ALU.mult, op1=ALU.add)
            P.tt(Kf[:], Kf[:], RNb[:], ALU.mult)
            for h in range(2):
                hs = slice(h * 64, (h + 1) * 64)
                e1 = "vector"
                e2 = "vector"
                P.tt(bkx[hs, :, 0, h, :], v3(KKb, hs), v3(Eneg, hs), ALU.mult, eng=e1)
                P.tt(bkx[hs, :, 1, h, :], v3(Kf, hs), v3(Eneg, hs), ALU.mult, eng=e2)
                P.tt(bhx[hs, :, h, :], v3(KKb, hs), v3(Eend, hs), ALU.mult, eng=e1)
                P.tt(khx[hs, :, h, :], v3(Kf, hs), v3(Eend, hs), ALU.mult, eng=e2)
            if need_y:
                P.tt(arx[:, :, 128:192], Rf[:].rearrange("p (c t) -> p c t", t=64),
                     Epos[:].rearrange("p (c t) -> p c t", t=64), ALU.mult)
                P.tt(rkb[:], Rf[:], Kf[:], ALU.mult)
            r4 = lambda t_: t_.rearrange("p (k n) -> p k n", n=128)
            for g in range(NCU // 4):
                XX = XXg[g % 2]
                Pm = Pmg[g % 2]
                xx = XX[0]
                for k in range(4):
                    c = g * 4 + k
                    bcols = bkx[:, c, 0, :, :].rearrange("p a b -> p (a b)")
                    acols = arx[:, c, 0:128]
                    P.mm(pbw[:, k * 128:(k + 1) * 128], bcols, acols)
                    P.mm(pbw[:, 512 + k * 128:512 + (k + 1) * 128], acols, bcols)
                P.tt(r4(xx[:, 0:512]), r4(pbw[:, 0:512]), mS.unsqueeze(1).to_broadcast([128, 4, 128]), ALU.mult)
                P.tt(r4(xx[:, 512:1024]), r4(pbw[:, 512:1024]), maskL[:].unsqueeze(1).to_broadcast([128, 4, 128]), ALU.mult)
                P.tt(r4(Pm[:]), L["identf"][:].unsqueeze(1).to_broadcast([128, 4, 128]), r4(xx[:, 0:512]), ALU.subtract)
                for lvl in range(6):
                    if lvl < 5:
                        for k in range(4):
                            ks = slice(k * 128, (k + 1) * 128)
                            kt = slice(512 + k * 128, 512 + (k + 1) * 128)
                            P.mm(pbw[:, ks], xx[:, kt], xx[:, ks])
                            P.mm(pbw[:, kt], xx[:, ks], xx[:, kt])
                    if lvl >= 1:
                        for k in range(4):
                            ks = slice(k * 128, (k + 1) * 128)
                            kt = slice(512 + k * 128, 512 + (k + 1) * 128)
                            P.mm(pb[1][:, ks], xx[:, kt], Pm[:, ks])
                    if lvl < 5:
                        xn = XX[(lvl + 1) % 2]
                        P.act(xn[:], pbw[:], AF.Copy)
                    if lvl >= 1:
                        if lvl == 5:
                            P.tt(pall[:, g * 4:(g + 1) * 4, :], r4(Pm[:]), r4(pb[1][:]), ALU.add)
                        else:
                            P.tt(Pm[:], Pm[:], pb[1][:], ALU.add)
                    if lvl < 5:
                        xx = xn

        def rec(p, q, ub):
            t0 = q * UL
            need_y = q >= 2
            prm = rwprm[:, p, :]
            cs = slice(p * 128, (p + 1) * 128)
            arx, bkx, bhx, khx, vx, vc, rkb, pall, wc = ARx[ub], BKx[ub], BHx[ub], KHx[ub], Vx[ub], Vc[ub], RKb[ub], Pall[ub], WC[ub]
            if q == 0:
                P.memset(Hst[:], 0.0)
                P.memset(Hbf[:], 0.0)
            if q == 2:
                P.ts(rksel[:], hmask[:], prm[:, 7:8], None, op0=ALU.mult)
                P.dma("sync", lgb[:, 0:128], L["lnx_g"][:, cs].partition_broadcast(64))
                P.dma("sync", lgb[:, 128:256], L["lnx_b"][:, cs].partition_broadcast(64))
            for c in range(NCU):
                par = c % 2
                sc = SCm[par]; tm = TM[par]; zs = Zs[par]; us = Us[par]
                ppar = (c // 2) % 2
                tmc = TMc[ppar]
                bcols = bkx[:, c, 0, :, :].rearrange("p a b -> p (a b)")
                kcols = bkx[:, c, 1, :, :].rearrange("p a b -> p (a b)")
                acols = arx[:, c, 0:128]
                rcols = arx[:, c, 128:192]
                nar = 192 if need_y else 128
                P.mm(pb[3][:, 0:nar], bcols, arx[:, c, 0:nar])
                P.mm(pb[3][:, 192:192 + nar], kcols, arx[:, c, 0:nar])
                s3 = lambda t_: t_[:, 0:384].rearrange("p (a n) -> p a n", n=192)[:, :, 0:nar]
                P.tt(s3(sc), s3(pb[3]), s3(maskS), ALU.mult)
                nabT, nrbT, nakT, nrkT = sc[:, 0:128], sc[:, 128:192], sc[:, 192:320], sc[:, 320:384]
                bT = pbb(2)
                P.tr(bT[:, 0:128], bhx[:, c, :, :].rearrange("p a b -> p (a b)"), identb[:])
                P.tr(bT[:, 128:256], khx[:, c, :, :].rearrange("p a b -> p (a b)"), identb[:])
                P.tr(bT[:, 256:384], vx[:, c, :, :].rearrange("p a b -> p (a b)"), identb[:])
                P.act(tm[:, 0:384], bT[:, 0:384], AF.Copy)
                if need_y:
                    P.tr(bT[0:64, 384:512], vc[:, c * 64:(c + 1) * 64], identb[:])
                    P.act(tmc[:, c % 2, :], bT[0:64, 384:512], AF.Copy)
                bht, kht, vt = tm[:, 0:128], tm[:, 128:256], tm[:, 256:384]
                P.mm(pb[4][:, 0:128], acols, Hbf[:], start=True, stop=False)
                P.mm(pb[4][:, 0:128], nakT, vt, start=False, stop=True)
                P.copy(zs[:], pb[4][:, 0:128])
                P.mm(pb[4][:, 128:256], pall[:, c, :], zs[:])
                P.ts(us[:], pb[4][:, 128:256], -1.0, None, op0=ALU.mult)
                if need_y:
                    yc = slice((c % 2) * 128, (c % 2 + 1) * 128)
                    P.mm(pb[5][0:64, yc], rcols, Hbf[:], start=True, stop=False)
                    P.mm(pb[5][0:64, yc], nrbT, us[:], start=False, stop=False)
                    P.mm(pb[5][0:64, yc], nrkT, vt, start=False, stop=True)
                P.mm(pb[4][:, 256:384], bht, us[:], start=True, stop=False)
                P.mm(pb[4][:, 256:384], kht, vt, start=False, stop=True)
                P.stt(Hbf[:], Hst[:], wc[:, c:c + 1], pb[4][:, 256:384], ALU.mult, ALU.add)
                P.stt(Hst[:], Hst[:], wc[:, c:c + 1], pb[4][:, 256:384], ALU.mult, ALU.add)
                if need_y and c % 2 == 1:
                    e = ep[ppar]; y = ey[ppar]; y2 = ey2[ppar]; o = eo[ppar]; gg = eg[ppar]
                    to0 = (q - 2) * UL + (c - 1) * 64
                    tu0 = (c - 1) * 64
                    P.copy(y[:], pb[5][0:64, 0:256])
                    for j in range(2):
                        for kc in range(2):
                            P.mm(pb[5][0:64, 256 + j * 128:256 + (j + 1) * 128], sgx[:, kc, to0 + j * 64:to0 + (j + 1) * 64],
                                 g2[:, kc, cs], start=(kc == 0), stop=(kc == 1))
                    P.act(gg[:], pb[5][0:64, 256:512], AF.Copy)
                    for j in range(2):
                        P.mm(pb[3][0:64, 384 + 2 * j:386 + 2 * j], rkb[:, tu0 + j * 64:tu0 + (j + 1) * 64], rksel[:])
                    P.act(e[:, 24:28], pb[3][0:64, 384:388], AF.Copy)
                    y4 = y[:].rearrange("p (a v) -> p a v", v=64)
                    P.reduce(e[:, 0:4], y4)
                    P.tt(y2[:], y[:], y[:], ALU.mult)
                    P.reduce(e[:, 4:8], y2[:].rearrange("p (a v) -> p a v", v=64))
                    P.ts(e[:, 8:12], e[:, 0:4], 1.0 / 64, None, op0=ALU.mult)
                    P.tt(e[:, 12:16], e[:, 8:12], e[:, 8:12], ALU.mult)
                    P.ts(e[:, 16:20], e[:, 4:8], 1.0 / 64, RW_LN_EPS, op0=ALU.mult, op1=ALU.add)
                    P.tt(e[:, 16:20], e[:, 16:20], e[:, 12:16], ALU.subtract)
                    P.act(e[:, 16:20], e[:, 16:20], AF.Ln)
                    P.act(e[:, 20:24], e[:, 16:20], AF.Exp, scale=-0.5)
                    P.tt(y4, y4, e[:, 8:12].unsqueeze(2).to_broadcast([64, 4, 64]), ALU.subtract)
                    P.tt(y4, y4, e[:, 20:24].unsqueeze(2).to_broadcast([64, 4, 64]), ALU.mult)
                    y3 = y[:].rearrange("p (j n) -> p j n", n=128)
                    P.tt(y3, y3, lgb[:, 0:128].unsqueeze(1).to_broadcast([64, 2, 128]), ALU.mult)
                    P.tt(y3, y3, lgb[:, 128:256].unsqueeze(1).to_broadcast([64, 2, 128]), ALU.add)
                    P.tt(y2[:].rearrange("p (a v) -> p a v", v=64), tmc[:].rearrange("p j (h v) -> p (j h) v", v=64),
                         e[:, 24:28].unsqueeze(2).to_broadcast([64, 4, 64]), ALU.mult)
                    P.tt(y[:], y[:], y2[:], ALU.add)
                    P.tt(o[:], y[:], gg[:], ALU.mult)
                    for j in range(2):
                        P.tr(bT[:, 512 + j * 64:512 + (j + 1) * 64], o[:, j * 128:(j + 1) * 128], identb[0:64, 0:64])
                    P.act(oaT_st[:, to0:to0 + 128], bT[:, 512:640], AF.Copy)
            if q == 3:
                P.dma("sync", L["oaT_d"][p], oaT_st[:])

        units = [(p, q) for p in range(8) for q in range(4)]
        for xi in range(3):
            wload(0, xi)
        prep(0, 0, 0)
        WIN = 8
        for u0 in range(0, len(units), WIN):
            items = []
            for u in range(u0, min(u0 + WIN, len(units))):
                p, q = units[u]
                items += P.record(lambda p=p, q=q, u=u: rec(p, q, u % 2))
                if u + 1 < len(units):
                    pn, qn = units[u + 1]
                    items += P.record(lambda pn=pn, qn=qn, u=u: prep(pn, qn, (u + 1) % 2))
            P.schedule(items)


def ret_phase(P, nc, L):
    pb = L["pb"]; pbb = L["pbb"]; hTs = L["hTs"]; identb = L["identb"]
    with P.scope():
        cst = P.sb([128, 16, 4, 64], F32, "cst")
        rmask = P.sb([128, 8, 128], F32, "rmask")
        rdec = P.sb([128, 16], F32, "rdec")
        P.dma("sync", cst[:], L["c_cs"])
        P.dma("sync", rmask[:], L["c_rmask"])
        P.dma("sync", rdec[:], L["c_rdec"])
        retg = P.sb([128, 256], F32, "retg")
        wr = [P.sb([128, 16, 768], BF16, f"wr{i}") for i in range(2)]
        S = P.sb([128, 256], F32, "S")
        Sbf = P.sb([128, 256], BF16, "Sbf")
        obT_st = P.sb([128, 2, 1024], BF16, "obT_st")
        A_ = [P.sb([128, 2, 128], F32, f"rA{i}") for i in range(2)]
        B_ = [P.sb([128, 2, 128], F32, f"rB{i}") for i in range(2)]
        rot = [P.sb([128, 2, 128], F32, f"rot{i}") for i in range(2)]
        qkb = [P.sb([128, 3, 128], BF16, f"qkb{i}") for i in range(2)]
        qkT = [P.sb([128, 2, 128], BF16, f"qkT{i}") for i in range(2)]
        vb = [P.sb([128, 256], BF16, f"vb{i}") for i in range(2)]
        scm = [P.sb([128, 128], BF16, f"scm{i}") for i in range(2)]
        sg = [P.sb([128, 256], F32, f"sg{i}") for i in range(2)]
        ob = [P.sb([128, 256], BF16, f"ob{i}") for i in range(2)]
        rs = [P.sb([128, 4], F32, f"rs{i}") for i in range(2)]
        junk = P.sb([128, 256], BF16, "rjunk")
        onec2 = P.sb([128, 2], F32, "onec2")
        P.memset(onec2[:], 1.0)
        lgam = [math.log1p(-2.0 ** (-5.0 - h)) for h in range(8)]
        P.dma("gpsimd", wr[0][:], L["w_ret"][0].rearrange("(c p) n -> p c n", p=128))
        def _head(h):
            wt = wr[h % 2]
            if h + 1 < 8:
                P.dma("gpsimd", wr[(h + 1) % 2][:], L["w_ret"][h + 1].rearrange("(c p) n -> p c n", p=128))
            P.dma("sync", retg[:], L["ret_g"][:, h * 256:(h + 1) * 256].partition_broadcast(128))
            P.memset(S[:], 0.0)
            P.memset(Sbf[:], 0.0)
            g128 = math.exp(128.0 * lgam[h])
            def proj(i):
                par = i % 2
                need_o = i >= 8
                t0 = i * 128
                pa = pb[par]
                lo = 0 if need_o else 128
                for c in range(16):
                    P.mm(pa[:, lo:512], hTs(c, t0, 128), wt[:, c, lo:512], start=(c == 0), stop=(c == 15))
                if need_o:
                    pg = pb[6] if par == 0 else pb[7]
                    for c in range(16):
                        P.mm(pg[:, 0:256], hTs(c, t0, 128), wt[:, c, 512:768], start=(c == 0), stop=(c == 15))

            proj(0)
            for i in range(16):
                par = i % 2
                need_o = i >= 8
                t0 = i * 128
                pa = pb[par]
                pg = pb[6] if par == 0 else pb[7]
                A = A_[par]; Bm = B_[par]; ro = rot[par]; qk = qkb[par]; qT = qkT[par]
                for xi in ([0, 1] if need_o else [1]):
                    xs = pa[:, xi * 128:(xi + 1) * 128].rearrange("p (a d) -> p a d", d=64)
                    ci = 0 if xi == 0 else 2
                    cosb = cst[:, i, ci, :].unsqueeze(1).to_broadcast([128, 2, 64])
                    sinb = cst[:, i, ci + 1, :].unsqueeze(1).to_broadcast([128, 2, 64])
                    Av = A[:, xi, :].rearrange("p (a d) -> p a d", d=64)
                    Bv = Bm[:, xi, :].rearrange("p (a d) -> p a d", d=64)
                    P.tt(Av, xs, cosb, ALU.mult)
                    P.tt(Bv, xs, sinb, ALU.mult)
                    P.tt(ro[:, xi, 0:64], A[:, xi, 0:64], Bm[:, xi, 64:128], ALU.subtract)
                    P.tt(ro[:, xi, 64:128], Bm[:, xi, 0:64], A[:, xi, 64:128], ALU.add)
                if need_o:
                    P.act(qk[:, 0, :], ro[:, 0, :], AF.Copy, scale=rdec[:, h:h + 1])
                    P.act(qk[:, 1, :], ro[:, 1, :], AF.Copy)
                P.act(qk[:, 2, :], ro[:, 1, :], AF.Copy, scale=rdec[:, 8 + h:9 + h])
                P.act(vb[par][:], pa[:, 256:512], AF.Copy)
                if i + 1 < 16:
                    proj(i + 1)
                if need_o:
                    bT = pbb(5)
                    P.tr(bT[:, 0:128], qk[:, 0, :], identb[:])
                    P.tr(bT[:, 128:256], qk[:, 1, :], identb[:])
                    P.copy(qT[:].rearrange("p a d -> p (a d)"), bT[:, 0:256])
                    P.mm(pb[2][:, 0:128], qT[:, 1, :], qT[:, 0, :])
                    P.tt(scm[par][:], pb[2][:, 0:128], rmask[:, h, :], ALU.mult)
                    P.mm(pb[3][:, 0:256], scm[par][:], vb[par][:], start=True, stop=False)
                    P.mm(pb[3][:, 0:256], qT[:, 0, :], Sbf[:], start=False, stop=True)
                P.mm(pb[4][:, 0:256], qk[:, 2, :], vb[par][:])
                P.stt(S[:], S[:], g128, pb[4][:, 0:256], ALU.mult, ALU.add)
                P.act(Sbf[:], S[:], AF.Copy)
                if need_o:
                    r = rs[par]
                    P.act(junk[:], pb[3][:, 0:256], AF.Square, accum_out=r[:, 0:1])
                    P.ts(r[:, 1:2], r[:, 0:1], 1.0 / 256, EPS, op0=ALU.mult, op1=ALU.add)
                    P.act(r[:, 2:3], r[:, 1:2], AF.Ln)
                    P.act(r[:, 3:4], r[:, 2:3], AF.Exp, scale=-0.5)
                    P.act(sg[par][:], pg[:, 0:256], AF.Exp, scale=-1.0)
                    P.act(sg[par][:], sg[par][:], AF.Ln, bias=onec2[:, 0:1])
                    P.act(sg[par][:], sg[par][:], AF.Exp, scale=-1.0)
                    P.tt(sg[par][:], sg[par][:], pg[:, 0:256], ALU.mult)
                    P.tt(sg[par][:], sg[par][:], retg[:], ALU.mult)
                    P.stt(ob[par][:], pb[3][:, 0:256], r[:, 3:4], sg[par][:], ALU.mult, ALU.mult)
                    bT2 = pbb(5)
                    for ec in range(2):
                        P.tr(bT2[:, 256 + ec * 128:256 + (ec + 1) * 128], ob[par][:, ec * 128:(ec + 1) * 128], identb[:])
                    P.act(obT_st[:, :, (i - 8) * 128:(i - 7) * 128], bT2[:, 256:512].rearrange("p (a d) -> p a d", d=128), AF.Copy)
            for ec in range(2):
                P.dma("sync", L["obT_d"][h * 2 + ec], obT_st[:, ec, :])


        def _allheads():
            for h in range(8):
                _head(h)
        P.region(_allheads)


def gate_phase(P, nc, L):
    pb = L["pb"]; hT_hi = L["hT_hi"]; w_in = L["w_in"]
    with P.scope():
        oaT = P.sb([128, 8, TO], BF16, "oaT")
        obT = P.sb([128, 16, TO], BF16, "obT")
        P.dma("sync", oaT[:], L["oaT_d"].rearrange("c p t -> p c t"))
        P.dma("sync", obT[:], L["obT_d"].rearrange("c p t -> p c t"))
        GA_ = [P.sb([128, 16, 512], BF16, f"GA{i}") for i in range(2)]
        GB_ = [P.sb([128, 16, 512], BF16, f"GB{i}") for i in range(2)]
        UA_ = [P.sb([128, 8, 512], BF16, f"UA{i}") for i in range(2)]
        UB_ = [P.sb([128, 16, 512], BF16, f"UB{i}") for i in range(2)]
        sA = [P.sb([128, 512], F32, f"sA{i}") for i in range(2)]
        sB = [P.sb([128, 512], F32, f"sB{i}") for i in range(2)]
        mT = [P.sb([128, TO], BF16, f"mT{i}") for i in range(2)]
        wv = w_in.rearrange("(c p) n -> p c n", p=128)
        def _quad(q):
            GA, GB, UA, UB = GA_[q % 2], GB_[q % 2], UA_[q % 2], UB_[q % 2]
            P.dma("gpsimd", GA[:], wv[:, :, GATE_OFF + q * 512:GATE_OFF + (q + 1) * 512])
            P.dma("gpsimd", GB[:], wv[:, :, GATE_OFF + 2048 + q * 512:GATE_OFF + 2048 + (q + 1) * 512])
            P.dma("gpsimd", UA[:], L["w_up_a"].rearrange("(c p) n -> p c n", p=128)[:, :, q * 512:(q + 1) * 512])
            P.dma("gpsimd", UB[:], L["w_up_b"].rearrange("(c p) n -> p c n", p=128)[:, :, q * 512:(q + 1) * 512])
            for jj in range(4):
                j = q * 4 + jj
                cols = slice(jj * 128, (jj + 1) * 128)
                mt = mT[j % 2]
                for blk in range(2):
                    par = blk
                    bs = slice(blk * 512, (blk + 1) * 512)
                    b0, b1, b2, b3 = pb[par * 4], pb[par * 4 + 1], pb[par * 4 + 2], pb[par * 4 + 3]
                    for c in range(16):
                        P.mm(b0[:], GA[:, c, cols], hT_hi[:, c, bs], start=(c == 0), stop=(c == 15))
                    for c in range(16):
                        P.mm(b1[:], GB[:, c, cols], hT_hi[:, c, bs], start=(c == 0), stop=(c == 15))
                    for c in range(8):
                        P.mm(b2[:], UA[:, c, cols], oaT[:, c, bs], start=(c == 0), stop=(c == 7))
                    for c in range(16):
                        P.mm(b3[:], UB[:, c, cols], obT[:, c, bs], start=(c == 0), stop=(c == 15))
                    P.act(sA[par][:], b0[:], AF.Sigmoid)
                    P.act(sB[par][:], b1[:], AF.Sigmoid)
                    P.tt(sA[par][:], sA[par][:], b2[:], ALU.mult)
                    P.tt(sB[par][:], sB[par][:], b3[:], ALU.mult)
                    P.tt(mt[:, bs], sA[par][:], sB[par][:], ALU.add)
                P.dma("sync", L["mT_d"][j], mt[:])


        def _allq():
            for q in range(4):
                _quad(q)
        P.region(_allq)


def tail_phase(P, nc, L):
    pb = L["pb"]; pbb = L["pbb"]; identb = L["identb"]; identf = L["identf"]
    x = L["x"]; out = L["out"]
    with P.scope():
        x2 = P.sb([128, 8, DM], F32, "x2")
        with P.scope():
            mg = P.sb([128, 16, TO], BF16, "mg")
            P.dma("sync", mg[:], L["mT_d"].rearrange("c p t -> p c t"))
            Wo = [P.sb([128, 16, 512], BF16, f"Wo{i}") for i in range(2)]
            xin = [P.sb([128, 512], F32, f"xin3_{i}") for i in range(2)]
            wov = L["w_out"].rearrange("(c p) n -> p c n", p=128)
            P.dma("gpsimd", Wo[0][:], wov[:, :, 0:512])
            def _wout():
                k = 0
                for nb in range(4):
                    if nb + 1 < 4:
                        P.dma("gpsimd", Wo[(nb + 1) % 2][:], wov[:, :, (nb + 1) * 512:(nb + 2) * 512])
                    for i in range(8):
                        bk = pb[k % 4]
                        xi = xin[k % 2]
                        k += 1
                        P.dma("sync", xi[:], x[1024 + i * 128:1024 + (i + 1) * 128, nb * 512:(nb + 1) * 512])
                        for c in range(16):
                            P.mm(bk[:], mg[:, c, i * 128:(i + 1) * 128], Wo[nb % 2][:, c, :], start=(c == 0), stop=(c == 15))
                        P.tt(x2[:, i, nb * 512:(nb + 1) * 512], bk[:], xi[:], ALU.add)

            P.region(_wout)

        with P.scope():
            h2 = P.sb([128, 8, DM], BF16, "h2")
            asg = P.sb([128, 8, 32], F32, "asg")
            asgb = P.sb([128, 8, 32], BF16, "asgb")
            wmat = P.sb([128, 8, 32], F32, "wmat")
            pos = P.sb([128, 8, 32], F32, "pos")
            iota = P.sb([128, 128], F32, "iota")
            tri = P.sb([128, 128], BF16, "tri")
            onesb = P.sb([128, 128], BF16, "onesb")
            P.dma("sync", iota[:], L["c_iota"])
            P.dma("sync", tri[:], L["c_tri"])
            P.memset(onesb[:], 1.0)
            Wg = [P.sb([128, 16, EFF], BF16, "Wg0")]
            Wu = [P.sb([128, 16, EFF], BF16, "Wu0")]
            P.dma("gpsimd", Wg[0][:], L["e_gate"][0].rearrange("(c p) f -> p c f", p=128))
            P.dma("gpsimd", Wu[0][:], L["e_up"][0].rearrange("(c p) f -> p c f", p=128))
            with P.scope():
                g2b = P.sb([128, DM], F32, "g2b")
                P.dma("sync", g2b[:], L["g2_d"].partition_broadcast(128))
                wrt = P.sb([128, 16, 36], F32, "wrt")
                P.dma("sync", wrt[:], L["w_rt"].rearrange("(c p) n -> p c n", p=128))
                brt = P.sb([128, 36], F32, "brt")
                P.dma("sync", brt[:], L["b_rt"].partition_broadcast(128))
                h2f = [P.sb([128, DM], F32, f"h2f{i}") for i in range(2)]
                h2T = [P.sb([128, 16, 128], F32, f"h2T{i}") for i in range(2)]
                junk = P.sb([128, DM], BF16, "junk4")
                rr = [P.sb([128, 24], F32, f"rr{i}") for i in range(2)]
                lgt = [P.sb([128, 36], F32, f"lgt{i}") for i in range(2)]
                em = [P.sb([128, 3, 32], F32, f"em{i}") for i in range(2)]
                def _router():
                    for i in range(8):
                        par = i % 2
                        r = rr[par]; hf = h2f[par]; hT_ = h2T[par]; lg = lgt[par]; e_ = em[par]
                        P.act(junk[:], x2[:, i, :], AF.Square, accum_out=r[:, 0:1])
                        P.ts(r[:, 1:2], r[:, 0:1], 1.0 / DM, EPS, op0=ALU.mult, op1=ALU.add)
                        P.act(r[:, 2:3], r[:, 1:2], AF.Ln)
                        P.act(r[:, 3:4], r[:, 2:3], AF.Exp, scale=-0.5)
                        P.stt(hf[:], x2[:, i, :], r[:, 3:4], g2b[:], ALU.mult, ALU.mult)
                        P.act(h2[:, i, :], hf[:], AF.Copy)
                        for c4 in range(4):
                            bk = pb[c4 % 2]
                            for kk in range(4):
                                c = c4 * 4 + kk
                                P.tr(bk[:, kk * 128:(kk + 1) * 128], hf[:, c * 128:(c + 1) * 128], identf[:])
                            P.act(hT_[:, c4 * 4:(c4 + 1) * 4, :], bk[:].rearrange("p (a d) -> p a d", d=128), AF.Copy)
                        for c in range(16):
                            P.mm(pb[2][:, 0:36], hT_[:, c, :], wrt[:, c, :], start=(c == 0), stop=(c == 15))
                        P.tt(lg[:], pb[2][:, 0:36], brt[:], ALU.add)
                        P.reduce(r[:, 4:5], lg[:, 0:4], op=ALU.max)
                        P.ts(r[:, 8:12], lg[:, 0:4], r[:, 4:5], None, op0=ALU.is_equal)
                        P.ts(r[:, 5:6], r[:, 4:5], -1.0, None, op0=ALU.mult)
                        P.act(r[:, 12:16], lg[:, 0:4], AF.Exp, bias=r[:, 5:6], accum_out=r[:, 6:7])
                        P.recip(r[:, 7:8], r[:, 6:7])
                        P.ts(r[:, 16:20], r[:, 8:12], 1e30, -1e30, op0=ALU.mult, op1=ALU.add)
                        ev = e_[:, 0, :].rearrange("p (g k) -> p g k", k=8)
                        P.tt(ev, lg[:, 4:36].rearrange("p (g k) -> p g k", k=8),
                             r[:, 16:20].unsqueeze(2).to_broadcast([128, 4, 8]), ALU.add)
                        P.reduce(r[:, 20:21], e_[:, 0, :], op=ALU.max)
                        P.ts(e_[:, 1, :], e_[:, 0, :], r[:, 20:21], None, op0=ALU.is_equal)
                        P.stt(e_[:, 0, :], e_[:, 1, :], -1e30, e_[:, 0, :], ALU.mult, ALU.add)
                        P.reduce(r[:, 21:22], e_[:, 0, :], op=ALU.max)
                        P.ts(e_[:, 2, :], e_[:, 0, :], r[:, 21:22], None, op0=ALU.is_equal)
                        P.tt(r[:, 22:23], r[:, 20:21], r[:, 21:22], ALU.subtract)
                        P.act(r[:, 22:23], r[:, 22:23], AF.Exp, scale=-1.0)
                        P.ts(r[:, 22:23], r[:, 22:23], 1.0, None, op0=ALU.add)
                        P.recip(r[:, 22:23], r[:, 22:23])
                        P.tt(r[:, 22:23], r[:, 22:23], r[:, 7:8], ALU.mult)
                        P.tt(r[:, 23:24], r[:, 7:8], r[:, 22:23], ALU.subtract)
                        P.tt(asg[:, i, :], e_[:, 1, :], e_[:, 2, :], ALU.add)
                        P.copy(asgb[:, i, :], asg[:, i, :])
                        P.ts(wmat[:, i, :], e_[:, 1, :], r[:, 22:23], None, op0=ALU.mult)
                        P.stt(wmat[:, i, :], e_[:, 2, :], r[:, 23:24], wmat[:, i, :], ALU.mult, ALU.add)
                    for i in range(8):
                        for j in range(i):
                            P.mm(pb[3][:, 0:32], onesb[:], asgb[:, j, :], start=(j == 0), stop=False)
                        P.mm(pb[3][:, 0:32], tri[:], asgb[:, i, :], start=(i == 0), stop=True)
                        P.copy(pos[:, i, :], pb[3][:, 0:32])

                P.region(_router)

            Wg.append(P.sb([128, 16, EFF], BF16, "Wg1"))
            Wu.append(P.sb([128, 16, EFF], BF16, "Wu1"))
            Wd = [P.sb([128, 4, DM], BF16, f"Wd{i}") for i in range(1)]
            sel = [P.sb([128, 8, 128], BF16, f"sel{i}") for i in range(2)]
            selw = [P.sb([128, 8, 128], BF16, f"selw{i}") for i in range(2)]
            selwT = P.sb([128, 8, 128], BF16, "selwT")
            XeT = P.sb([128, 16, 128], BF16, "XeT")
            sgt = P.sb([128, 512], F32, "sgt")
            hidT = [P.sb([128, 4, 128], BF16, f"hidT{i}") for i in range(2)]
            ye = P.sb([128, DM], BF16, "ye")

            def load_e(e):
                P.dma("gpsimd", Wg[e % 2][:], L["e_gate"][e].rearrange("(c p) f -> p c f", p=128))
                P.dma("gpsimd", Wu[e % 2][:], L["e_up"][e].rearrange("(c p) f -> p c f", p=128))

            def stageA(e):
                par = e % 2
                if e + 1 < NEXP:
                    load_e(e + 1)
                sl = sel[par]; sw = selw[par]
                for i in range(8):
                    P.ts(sl[:, i, :], iota[:], pos[:, i, e:e + 1], asg[:, i, e:e + 1], op0=ALU.is_equal, op1=ALU.mult)
                    P.ts(sw[:, i, :], iota[:], pos[:, i, e:e + 1], wmat[:, i, e:e + 1], op0=ALU.is_equal, op1=ALU.mult)
                for c4 in range(4):
                    bk = pb[c4 % 2]
                    for kk in range(4):
                        c = c4 * 4 + kk
                        for i in range(8):
                            P.mm(bk[:, kk * 128:(kk + 1) * 128], h2[:, i, c * 128:(c + 1) * 128], sl[:, i, :],
                                 start=(i == 0), stop=(i == 7))
                    P.act(XeT[:, c4 * 4:(c4 + 1) * 4, :], bk[:].rearrange("p (a d) -> p a d", d=128), AF.Copy)
                for fc in range(4):
                    for c in range(16):
                        P.mm(pb[6][:, fc * 128:(fc + 1) * 128], Wg[par][:, c, fc * 128:(fc + 1) * 128], XeT[:, c, :],
                             start=(c == 0), stop=(c == 15))
                for fc in range(4):
                    for c in range(16):
                        P.mm(pb[7][:, fc * 128:(fc + 1) * 128], Wu[par][:, c, fc * 128:(fc + 1) * 128], XeT[:, c, :],
                             start=(c == 0), stop=(c == 15))
                P.act(sgt[:], pb[6][:], AF.Silu)
                P.tt(hidT[par][:].rearrange("p a d -> p (a d)"), sgt[:], pb[7][:], ALU.mult)

            def stageB(e):
                par = e % 2
                sw = selw[par]
                P.dma("gpsimd", Wd[0][:], L["e_down"][e].rearrange("(c p) n -> p c n", p=128))
                bT = pbb(5)
                for i in range(8):
                    P.tr(bT[:, i * 128:(i + 1) * 128], sw[:, i, :], identb[:])
                P.act(selwT[:].rearrange("p a d -> p (a d)"), bT[:, 0:1024], AF.Copy)
                for nb in range(4):
                    bk = pb[2 + nb % 2]
                    for fc in range(4):
                        P.mm(bk[:], hidT[par][:, fc, :], Wd[0][:, fc, nb * 512:(nb + 1) * 512], start=(fc == 0), stop=(fc == 3))
                    P.act(ye[:, nb * 512:(nb + 1) * 512], bk[:], AF.Copy)
                k = 0
                for i in range(8):
                    for nb in range(4):
                        bk = pb[2 + (k % 3)]
                        k += 1
                        P.mm(bk[:], selwT[:, i, :], ye[:, nb * 512:(nb + 1) * 512])
                        P.tt(x2[:, i, nb * 512:(nb + 1) * 512], x2[:, i, nb * 512:(nb + 1) * 512], bk[:], ALU.add)

            P.region(lambda: stageA(0))
            EW = 8
            for e0 in range(0, NEXP, EW):
                items = []
                for e in range(e0, min(e0 + EW, NEXP)):
                    items += P.record(lambda e=e: stageB(e))
                    if e + 1 < NEXP:
                        items += P.record(lambda e=e: stageA(e + 1))
                P.schedule(items)

        with P.scope():
            gfb = P.sb([128, DM], F32, "gfb")
            P.dma("sync", gfb[:], L["gf_d"].partition_broadcast(128))
            ot = [P.sb([128, DM], F32, f"ot{i}") for i in range(2)]
            junk = P.sb([128, DM], BF16, "junk5")
            rr = [P.sb([128, 4], F32, f"rf{i}") for i in range(2)]
            def _final():
                for i in range(8):
                    r = rr[i % 2]
                    P.act(junk[:], x2[:, i, :], AF.Square, accum_out=r[:, 0:1])
                    P.ts(r[:, 1:2], r[:, 0:1], 1.0 / DM, EPS, op0=ALU.mult, op1=ALU.add)
                    P.act(r[:, 2:3], r[:, 1:2], AF.Sqrt)
                    P.recip(r[:, 3:4], r[:, 2:3])
                    P.stt(ot[i % 2][:], x2[:, i, :], r[:, 3:4], gfb[:], ALU.mult, ALU.mult)
                    P.dma("sync", out[i * 128:(i + 1) * 128, :], ot[i % 2][:])

            P.region(_final)


def _constants(core_half):
    bf = ml_dtypes.bfloat16
    c = {}
    c["c_identb"] = np.eye(128, dtype=np.float32).astype(bf)
    c["c_identf"] = np.eye(128, dtype=np.float32)
    hp = np.arange(128) // 64
    sp = np.arange(128) % 64
    same = (hp[:, None] == hp[None, :])
    strict_bd = (same & (sp[:, None] < sp[None, :])).astype(np.float32)
    incl_c = (sp[:, None] <= np.arange(64)[None, :]).astype(np.float32)
    c["c_maskS"] = np.concatenate([strict_bd, incl_c, strict_bd, incl_c], axis=1)
    c["c_maskL"] = (same & (sp[None, :] < sp[:, None])).astype(np.float32)
    c["c_bones"] = same.astype(np.float32).astype(bf)
    c["c_hmask"] = np.stack([(hp == 0), (hp == 1)], axis=1).astype(np.float32)
    rs = np.ones((128, 1024), np.float32)
    rs[:, ::64] = 0.0
    c["c_reset"] = rs.astype(bf)
    pos = np.arange(T, dtype=np.float32) - (1024.0 if core_half == 0 else 0.0)
    inv = (10000.0 ** (-np.arange(64, dtype=np.float32) / 64)).astype(np.float32)
    ang = pos[:, None] * inv[None, :]
    cos, sin = np.cos(ang).astype(np.float32), np.sin(ang).astype(np.float32)
    sc = np.float32(128 ** -0.5)
    cs = np.stack([cos, sin, cos * sc, sin * sc], axis=1)
    c["c_cs"] = np.ascontiguousarray(cs.reshape(16, 128, 4, 64).transpose(1, 0, 2, 3))
    lg = np.log1p(-np.exp2(-5.0 - np.arange(8, dtype=np.float64)))
    idx = np.arange(128, dtype=np.float64)
    j = idx[:, None]
    i = idx[None, :]
    same_c = (j // 64 == i // 64)
    rm = np.zeros((128, 8, 128), np.float64)
    for h in range(8):
        m = np.where(same_c, np.exp(np.abs(i - j) * lg[h]), np.where(j < i, np.exp((i - j) * lg[h]), 0.0))
        rm[:, h, :] = m * np.exp(-(i + 1) * lg[h])
    c["c_rmask"] = rm.astype(np.float32)
    rd = np.zeros((128, 16), np.float64)
    for h in range(8):
        rd[:, h] = np.exp((idx + 1) * lg[h])
        rd[:, 8 + h] = np.exp((127 - idx) * lg[h])
    c["c_rdec"] = rd.astype(np.float32)
    c["c_tri"] = (idx[:, None] < idx[None, :]).astype(np.float32).astype(bf)
    c["c_iota"] = np.tile(np.arange(128, dtype=np.float32)[None, :], (128, 1))
    return c


def _prep_shared(inp):
    f = np.float32
    w_in = np.asarray(inp["w_in"][0], f)
    d = {}
    d["g1T"] = np.ascontiguousarray(np.asarray(inp["norm1_g"][0], f).reshape(16, 128).T)
    d["w_lora"] = np.ascontiguousarray(w_in[:, 3072:3520])
    d["w_rw"] = np.ascontiguousarray(np.stack(
        [np.concatenate([w_in[:, k * 1024 + p * 128:k * 1024 + (p + 1) * 128] for k in range(3)], axis=1) for p in range(8)]))
    ro = RET_OFF
    d["w_ret"] = np.ascontiguousarray(np.stack(
        [np.concatenate([w_in[:, ro + h * 128:ro + (h + 1) * 128],
                         w_in[:, ro + 1024 + h * 128:ro + 1024 + (h + 1) * 128],
                         w_in[:, ro + 2048 + h * 256:ro + 2048 + (h + 1) * 256],
                         w_in[:, ro + 4096 + h * 256:ro + 4096 + (h + 1) * 256]], axis=1) for h in range(8)]))
    d["w_in"] = w_in
    mu = np.asarray(inp["mu_shift"][0], f)
    lp = np.zeros((128, 4), f)
    lp[:96, 0] = mu[3072:3168]
    lp[:96, 1] = mu[3168:3264]
    lp[:, 2] = mu[3264:3392]
    lp[:, 3] = mu[3392:3520]
    d["lprm"] = lp
    vecs = [mu[0:1024], mu[1024:2048], mu[2048:3072], np.asarray(inp["rw_w0"][0], f), np.asarray(inp["rw_a0"][0], f),
            np.asarray(inp["rw_k_k"][0], f), np.asarray(inp["rw_k_a"][0], f), np.asarray(inp["rw_r_k"][0], f)]
    d["rwprm"] = np.ascontiguousarray(np.stack([v.reshape(8, 128).T for v in vecs], axis=2))
    d["rw_w2"] = np.asarray(inp["rw_w2"][0], f)
    d["rw_a2"] = np.asarray(inp["rw_a2"][0], f)
    d["rw_g2"] = np.asarray(inp["rw_g2"][0], f)
    d["lnx_g"] = np.asarray(inp["rw_lnx_g"][0], f).reshape(1, 1024)
    d["lnx_b"] = np.asarray(inp["rw_lnx_b"][0], f).reshape(1, 1024)
    d["ret_g"] = np.asarray(inp["ret_norm_g"][0], f).reshape(1, 2048)
    d["w_up_a"] = np.asarray(inp["w_up_a"][0], f)
    d["w_up_b"] = np.asarray(inp["w_up_b"][0], f)
    d["w_out"] = np.asarray(inp["w_out"][0], f)
    d["g2"] = np.asarray(inp["norm2_g"][0], f).reshape(1, DM)
    d["gf"] = np.asarray(inp["final_norm_g"], f).reshape(1, DM)
    d["w_rt"] = np.ascontiguousarray(np.concatenate([np.asarray(inp["w_group"][0], f), np.asarray(inp["w_expert"][0], f)], axis=1))
    d["b_rt"] = np.concatenate([np.asarray(inp["b_group"][0], f), np.asarray(inp["b_expert"][0], f)]).reshape(1, 36)
    d["e_gate"] = np.asarray(inp["e_gate"][0], f)
    d["e_up"] = np.asarray(inp["e_up"][0], f)
    d["e_down"] = np.asarray(inp["e_down"][0], f)
    return d


_CACHE = {}


def run(inp, stop_after=99, cores=None, trace=False):
    key = stop_after
    if key not in _CACHE:
        _CACHE[key] = build_program(stop_after)
    nc, declared = _CACHE[key]
    shared = _prep_shared(inp)
    consts = [_constants(0), _constants(1)]
    xfull = np.asarray(inp["x"], np.float32)
    cores = list(range(8)) if cores is None else cores
    in_maps = []
    for c in cores:
        b, half = c // 2, c % 2
        if half == 0:
            xs = np.concatenate([np.zeros((1024, DM), np.float32), xfull[b, :1024]], axis=0)
        else:
            xs = xfull[b]
        m = {"x": np.ascontiguousarray(xs)}
        m.update(shared)
        m.update(consts[half])
        in_maps.append({k: m[k] for k in declared})
    res = run_bass_kernel_spmd(nc, in_maps, core_ids=list(range(len(cores))), trace=trace)
    return res


def kernel(**inputs):
    res = run(inputs)
    outp = np.zeros((4, 2048, DM), np.float32)
    for c in range(8):
        b, half = c // 2, c % 2
        outp[b, half * 1024:(half + 1) * 1024] = res.results[c]["out"]
    return outp
```
